# Optimizing a Trainium2 kernel written in Bass

```python
import math
import jax
import jax.numpy as jnp
from jax import lax
import numpy as np

D_MODEL = 2048
BATCH = 2
SEQ = 8192
DEPTH = 1

D_MIX = D_MODEL
HEAD_DIM = 64
NSA_HEADS = 16
NSA_KV_GROUPS = 2
NSA_REP = NSA_HEADS // NSA_KV_GROUPS
NSA_WIDTH = NSA_HEADS * HEAD_DIM
KV_WIDTH = NSA_KV_GROUPS * HEAD_DIM
CMP_BLOCK = 32
CMP_STRIDE = 16
CMP_HIDDEN = 256
SEL_BLOCK = 64
SEL_TOPK = 16
WINDOW = 512
Q_BLOCK = 128
FORCED_SCORE = 1.0e4
SSM_WIDTH = D_MIX - NSA_WIDTH
SSM_HEADDIM = 64
SSM_HEADS = SSM_WIDTH // SSM_HEADDIM
SSM_GROUPS = 4
SSM_REP = SSM_HEADS // SSM_GROUPS
SSM_STATE = 128
CONV_WIDTH = 4
CHUNK = 256
XBC_WIDTH = SSM_WIDTH + 2 * SSM_GROUPS * SSM_STATE
ROPE_THETA = 500000.0
ROT_DIM = HEAD_DIM // 4
N_EXPERT_GROUPS = 4
EXPERTS_PER_GROUP = 8
N_EXPERTS = N_EXPERT_GROUPS * EXPERTS_PER_GROUP
EXPERT_TOPK = 2
D_EXPERT = 512
MOE_BLOCK = 128
DEEPNORM_ALPHA = (2 * DEPTH) ** 0.25
DEEPNORM_BETA = (8 * DEPTH) ** -0.25
NORM_EPS = 1e-5
PROJ_WIDTHS = (NSA_WIDTH, 6 * KV_WIDTH, 3 * NSA_HEADS, SSM_WIDTH, XBC_WIDTH, SSM_HEADS)
D_IN_PROJ = sum(PROJ_WIDTHS)

kernel_name = "hymba_nsa_mamba2_hmoe_deepnorm_block"


def layer_norm(x, g, b):
    xf = x.astype(jnp.float32)
    mu = xf.mean(-1, keepdims=True)
    var = jnp.square(xf - mu).mean(-1, keepdims=True)
    return ((xf - mu) * lax.rsqrt(var + NORM_EPS) * g + b).astype(x.dtype)


def rope_tables(pos):
    inv_freq = ROPE_THETA ** (-jnp.arange(0, ROT_DIM, 2, dtype=jnp.float32) / ROT_DIM)
    ang = pos.astype(jnp.float32)[..., None] * inv_freq
    return jnp.cos(ang), jnp.sin(ang)


def apply_partial_rope(x, cos, sin):
    half = ROT_DIM // 2
    x1, x2, rest = x[..., :half], x[..., half:ROT_DIM], x[..., ROT_DIM:]
    out = jnp.concatenate([x1 * cos - x2 * sin, x2 * cos + x1 * sin, rest.astype(jnp.float32)], axis=-1)
    return out.astype(x.dtype)


def masked_softmax(s, mask):
    s = jnp.where(mask, s.astype(jnp.float32), jnp.finfo(jnp.float32).min)
    e = jnp.where(mask, jnp.exp(s - s.max(-1, keepdims=True)), 0.0)
    return e / jnp.maximum(e.sum(-1, keepdims=True), jnp.finfo(jnp.float32).tiny)


def compress_blocks(blocks, pe, w1, b1, w2):
    bsz, g, n = blocks.shape[:3]
    h = (blocks + pe).reshape(bsz, g, n, CMP_BLOCK * HEAD_DIM)
    return jax.nn.gelu(h @ w1 + b1) @ w2


def nsa_mixer(q, k_cmp, v_cmp, k_sel, v_sel, k_win, v_win, gate_logits, positions,
              cmp_k_pe, cmp_k_w1, cmp_k_b1, cmp_k_w2, cmp_v_pe, cmp_v_w1, cmp_v_b1, cmp_v_w2):
    b, t = q.shape[:2]
    cos, sin = rope_tables(positions)
    cos_h, sin_h = cos[:, :, None], sin[:, :, None]
    q = apply_partial_rope(q, cos_h, sin_h)
    k_sel = apply_partial_rope(k_sel, cos_h, sin_h)
    k_win = apply_partial_rope(k_win, cos_h, sin_h)
    q = q.reshape(b, t, NSA_KV_GROUPS, NSA_REP, HEAD_DIM).transpose(0, 2, 3, 1, 4)
    k_cmp, v_cmp, k_sel, v_sel, k_win, v_win = [a.transpose(0, 2, 1, 3) for a in (k_cmp, v_cmp, k_sel, v_sel, k_win, v_win)]
    gates = jax.nn.sigmoid(gate_logits.astype(jnp.float32)).reshape(b, t, NSA_KV_GROUPS, NSA_REP, 3).transpose(0, 2, 3, 1, 4)
    scale = HEAD_DIM ** -0.5

    n_cmp = (t - CMP_BLOCK) // CMP_STRIDE + 1
    cmp_idx = jnp.arange(n_cmp)[:, None] * CMP_STRIDE + jnp.arange(CMP_BLOCK)[None, :]
    cmp_end = cmp_idx[:, -1]
    kc = compress_blocks(k_cmp[:, :, cmp_idx], cmp_k_pe, cmp_k_w1, cmp_k_b1, cmp_k_w2)
    vc = compress_blocks(v_cmp[:, :, cmp_idx], cmp_v_pe, cmp_v_w1, cmp_v_b1, cmp_v_w2)
    cos_c, sin_c = rope_tables(positions[:, cmp_end])
    kc = apply_partial_rope(kc, cos_c[:, None], sin_c[:, None])

    n_sel = t // SEL_BLOCK
    sel_k = min(SEL_TOPK, n_sel)
    c_start = jnp.arange(n_cmp) * CMP_STRIDE
    s_start = jnp.arange(n_sel) * SEL_BLOCK
    cover = ((c_start[:, None] < s_start[None, :] + SEL_BLOCK) &
             (c_start[:, None] + CMP_BLOCK > s_start[None, :])).astype(jnp.float32)
    k_blocks = k_sel.reshape(b, NSA_KV_GROUPS, n_sel, SEL_BLOCK * HEAD_DIM)
    v_blocks = v_sel.reshape(b, NSA_KV_GROUPS, n_sel, SEL_BLOCK * HEAD_DIM)

    k_pad = jnp.pad(k_win, ((0, 0), (0, 0), (WINDOW, 0), (0, 0)))
    v_pad = jnp.pad(v_win, ((0, 0), (0, 0), (WINDOW, 0), (0, 0)))
    sel_j = jnp.arange(n_sel)

    def query_block(start):
        tq = start + jnp.arange(Q_BLOCK)
        qb = lax.dynamic_slice_in_dim(q, start, Q_BLOCK, axis=3)
        s_c = jnp.einsum('bgrqd,bgcd->bgrqc', qb, kc) * scale
        p_c = masked_softmax(s_c, cmp_end[None, :] <= tq[:, None])
        o_c = jnp.einsum('bgrqc,bgcd->bgrqd', p_c, vc)
        imp = jnp.einsum('bgrqc,cj->bgqj', p_c, cover)
        cur = tq // SEL_BLOCK
        forced = (sel_j[None] == 0) | (sel_j[None] == cur[:, None]) | (sel_j[None] == cur[:, None] - 1)
        valid = sel_j[None] * SEL_BLOCK <= tq[:, None]
        imp = jnp.where(valid, jnp.where(forced, FORCED_SCORE, imp), -FORCED_SCORE)
        _, sel_idx = lax.top_k(imp, sel_k)
        flat = sel_idx.reshape(b, NSA_KV_GROUPS, Q_BLOCK * sel_k)[..., None]
        ks = jnp.take_along_axis(k_blocks, flat, axis=2).reshape(b, NSA_KV_GROUPS, Q_BLOCK, sel_k * SEL_BLOCK, HEAD_DIM)
        vs = jnp.take_along_axis(v_blocks, flat, axis=2).reshape(b, NSA_KV_GROUPS, Q_BLOCK, sel_k * SEL_BLOCK, HEAD_DIM)
        pos_s = (sel_idx[..., None] * SEL_BLOCK + jnp.arange(SEL_BLOCK)).reshape(b, NSA_KV_GROUPS, Q_BLOCK, sel_k * SEL_BLOCK)
        s_s = jnp.einsum('bgrqd,bgqkd->bgrqk', qb, ks) * scale
        p_s = masked_softmax(s_s, (pos_s <= tq[None, None, :, None])[:, :, None])
        o_s = jnp.einsum('bgrqk,bgqkd->bgrqd', p_s, vs)
        kw = lax.dynamic_slice_in_dim(k_pad, start, WINDOW + Q_BLOCK, axis=2)
        vw = lax.dynamic_slice_in_dim(v_pad, start, WINDOW + Q_BLOCK, axis=2)
        pos_w = start - WINDOW + jnp.arange(WINDOW + Q_BLOCK)
        m_w = (pos_w[None] <= tq[:, None]) & (pos_w[None] > tq[:, None] - WINDOW) & (pos_w[None] >= 0)
        s_w = jnp.einsum('bgrqd,bgkd->bgrqk', qb, kw) * scale
        o_w = jnp.einsum('bgrqk,bgkd->bgrqd', masked_softmax(s_w, m_w), vw)
        gb = lax.dynamic_slice_in_dim(gates, start, Q_BLOCK, axis=3)
        return gb[..., 0:1] * o_c + gb[..., 1:2] * o_s + gb[..., 2:3] * o_w

    starts = jnp.arange(t // Q_BLOCK) * Q_BLOCK
    out = lax.map(query_block, starts)
    return out.transpose(1, 0, 4, 2, 3, 5).reshape(b, t, NSA_WIDTH)


def ssd_chunked(xs, dt, a, bm, cm):
    b, t = xs.shape[:2]
    q_len = math.gcd(CHUNK, t)
    c = t // q_len
    x = xs.astype(jnp.float32).reshape(b, c, q_len, SSM_GROUPS, SSM_REP, SSM_HEADDIM)
    bc = bm.astype(jnp.float32).reshape(b, c, q_len, SSM_GROUPS, SSM_STATE)
    cc = cm.astype(jnp.float32).reshape(b, c, q_len, SSM_GROUPS, SSM_STATE)
    dtc = dt.reshape(b, c, q_len, SSM_GROUPS, SSM_REP).transpose(0, 1, 3, 4, 2)
    a_cs = jnp.cumsum(dtc * a.reshape(SSM_GROUPS, SSM_REP)[:, :, None], axis=-1)
    seg = a_cs[..., :, None] - a_cs[..., None, :]
    causal = jnp.tril(jnp.ones((q_len, q_len), dtype=bool))
    decay = jnp.where(causal, jnp.exp(jnp.where(causal, seg, 0.0)), 0.0)
    cb = jnp.einsum('bclgn,bcsgn->bcgls', cc, bc)
    w_diag = cb[:, :, :, None] * decay * dtc[..., None, :]
    y_diag = jnp.einsum('bcgrls,bcsgrp->bclgrp', w_diag, x)
    decay_to_end = jnp.exp(a_cs[..., -1:] - a_cs)
    states = jnp.einsum('bcsgn,bcgrs,bcsgrp->bcgrpn', bc, decay_to_end * dtc, x)
    chunk_decay = jnp.exp(a_cs[..., -1])

    def step(h, inp):
        dec, st = inp
        return dec[..., None, None] * h + st, h

    h0 = jnp.zeros((b, SSM_GROUPS, SSM_REP, SSM_HEADDIM, SSM_STATE), jnp.float32)
    _, prev = lax.scan(step, h0, (chunk_decay.transpose(1, 0, 2, 3), states.transpose(1, 0, 2, 3, 4, 5)))
    prev = prev.transpose(1, 0, 2, 3, 4, 5)
    y_off = jnp.einsum('bclgn,bcgrpn,bcgrl->bclgrp', cc, prev, jnp.exp(a_cs))
    return (y_diag + y_off).reshape(b, t, SSM_HEADS, SSM_HEADDIM)


def mamba2_mixer(z, xbc, dt_raw, conv_w, conv_b, dt_bias, a_log, d_skip, norm_w):
    b, t = z.shape[:2]
    xbc = lax.conv_general_dilated(xbc, conv_w, window_strides=(1,), padding=[(CONV_WIDTH - 1, 0)],
                                   dimension_numbers=('NWC', 'WIO', 'NWC'), feature_group_count=XBC_WIDTH) + conv_b
    xbc = jax.nn.silu(xbc)
    xs, bm, cm = jnp.split(xbc, [SSM_WIDTH, SSM_WIDTH + SSM_GROUPS * SSM_STATE], axis=-1)
    xs = xs.reshape(b, t, SSM_HEADS, SSM_HEADDIM)
    bm = bm.reshape(b, t, SSM_GROUPS, SSM_STATE)
    cm = cm.reshape(b, t, SSM_GROUPS, SSM_STATE)
    dt = jax.nn.softplus(dt_raw.astype(jnp.float32) + dt_bias.astype(jnp.float32))
    a = -jnp.exp(a_log.astype(jnp.float32))
    y = ssd_chunked(xs, dt, a, bm, cm) + d_skip[:, None] * xs
    y = y.reshape(b, t, SSM_WIDTH) * jax.nn.silu(z.astype(jnp.float32))
    yg = y.reshape(b, t, SSM_GROUPS, SSM_WIDTH // SSM_GROUPS)
    yg = yg * lax.rsqrt(jnp.mean(jnp.square(yg), axis=-1, keepdims=True) + NORM_EPS)
    return (yg.reshape(b, t, SSM_WIDTH) * norm_w).astype(z.dtype)


def hierarchical_moe(h, w_router_group, b_router_group, w_router_expert, b_router_expert, w_gate, w_up, w_down):
    n, d = h.shape
    p_group = jax.nn.softmax((h @ w_router_group).astype(jnp.float32) + b_router_group, axis=-1)
    g_sel = jnp.argmax(p_group, axis=-1)
    g_gate = jnp.take_along_axis(p_group, g_sel[:, None], axis=-1)
    e_logits = ((h @ w_router_expert).astype(jnp.float32) + b_router_expert).reshape(n, N_EXPERT_GROUPS, EXPERTS_PER_GROUP)
    e_logits = jnp.take_along_axis(e_logits, g_sel[:, None, None], axis=1)[:, 0]
    top_p, top_i = lax.top_k(jax.nn.softmax(e_logits, axis=-1), EXPERT_TOPK)
    weights = g_gate * top_p / top_p.sum(-1, keepdims=True)
    flat_e = (g_sel[:, None] * EXPERTS_PER_GROUP + top_i).reshape(-1).astype(jnp.int32)
    flat_w = weights.reshape(-1)
    flat_tok = jnp.repeat(jnp.arange(n, dtype=jnp.int32), EXPERT_TOPK)
    n_assign = n * EXPERT_TOPK
    order = jnp.argsort(flat_e)
    sorted_e = flat_e[order]
    counts = jax.ops.segment_sum(jnp.ones_like(flat_e), flat_e, num_segments=N_EXPERTS)
    starts = jnp.cumsum(counts) - counts
    padded = (counts + MOE_BLOCK - 1) // MOE_BLOCK * MOE_BLOCK
    pad_ends = jnp.cumsum(padded)
    pad_starts = pad_ends - padded
    dest = pad_starts[sorted_e] + jnp.arange(n_assign, dtype=jnp.int32) - starts[sorted_e]
    buf_len = -(-n_assign // MOE_BLOCK) * MOE_BLOCK + N_EXPERTS * MOE_BLOCK
    n_blocks = buf_len // MOE_BLOCK
    buf_tok = jnp.zeros((buf_len,), jnp.int32).at[dest].set(flat_tok[order])
    buf_w = jnp.zeros((buf_len,), jnp.float32).at[dest].set(flat_w[order])
    block_start = jnp.arange(n_blocks, dtype=jnp.int32) * MOE_BLOCK
    block_e = jnp.minimum((block_start[:, None] >= pad_ends[None, :]).sum(-1), N_EXPERTS - 1)

    def expert_block(args):
        tok, e = args
        xb = h[tok]
        return (jax.nn.silu(xb @ w_gate[e]) * (xb @ w_up[e])) @ w_down[e]

    y = lax.map(expert_block, (buf_tok.reshape(n_blocks, MOE_BLOCK), block_e))
    out = jnp.zeros((n, d), jnp.float32).at[buf_tok].add(y.reshape(buf_len, d) * buf_w[:, None])
    return out.astype(h.dtype)


def hybrid_layer(x, positions, w_in, cmp_k_pe, cmp_k_w1, cmp_k_b1, cmp_k_w2, cmp_v_pe, cmp_v_w1, cmp_v_b1, cmp_v_w2,
                 conv_w, conv_b, dt_bias, a_log, d_skip, ssm_norm_w, w_out, ln1_g, ln1_b,
                 w_router_group, b_router_group, w_router_expert, b_router_expert, w_gate, w_up, w_down, ln2_g, ln2_b):
    b, t, d = x.shape
    proj = x @ w_in
    cuts = np.cumsum(PROJ_WIDTHS)[:-1].tolist()
    q, kv, gate_logits, z, xbc, dt_raw = jnp.split(proj, cuts, axis=-1)
    q = q.reshape(b, t, NSA_HEADS, HEAD_DIM)
    k_cmp, v_cmp, k_sel, v_sel, k_win, v_win = [a.reshape(b, t, NSA_KV_GROUPS, HEAD_DIM) for a in jnp.split(kv, 6, axis=-1)]
    y_nsa = nsa_mixer(q, k_cmp, v_cmp, k_sel, v_sel, k_win, v_win, gate_logits, positions,
                      cmp_k_pe, cmp_k_w1, cmp_k_b1, cmp_k_w2, cmp_v_pe, cmp_v_w1, cmp_v_b1, cmp_v_w2)
    y_ssm = mamba2_mixer(z, xbc, dt_raw, conv_w, conv_b, dt_bias, a_log, d_skip, ssm_norm_w)
    mix = jnp.concatenate([y_nsa.astype(x.dtype), y_ssm.astype(x.dtype)], axis=-1) @ w_out
    h = layer_norm(DEEPNORM_ALPHA * x + mix, ln1_g, ln1_b)
    ffn = hierarchical_moe(h.reshape(b * t, d), w_router_group, b_router_group, w_router_expert, b_router_expert,
                           w_gate, w_up, w_down).reshape(b, t, d)
    return layer_norm(DEEPNORM_ALPHA * h + ffn, ln2_g, ln2_b)


def setup_inputs(seed: int = 0) -> dict:
    key = jax.random.key(seed)
    ks = jax.random.split(key, 32)
    f32 = jnp.float32
    nrm = lambda k, shape, s: jax.random.normal(k, shape, f32) * s
    x = jax.random.normal(ks[0], (BATCH, SEQ, D_MODEL), f32)
    offset = jax.random.randint(ks[1], (BATCH, 1), 0, 4096, dtype=jnp.int32)
    positions = offset + jnp.arange(SEQ, dtype=jnp.int32)[None, :]
    dt_init = jnp.exp(jax.random.uniform(ks[2], (DEPTH, SSM_HEADS), f32, math.log(1e-3), math.log(1e-1)))
    return {
        'x': x,
        'positions': positions,
        'w_in': nrm(ks[3], (DEPTH, D_MODEL, D_IN_PROJ), D_MODEL ** -0.5),
        'cmp_k_pe': nrm(ks[4], (DEPTH, CMP_BLOCK, HEAD_DIM), 0.1),
        'cmp_k_w1': nrm(ks[5], (DEPTH, CMP_BLOCK * HEAD_DIM, CMP_HIDDEN), (CMP_BLOCK * HEAD_DIM) ** -0.5),
        'cmp_k_b1': nrm(ks[6], (DEPTH, CMP_HIDDEN), 0.01),
        'cmp_k_w2': nrm(ks[7], (DEPTH, CMP_HIDDEN, HEAD_DIM), CMP_HIDDEN ** -0.5),
        'cmp_v_pe': nrm(ks[8], (DEPTH, CMP_BLOCK, HEAD_DIM), 0.1),
        'cmp_v_w1': nrm(ks[9], (DEPTH, CMP_BLOCK * HEAD_DIM, CMP_HIDDEN), (CMP_BLOCK * HEAD_DIM) ** -0.5),
        'cmp_v_b1': nrm(ks[10], (DEPTH, CMP_HIDDEN), 0.01),
        'cmp_v_w2': nrm(ks[11], (DEPTH, CMP_HIDDEN, HEAD_DIM), CMP_HIDDEN ** -0.5),
        'conv_w': nrm(ks[12], (DEPTH, CONV_WIDTH, 1, XBC_WIDTH), CONV_WIDTH ** -0.5),
        'conv_b': nrm(ks[13], (DEPTH, XBC_WIDTH), 0.01),
        'dt_bias': dt_init + jnp.log(-jnp.expm1(-dt_init)),
        'a_log': jnp.log(jax.random.uniform(ks[14], (DEPTH, SSM_HEADS), f32, 1.0, 16.0)),
        'd_skip': 1.0 + nrm(ks[15], (DEPTH, SSM_HEADS), 0.1),
        'ssm_norm_w': 1.0 + nrm(ks[16], (DEPTH, SSM_WIDTH), 0.02),
        'w_out': nrm(ks[17], (DEPTH, D_MIX, D_MODEL), D_MIX ** -0.5 * DEEPNORM_BETA),
        'ln1_g': 1.0 + nrm(ks[18], (DEPTH, D_MODEL), 0.02),
        'ln1_b': nrm(ks[19], (DEPTH, D_MODEL), 0.02),
        'w_router_group': nrm(ks[20], (DEPTH, D_MODEL, N_EXPERT_GROUPS), D_MODEL ** -0.5),
        'b_router_group': nrm(ks[21], (DEPTH, N_EXPERT_GROUPS), 0.01),
        'w_router_expert': nrm(ks[22], (DEPTH, D_MODEL, N_EXPERTS), D_MODEL ** -0.5),
        'b_router_expert': nrm(ks[23], (DEPTH, N_EXPERTS), 0.01),
        'w_gate': nrm(ks[24], (DEPTH, N_EXPERTS, D_MODEL, D_EXPERT), D_MODEL ** -0.5),
        'w_up': nrm(ks[25], (DEPTH, N_EXPERTS, D_MODEL, D_EXPERT), D_MODEL ** -0.5),
        'w_down': nrm(ks[26], (DEPTH, N_EXPERTS, D_EXPERT, D_MODEL), D_EXPERT ** -0.5 * DEEPNORM_BETA),
        'ln2_g': 1.0 + nrm(ks[27], (DEPTH, D_MODEL), 0.02),
        'ln2_b': nrm(ks[28], (DEPTH, D_MODEL), 0.02),
    }


def reference(x, positions, w_in, cmp_k_pe, cmp_k_w1, cmp_k_b1, cmp_k_w2, cmp_v_pe, cmp_v_w1, cmp_v_b1, cmp_v_w2,
              conv_w, conv_b, dt_bias, a_log, d_skip, ssm_norm_w, w_out, ln1_g, ln1_b,
              w_router_group, b_router_group, w_router_expert, b_router_expert, w_gate, w_up, w_down, ln2_g, ln2_b):
    for l in range(DEPTH):
        x = hybrid_layer(x, positions, w_in[l], cmp_k_pe[l], cmp_k_w1[l], cmp_k_b1[l], cmp_k_w2[l],
                         cmp_v_pe[l], cmp_v_w1[l], cmp_v_b1[l], cmp_v_w2[l],
                         conv_w[l], conv_b[l], dt_bias[l], a_log[l], d_skip[l], ssm_norm_w[l], w_out[l],
                         ln1_g[l], ln1_b[l], w_router_group[l], b_router_group[l], w_router_expert[l],
                         b_router_expert[l], w_gate[l], w_up[l], w_down[l], ln2_g[l], ln2_b[l])
    return x
```

```python
import numpy as np
from contextlib import ExitStack
import concourse.bass as bass
import concourse.mybir as mybir
from concourse.bass_utils import run_bass_kernel_spmd

F32 = mybir.dt.float32
BF16 = mybir.dt.bfloat16
I32 = mybir.dt.int32
AF = mybir.ActivationFunctionType
ALU = mybir.AluOpType
AX = mybir.AxisListType

D = 2048
NEG = -30000.0
ALPHA = 2.0 ** 0.25
EPS = 1e-5
C_Q, C_KV, C_GATE, C_Z, C_XS, C_B, C_C, C_DT = 0, 1024, 1792, 1840, 2864, 3888, 4400, 4912


class Prog:
    ENGS = ("tensor", "vector", "scalar", "gpsimd", "sync")

    def __init__(self, nc, stack):
        self.nc = nc
        self.ops = {e: [] for e in self.ENGS}
        self.cnt = {e: 0 for e in self.ENGS}
        self.seen = {e: {} for e in self.ENGS}
        self.last_w = {}
        self.readers = {}
        self.ndma = 0
        self.DMAK = 32
        self.dma_cnt = [0] * self.DMAK
        self.sems = {}
        for e in self.ENGS:
            self.sems[e] = stack.enter_context(nc.semaphore("s_" + e))
        for i in range(self.DMAK):
            self.sems[("dma", i)] = stack.enter_context(nc.semaphore("s_dma%d" % i))
        self.nphase = 0

    def _need(self, eng, dep):
        key, val = dep
        if key == eng and eng == "tensor":
            return
        if self.seen[eng].get(key, 0) >= val:
            return
        self.seen[eng][key] = val
        self.ops[eng].append(("wait", (key, val)))

    def _deps(self, eng, reads, writes):
        for r in reads:
            if r in self.last_w:
                self._need(eng, self.last_w[r])
        for w in writes:
            if w in self.last_w:
                self._need(eng, self.last_w[w])
            for k, v in self.readers.get(w, {}).items():
                self._need(eng, (k, v))

    def _mark(self, me, reads, writes):
        for r in reads:
            d = self.readers.setdefault(r, {})
            if d.get(me[0], 0) < me[1]:
                d[me[0]] = me[1]
        for w in writes:
            self.last_w[w] = me
            self.readers[w] = {}

    @staticmethod
    def _excl(reads, writes):
        def isp(r):
            n = r[0] if isinstance(r, tuple) else r
            return isinstance(n, str) and n.startswith("p_")
        ex = [r for r in reads if isp(r)]
        if ex:
            reads = [r for r in reads if not isp(r)]
            writes = list(writes) + ex
        return reads, writes

    def op(self, eng, fn, reads=(), writes=()):
        reads, writes = self._excl(reads, writes)
        self._deps(eng, reads, writes)
        self.cnt[eng] += 1
        me = (eng, self.cnt[eng])
        self.ops[eng].append(("op", fn))
        self._mark(me, reads, writes)

    def dma(self, eng, out, in_, reads=(), writes=()):
        slot = self.ndma % self.DMAK
        self.ndma += 1
        key = ("dma", slot)
        if self.dma_cnt[slot] > 0:
            self._need(eng, (key, 16 * self.dma_cnt[slot]))
        self._deps(eng, reads, writes)
        self.dma_cnt[slot] += 1
        me = (key, 16 * self.dma_cnt[slot])
        self.ops[eng].append(("dma", (out, in_, slot)))
        self._mark(me, reads, writes)

    def end_phase(self):
        for slot in range(self.DMAK):
            if self.dma_cnt[slot] > 0:
                self._need("sync", (("dma", slot), 16 * self.dma_cnt[slot]))
        nc = self.nc
        sems = self.sems
        ops = self.ops
        with nc.Block() as block:
            def mk(e):
                def body(engine):
                    for kind, p in ops[e]:
                        if kind == "wait":
                            engine.wait_ge(sems[p[0]], p[1])
                        elif kind == "op":
                            p(engine).then_inc(sems[e], 1)
                        else:
                            out, in_, slot = p
                            engine.dma_start(out=out, in_=in_).then_inc(sems[("dma", slot)], 16)
                return body
            for e in self.ENGS:
                if ops[e]:
                    getattr(block, e)(mk(e))
        self.ops = {e: [] for e in self.ENGS}
        self.last_w = {}
        self.readers = {}
        self.nphase += 1


class KB:
    def __init__(self, nc, P):
        self.nc = nc
        self.P = P

    def e(self, eng, method, *args, r=(), w=(), **kw):
        self.P.op(eng, lambda en: getattr(en, method)(*args, **kw), r, w)

    def mm(self, out, lhsT, rhs, start, stop, r=(), w=()):
        self.P.op("tensor", lambda en: en.matmul(out, lhsT=lhsT, rhs=rhs, start=start, stop=stop,
                                                 skip_group_check=True), r, w)

    def tr(self, out, in_, ident, r=(), w=()):
        self.P.op("tensor", lambda en: en.transpose(out, in_, ident), r, w)

    def act(self, out, in_, func, r=(), w=(), **kw):
        self.P.op("scalar", lambda en: en.activation(out=out, in_=in_, func=func, **kw), r, w)

    def dma(self, out, in_, r=(), w=(), eng="sync"):
        self.P.dma(eng, out, in_, r, w)


def bc(ap, shape):
    return ap.broadcast_to(list(shape))


def make_consts():
    c = {}
    k = np.arange(128)[:, None]
    q = np.arange(128)[None, :]
    c["ident"] = np.eye(128, dtype=np.float32)
    c["triL"] = (k <= q).astype(np.float32)
    c["triU"] = (k > q).astype(np.float32)
    c["triS"] = (k < q).astype(np.float32)
    l = np.arange(256)[None, :]
    c["triA"] = (k <= l).astype(np.float32)
    c["triB"] = ((k + 128) <= l).astype(np.float32)
    c["negA"] = np.where(k <= l, 0.0, NEG).astype(np.float32)
    c["negB"] = np.where((k + 128) <= l, 0.0, NEG).astype(np.float32)
    c["ones"] = np.ones((128, 128), np.float32)
    c["iota_row"] = np.broadcast_to(np.arange(128, dtype=np.float32)[None, :], (128, 128)).copy()
    c["iota_col"] = np.arange(128, dtype=np.float32)[:, None].copy()
    c["qhalf"] = (np.arange(128) >= 64).astype(np.float32)[:, None].copy()
    key = np.arange(8192)[None, :]
    c["ebig"] = ((key // 64) == k).astype(np.float32)
    n = np.arange(512)[:, None]
    b = np.arange(128)[None, :]
    cover = ((n >= 4 * b - 1) & (n <= 4 * b + 3)).astype(np.float32)
    c["cover"] = cover.reshape(4, 128, 128).transpose(1, 0, 2).copy()
    cm = np.zeros((128, 4, 128), np.float32)
    for v in range(4):
        o = 48 + 64 * (v // 2) + 8 * (v % 2)
        cm[:, v, :] = ((16 * k + 31) <= (16 * o + q)).astype(np.float32)
    c["cmpmask"] = cm
    inv = (np.float32(500000.0) ** (-np.arange(0, 16, 2, dtype=np.float32) / np.float32(16))).astype(np.float32)
    c["invfreq"] = np.broadcast_to(inv[None, :], (128, 8)).copy()
    return c


CONST_SHAPES = {
    "ident": [128, 128], "triL": [128, 128], "triU": [128, 128], "triS": [128, 128],
    "triA": [128, 256], "triB": [128, 256], "negA": [128, 256], "negB": [128, 256],
    "ones": [128, 128], "iota_row": [128, 128], "iota_col": [128, 1], "qhalf": [128, 1],
    "ebig": [128, 8192], "cover": [128, 4, 128], "cmpmask": [128, 4, 128], "invfreq": [128, 8],
}


def prepare_inputs(inp, SEQ, B):
    NCH = SEQ // 256
    NOWN = NCH // 4
    NPOS = SEQ + 768
    NT = NPOS // 128
    consts = make_consts()
    x = np.asarray(inp["x"], np.float32)
    positions = np.asarray(inp["positions"], np.int32)
    g = lambda n: np.asarray(inp[n])[0]
    rep = lambda v: np.broadcast_to(np.asarray(v, np.float32).reshape(1, -1), (128, np.asarray(v).size)).copy()
    shared = {}
    shared["w_in"] = np.ascontiguousarray(g("w_in"), np.float32)
    shared["cmp_k_w1"] = np.ascontiguousarray(g("cmp_k_w1"))
    shared["cmp_k_w2"] = np.ascontiguousarray(g("cmp_k_w2"))
    shared["cmp_k_peT"] = np.ascontiguousarray(g("cmp_k_pe").T)
    shared["cmp_k_b1"] = np.ascontiguousarray(g("cmp_k_b1").reshape(2, 128).T)
    shared["cmp_v_w1"] = np.ascontiguousarray(g("cmp_v_w1"))
    shared["cmp_v_w2"] = np.ascontiguousarray(g("cmp_v_w2"))
    shared["cmp_v_peT"] = np.ascontiguousarray(g("cmp_v_pe").T)
    shared["cmp_v_b1"] = np.ascontiguousarray(g("cmp_v_b1").reshape(2, 128).T)
    cw = g("conv_w").reshape(4, 2048)
    shared["conv_w"] = np.ascontiguousarray(cw.T.reshape(16, 128, 4).transpose(1, 0, 2))
    shared["conv_b"] = np.ascontiguousarray(g("conv_b").reshape(16, 128).T)
    shared["dt_bias"] = rep(g("dt_bias"))
    shared["a_log"] = rep(g("a_log"))
    shared["d_skip"] = rep(g("d_skip"))
    shared["ssm_norm_w"] = rep(g("ssm_norm_w"))
    shared["w_out"] = np.ascontiguousarray(g("w_out"))
    shared["ln1_g"] = rep(g("ln1_g"))
    shared["ln1_b"] = rep(g("ln1_b"))
    shared["ln2_g"] = rep(g("ln2_g"))
    shared["ln2_b"] = rep(g("ln2_b"))
    shared["w_router"] = np.ascontiguousarray(np.concatenate([g("w_router_group"), g("w_router_expert")], axis=1))
    shared["b_router"] = rep(np.concatenate([g("b_router_group"), g("b_router_expert")]))
    shared["w_gate"] = np.ascontiguousarray(g("w_gate"))
    shared["w_up"] = np.ascontiguousarray(g("w_up"))
    shared["w_down"] = np.ascontiguousarray(g("w_down"))
    for k_, v_ in consts.items():
        shared["c_" + k_] = v_
    maps = []
    for b in range(B):
        xTb = np.ascontiguousarray(x[b].T)
        for j in range(4):
            m = dict(shared)
            off = 768 - 256 * j
            xT = np.zeros((D, NPOS), np.float32)
            xT[:, off:off + SEQ] = xTb
            m["xT"] = xT
            own = np.concatenate([x[b, 256 * (4 * i + j):256 * (4 * i + j + 1)] for i in range(NOWN)], axis=0)
            m["x_own"] = np.ascontiguousarray(own)
            posb = np.zeros((NPOS,), np.int32)
            posb[off:off + SEQ] = positions[b]
            m["pos_tm"] = np.ascontiguousarray(posb.reshape(NT, 128).T)
            NCT = NOWN // 2
            cidx = 16 * np.arange(128 * NCT) + 31
            m["cpos"] = np.ascontiguousarray(posb[cidx].reshape(NCT, 128).T)
            kval = np.zeros((NPOS,), np.float32)
            kval[off:off + SEQ] = 1.0
            m["kvalid"] = np.ascontiguousarray(kval.reshape(NT, 128).T)
            cst = 16 * np.arange(128 * NCT)
            cval = ((cst >= off) & (cst + 32 <= off + SEQ)).astype(np.float32)
            m["cvalid"] = np.ascontiguousarray(cval.reshape(NCT, 128).T)
            f0 = np.zeros((128, 128), np.float32)
            f0[:, 12 - 4 * j] = 1.0
            m["forced0"] = f0
            maps.append(m)
    return maps


def build(SEQ, stop="all", E_N=32, debug=False):
    NCH = SEQ // 256
    NOWN = NCH // 4
    NBC = NCH + 3
    NPOS = NBC * 256
    NT = NPOS // 128
    NOT = NOWN * 2
    NTOK = NOWN * 256
    nc = bass.Bass("TRN2", target_bir_lowering=False)

    def din(name, shape, dt=F32):
        return nc.dram_tensor(name, list(shape), dt, kind="ExternalInput").ap()

    def dscr(name, shape, dt):
        return nc.dram_tensor(name, list(shape), dt, kind="Internal").ap()

    xT = din("xT", [D, NPOS])
    x_own = din("x_own", [NTOK, D])
    pos_tm = din("pos_tm", [128, NT], I32)
    NCT = NOWN // 2
    cpos = din("cpos", [128, NCT], I32)
    kvalid_d = din("kvalid", [128, NT])
    cvalid_d = din("cvalid", [128, NCT])
    forced0_d = din("forced0", [128, 128])
    w_in = din("w_in", [D, 4928])
    cmpw = {}
    for kv in ("k", "v"):
        cmpw[kv] = dict(w1=din("cmp_%s_w1" % kv, [2048, 256]), w2=din("cmp_%s_w2" % kv, [256, 64]),
                        peT=din("cmp_%s_peT" % kv, [64, 32]), b1=din("cmp_%s_b1" % kv, [128, 2]))
    conv_w_d = din("conv_w", [128, 16, 4])
    conv_b_d = din("conv_b", [128, 16])
    dt_bias_d = din("dt_bias", [128, 16])
    a_log_d = din("a_log", [128, 16])
    d_skip_d = din("d_skip", [128, 16])
    normw_d = din("ssm_norm_w", [128, 1024])
    w_out_d = din("w_out", [2048, 2048])
    ln1g_d, ln1b_d = din("ln1_g", [128, 2048]), din("ln1_b", [128, 2048])
    ln2g_d, ln2b_d = din("ln2_g", [128, 2048]), din("ln2_b", [128, 2048])
    w_router_d = din("w_router", [2048, 36])
    b_router_d = din("b_router", [128, 36])
    w_gate_d = din("w_gate", [E_N, 2048, 512])
    w_up_d = din("w_up", [E_N, 2048, 512])
    w_down_d = din("w_down", [E_N, 512, 2048])
    cd = {k_: din("c_" + k_, shp) for k_, shp in CONST_SHAPES.items()}

    out_d = nc.dram_tensor("out", [NTOK, D], F32, kind="ExternalOutput").ap()
    mix_d = dscr("mix_scr", [NTOK, 2048], BF16)
    st_xs = dscr("st_xs", [NOT, 128, 1024], BF16)
    st_H = dscr("st_H", [NOWN, 128, 1024], BF16)
    st_BT = dscr("st_BT", [NOWN, 128, 4 * 256], BF16)
    st_sm = dscr("st_sm", [NOT, 128, 48], F32)
    h32_d = dscr("h32_scr", [NTOK, 2048], F32)
    hbf_d = dscr("hbf_scr", [NTOK, 2048], BF16)
    dbg = {}
    if debug:
        dbg["mix"] = nc.dram_tensor("dbg_mix", [NTOK, 2048], F32, kind="ExternalOutput").ap()
        dbg["h"] = nc.dram_tensor("dbg_h", [NTOK, 2048], F32, kind="ExternalOutput").ap()

    with ExitStack() as top:
        P = Prog(nc, top)
        kb = KB(nc, P)

        uid = [0]

        def sb(st, name, shape, dt=F32):
            uid[0] += 1
            return st.enter_context(nc.sbuf_tensor("%s_%d" % (name, uid[0]), list(shape), dt))

        def ps(st, name, shape, dt=F32):
            uid[0] += 1
            return st.enter_context(nc.psum_tensor("%s_%d" % (name, uid[0]), list(shape), dt))

        stage_ctr = [0]

        def load_cast(st_tiles, dst, src, nelem_shape, res_dst, eng_cast="auto", pr=(0, 128)):
            i = stage_ctr[0] % len(st_tiles)
            stage_ctr[0] += 1
            stg = st_tiles[i]
            n = 1
            for s_ in nelem_shape[1:]:
                n *= s_
            view = stg[pr[0]:pr[1], 0:n]
            if len(nelem_shape) == 3:
                view = view.rearrange("p (a b) -> p a b", a=nelem_shape[1])
            kb.dma(view, src, r=(), w=[("stage", i)])
            if eng_cast == "auto":
                eng_cast = ("scalar", "gpsimd")[stage_ctr[0] % 2]
            if eng_cast == "scalar":
                kb.act(dst, view, AF.Copy, r=[("stage", i)], w=res_dst)
            else:
                kb.e(eng_cast, "tensor_copy", out=dst, in_=view, r=[("stage", i)], w=res_dst)

        def load_consts(st, names, bf=()):
            t = {}
            for n_ in names:
                shp = CONST_SHAPES[n_]
                t[n_] = sb(st, "c_" + n_, shp)
                kb.dma(t[n_][:], cd[n_], w=["c_" + n_])
            for n_ in bf:
                shp = CONST_SHAPES[n_]
                t[n_ + "_bf"] = sb(st, "cb_" + n_, shp, BF16)
                kb.e("vector", "tensor_copy", out=t[n_ + "_bf"][:], in_=t[n_][:], r=["c_" + n_], w=["cb_" + n_])
            return t

        def phase_S1():
            with ExitStack() as st:
                C = load_consts(st, ["ident", "triL", "ones"], bf=["ident"])
                wS = sb(st, "wS", [128, 16, 1552], BF16)
                stg = [sb(st, "stgA", [128, 4096]), sb(st, "stgB", [128, 4096])]
                xTc = [sb(st, "xTc0", [128, 16, 256], BF16), sb(st, "xTc1", [128, 16, 256], BF16)]
                raw = [sb(st, "raw0", [128, 12, 259]), sb(st, "raw1", [128, 12, 259])]
                cacc_l = [sb(st, "cacc0", [128, 12, 256]), sb(st, "cacc1", [128, 12, 256])]
                xbT_l = [sb(st, "xbT0", [128, 12, 256], BF16), sb(st, "xbT1", [128, 12, 256], BF16)]
                cw = sb(st, "cw", [128, 16, 4])
                cb_ = sb(st, "cb", [128, 16])
                dtb = sb(st, "dtb", [128, 16])
                aneg = sb(st, "aneg", [128, 16])
                kval = sb(st, "kval", [128, NT])
                H = sb(st, "H", [128, 1024])
                Hbf = sb(st, "Hbf", [128, 1024], BF16)
                sm_l = [sb(st, "sm0", [128, 2, 48]), sb(st, "sm1", [128, 2, 48])]
                tmp16_l = [sb(st, "tmp160", [128, 2, 16]), sb(st, "tmp161", [128, 2, 16])]
                wst_l = [sb(st, "wst0", [128, 2, 16]), sb(st, "wst1", [128, 2, 16])]
                dec_l = [sb(st, "dec0", [128, 16]), sb(st, "dec1", [128, 16])]
                xs_tok_l = [sb(st, "xs_tok0", [128, 2, 1024], BF16), sb(st, "xs_tok1", [128, 2, 1024], BF16)]
                xw_l = [sb(st, "xw0", [128, 2, 1024], BF16), sb(st, "xw1", [128, 2, 1024], BF16)]
                B_tok_l = [sb(st, "B_tok0", [128, 2, 512], BF16), sb(st, "B_tok1", [128, 2, 512], BF16)]
                p_fm = [ps(st, "p_fm0", [128, 512]), ps(st, "p_fm1", [128, 512])]
                p_dt2 = [ps(st, "p_dt0", [128, 512]), ps(st, "p_dt1", [128, 512])]
                p_trx = ps(st, "p_trx", [128, 1024], BF16)
                p_trb = ps(st, "p_trb", [128, 1024], BF16)
                p_st = ps(st, "p_st", [128, 1024])

                kb.dma(cw[:], conv_w_d, w=["cw"])
                kb.dma(cb_[:], conv_b_d, w=["cb"])
                kb.dma(dtb[:], dt_bias_d, w=["dtb"])
                kb.dma(aneg[:], a_log_d, w=["aneg"])
                kb.dma(kval[:], kvalid_d, w=["kval"])
                kb.act(aneg[:], aneg[:], AF.Exp, r=["aneg"], w=["aneg"])
                kb.e("vector", "tensor_scalar", out=aneg[:], in0=aneg[:], scalar1=-1.0, scalar2=None, op0=ALU.mult,
                     r=["aneg"], w=["aneg"])
                kb.e("vector", "memset", H[:], 0.0, w=["H"])
                kb.e("vector", "memset", raw[0][:, :, 0:3], 0.0, w=[("raw", 0)])
                for k2 in range(0, 16, 2):
                    src = w_in[k2 * 128:(k2 + 2) * 128, C_XS:C_XS + 1536].rearrange("(k p) n -> p k n", p=128)
                    load_cast(stg, wS[:, k2:k2 + 2, 0:1536], src, [128, 2, 1536], ["wS"])
                srcd = w_in[:, C_DT:C_DT + 16].rearrange("(k p) n -> p k n", p=128)
                load_cast(stg, wS[:, :, 1536:1552], srcd, [128, 16, 16], ["wS"])

                def stage_A(c):
                    xb = xTc[c % 2]
                    rw = raw[c % 2]
                    rwn = raw[(c + 1) % 2]
                    rxb = ("xTc", c % 2)
                    cacc, tmp16, wst, dec, sm = cacc_l[c % 2], tmp16_l[c % 2], wst_l[c % 2], dec_l[c % 2], sm_l[c % 2]
                    xbT, xs_tok, xw, B_tok = xbT_l[c % 2], xs_tok_l[c % 2], xw_l[c % 2], B_tok_l[c % 2]
                    Rn = lambda n_: (n_, c % 2)
                    p_dt = p_dt2[c % 2]
                    for k4 in range(0, 16, 4):
                        src = xT[k4 * 128:(k4 + 4) * 128, c * 256:(c + 1) * 256].rearrange("(k p) n -> p k n", p=128)
                        load_cast(stg, xb[:, k4:k4 + 4, :], src, [128, 4, 256], [rxb], eng_cast="scalar")
                    for m in range(12):
                        pf = p_fm[m % 2]
                        for k in range(16):
                            kb.mm(pf[:, 0:256], wS[:, k, m * 128:(m + 1) * 128], xb[:, k, :], k == 0, k == 15,
                                  r=["wS", rxb], w=[("p_fm", m % 2)])
                        kb.act(rw[:, m, 3:259], pf[:, 0:256], AF.Copy, r=[("p_fm", m % 2)], w=[("raw", c % 2)])
                    for t in range(2):
                        for k in range(16):
                            kb.mm(p_dt[:, t * 16:(t + 1) * 16], xb[:, k, t * 128:(t + 1) * 128], wS[:, k, 1536:1552],
                                  k == 0, k == 15, r=["wS", rxb], w=[("p_dt", c % 2)])
                def stage_B1(c):
                    xb = xTc[c % 2]
                    rw = raw[c % 2]
                    rwn = raw[(c + 1) % 2]
                    rxb = ("xTc", c % 2)
                    cacc, tmp16, wst, dec, sm = cacc_l[c % 2], tmp16_l[c % 2], wst_l[c % 2], dec_l[c % 2], sm_l[c % 2]
                    xbT, xs_tok, xw, B_tok = xbT_l[c % 2], xs_tok_l[c % 2], xw_l[c % 2], B_tok_l[c % 2]
                    Rn = lambda n_: (n_, c % 2)
                    p_dt = p_dt2[c % 2]
                    kb.e("gpsimd", "tensor_copy", out=rwn[:, :, 0:3], in_=rw[:, :, 256:259],
                         r=[("raw", c % 2)], w=[("raw", (c + 1) % 2)])
                    for m in range(12):
                        kb.e("vector", "tensor_scalar", out=cacc[:, m, :], in0=rw[:, m, 0:256], scalar1=cw[:, m, 0:1],
                             scalar2=cb_[:, m:m + 1], op0=ALU.mult, op1=ALU.add, r=[("raw", c % 2), "cw", "cb"], w=[("cacc", c % 2, m)])
                    for tp in range(1, 4):
                        for m in range(12):
                            kb.e("vector", "scalar_tensor_tensor", out=cacc[:, m, :], in0=rw[:, m, tp:tp + 256],
                                 scalar=cw[:, m, tp:tp + 1], in1=cacc[:, m, :], op0=ALU.mult, op1=ALU.add,
                                 r=[("raw", c % 2), "cw", ("cacc", c % 2, m)], w=[("cacc", c % 2, m)])
                    kb.act(xbT[:], cacc[:], AF.Silu, r=[("cacc", c % 2, m) for m in range(12)], w=[Rn("xbT")])
                    pdt = p_dt[:, 0:32].rearrange("p (t h) -> p t h", t=2)
                    kb.e("vector", "tensor_tensor", out=tmp16[:], in0=pdt, in1=bc(dtb[:].unsqueeze(1), [128, 2, 16]),
                         op=ALU.add, r=[("p_dt", c % 2), "dtb"], w=[Rn("tmp16")])
                    kb.act(tmp16[:], tmp16[:], AF.Exp, r=[Rn("tmp16")], w=[Rn("tmp16")])
                    kb.act(tmp16[:], tmp16[:], AF.Ln, bias=1.0, r=[Rn("tmp16")], w=[Rn("tmp16")])
                    kb.e("vector", "tensor_tensor", out=sm[:, :, 0:16], in0=tmp16[:],
                         in1=bc(kval[:, 2 * c:2 * c + 2].unsqueeze(2), [128, 2, 16]), op=ALU.mult,
                         r=[Rn("tmp16"), "kval"], w=[Rn("sm")])
                    kb.e("vector", "tensor_tensor", out=sm[:, :, 32:48], in0=sm[:, :, 0:16],
                         in1=bc(aneg[:].unsqueeze(1), [128, 2, 16]), op=ALU.mult, r=[Rn("sm"), "aneg"], w=[Rn("sm")])
                    kb.mm(p_dt[:, 64:80], C["triL"][:], sm[:, 0, 32:48], True, True, r=[Rn("sm"), "c_triL"], w=[("p_dt", c % 2)])
                    kb.mm(p_dt[:, 80:96], C["ones"][:], sm[:, 0, 32:48], True, False, r=[Rn("sm"), "c_ones"], w=[("p_dt", c % 2)])
                    kb.mm(p_dt[:, 80:96], C["triL"][:], sm[:, 1, 32:48], False, True, r=[Rn("sm"), "c_triL"], w=[("p_dt", c % 2)])
                    kb.mm(p_dt[:, 128:144], C["ones"][:], sm[:, 0, 32:48], True, False, r=[Rn("sm"), "c_ones"], w=[("p_dt", c % 2)])
                    kb.mm(p_dt[:, 128:144], C["ones"][:], sm[:, 1, 32:48], False, True, r=[Rn("sm"), "c_ones"], w=[("p_dt", c % 2)])
                    pacs = p_dt[:, 64:96].rearrange("p (t h) -> p t h", t=2)
                    kb.act(sm[:, :, 16:32], pacs, AF.Copy, r=[("p_dt", c % 2)], w=[Rn("sm")])
                    kb.e("vector", "tensor_tensor", out=wst[:], in0=bc(p_dt[:, 128:144].unsqueeze(1), [128, 2, 16]),
                         in1=sm[:, :, 16:32], op=ALU.subtract, r=[("p_dt", c % 2), Rn("sm")], w=[Rn("wst")])
                    kb.act(wst[:], wst[:], AF.Exp, r=[Rn("wst")], w=[Rn("wst")])
                    kb.e("vector", "tensor_tensor", out=wst[:], in0=wst[:], in1=sm[:, :, 0:16], op=ALU.mult,
                         r=[Rn("wst"), Rn("sm")], w=[Rn("wst")])
                    kb.act(dec[:], p_dt[:, 128:144], AF.Exp, r=[("p_dt", c % 2)], w=[Rn("dec")])
                def stage_B2(c):
                    xb = xTc[c % 2]
                    rw = raw[c % 2]
                    rwn = raw[(c + 1) % 2]
                    rxb = ("xTc", c % 2)
                    cacc, tmp16, wst, dec, sm = cacc_l[c % 2], tmp16_l[c % 2], wst_l[c % 2], dec_l[c % 2], sm_l[c % 2]
                    xbT, xs_tok, xw, B_tok = xbT_l[c % 2], xs_tok_l[c % 2], xw_l[c % 2], B_tok_l[c % 2]
                    Rn = lambda n_: (n_, c % 2)
                    p_dt = p_dt2[c % 2]
                    for t in range(2):
                        for m in range(8):
                            kb.tr(p_trx[:, m * 128:(m + 1) * 128], xbT[:, m, t * 128:(t + 1) * 128], C["ident_bf"][:],
                                  r=[Rn("xbT"), "cb_ident"], w=["p_trx"])
                        kb.act(xs_tok[:, t, :], p_trx[:], AF.Copy, r=["p_trx"], w=[Rn("xs_tok")])
                        kb.e("vector", "tensor_tensor", out=xw[:, t, :].rearrange("p (h d) -> p h d", h=16),
                             in0=p_trx[:].rearrange("p (h d) -> p h d", h=16),
                             in1=bc(wst[:, t, :].unsqueeze(2), [128, 16, 64]), op=ALU.mult,
                             r=["p_trx", Rn("wst")], w=[Rn("xw")])
                        for g_ in range(4):
                            kb.tr(p_trb[:, g_ * 128:(g_ + 1) * 128], xbT[:, 8 + g_, t * 128:(t + 1) * 128], C["ident_bf"][:],
                                  r=[Rn("xbT"), "cb_ident"], w=["p_trb"])
                        kb.act(B_tok[:, t, :], p_trb[:, 0:512], AF.Copy, r=["p_trb"], w=[Rn("B_tok")])
                    for g_ in range(4):
                        for t in range(2):
                            kb.mm(p_st[:, g_ * 256:(g_ + 1) * 256], B_tok[:, t, g_ * 128:(g_ + 1) * 128],
                                  xw[:, t, g_ * 256:(g_ + 1) * 256], (t == 0) and (g_ % 2 == 0), t == 1,
                                  r=[Rn("B_tok"), Rn("xw")], w=["p_st"])
                    if c >= 3 and (c - 3) % 4 == 0:
                        i = (c - 3) // 4
                        kb.e("gpsimd", "tensor_copy", out=Hbf[:], in_=H[:], r=["H"], w=["Hbf"])
                        kb.dma(st_H[i], Hbf[:], r=["Hbf"], w=["st_H"])
                        kb.dma(st_BT[i], xbT[:, 8:12, :].rearrange("p a b -> p (a b)"), r=[Rn("xbT")], w=["st_BT"])
                        for t in range(2):
                            kb.dma(st_xs[2 * i + t], xs_tok[:, t, :], r=[Rn("xs_tok")], w=["st_xs"])
                            kb.dma(st_sm[2 * i + t], sm[:, t, :], r=[Rn("sm")], w=["st_sm"])
                    kb.e("vector", "tensor_tensor", out=H[:].rearrange("p (h d) -> p h d", h=16),
                         in0=H[:].rearrange("p (h d) -> p h d", h=16), in1=bc(dec[:].unsqueeze(2), [128, 16, 64]),
                         op=ALU.mult, r=["H", Rn("dec")], w=["H"])
                    kb.e("vector", "tensor_tensor", out=H[:], in0=H[:], in1=p_st[:], op=ALU.add, r=["H", "p_st"], w=["H"])
                stage_A(0)
                if NBC > 1:
                    stage_A(1)
                stage_B1(0)
                for c in range(NBC):
                    if c + 2 < NBC:
                        stage_A(c + 2)
                    if c + 1 < NBC:
                        stage_B1(c + 1)
                    stage_B2(c)
                P.end_phase()

        def phase_S2():
            with ExitStack() as st:
                C = load_consts(st, ["ident", "triA", "triB", "negA", "negB", "ones"], bf=["ident"])
                wZC = sb(st, "wZC", [128, 16, 1536], BF16)
                stg = [sb(st, "stgA", [128, 4096]), sb(st, "stgB", [128, 4096])]
                xTo = sb(st, "xTo", [128, 16, 260], BF16)
                cw = sb(st, "cw", [128, 4, 4])
                cb_ = sb(st, "cb", [128, 4])
                dsk = sb(st, "dsk", [128, 16])
                normw = sb(st, "normw", [128, 1024])
                rawC = sb(st, "rawC", [128, 4, 259])
                caccC = sb(st, "caccC", [128, 4, 256])
                CT = sb(st, "CT", [128, 4, 256], BF16)
                BT = sb(st, "BT", [128, 4, 256], BF16)
                Hbf = sb(st, "Hbf", [128, 1024], BF16)
                xs_tok = sb(st, "xs_tok", [128, 2, 1024], BF16)
                sm = sb(st, "sm", [128, 2, 48])
                nacs = sb(st, "nacs", [128, 2, 16])
                ea = sb(st, "ea", [128, 2, 16])
                cbT = sb(st, "cbT", [128, 4, 2, 256])
                drep = [sb(st, "drep0", [128, 2, 128]), sb(st, "drep1", [128, 2, 128])]
                dtmp = [sb(st, "dtmp%d" % q_, [128, 256]) for q_ in range(4)]
                WT = [sb(st, "WT%d" % q_, [128, 256], BF16) for q_ in range(4)]
                sz = sb(st, "sz", [128, 1024])
                yv = sb(st, "yv", [128, 1024])
                y2 = sb(st, "y2", [128, 1024])
                ss = sb(st, "ss", [128, 4])
                ybf = sb(st, "ybf", [128, 1024], BF16)
                p_y = ps(st, "p_y", [128, 2048])
                p_w = ps(st, "p_w", [128, 1024])
                p_a = [ps(st, "p_a0", [128, 512]), ps(st, "p_a1", [128, 512])]

                kb.dma(cw[:], conv_w_d[:, 12:16, :], w=["cw"])
                kb.dma(cb_[:], conv_b_d[:, 12:16], w=["cb"])
                kb.dma(dsk[:], d_skip_d, w=["dsk"])
                kb.dma(normw[:], normw_d, w=["normw"])
                for k2 in range(0, 16, 2):
                    src = w_in[k2 * 128:(k2 + 2) * 128, C_Z:C_Z + 1024].rearrange("(k p) n -> p k n", p=128)
                    load_cast(stg, wZC[:, k2:k2 + 2, 0:1024], src, [128, 2, 1024], ["wZC"])
                for k4 in range(0, 16, 4):
                    src = w_in[k4 * 128:(k4 + 4) * 128, C_C:C_C + 512].rearrange("(k p) n -> p k n", p=128)
                    load_cast(stg, wZC[:, k4:k4 + 4, 1024:1536], src, [128, 4, 512], ["wZC"])

                for i in range(NOWN):
                    p0 = (3 + 4 * i) * 256
                    for k4 in range(0, 16, 4):
                        src = xT[k4 * 128:(k4 + 4) * 128, p0 - 4:p0 + 256].rearrange("(k p) n -> p k n", p=128)
                        load_cast(stg, xTo[:, k4:k4 + 4, :], src, [128, 4, 260], ["xTo"], eng_cast="scalar")
                    kb.dma(BT[:].rearrange("p a b -> p (a b)"), st_BT[i], w=["BT"])
                    kb.dma(Hbf[:], st_H[i], w=["Hbf"])
                    for t in range(2):
                        kb.dma(xs_tok[:, t, :], st_xs[2 * i + t], w=["xs_tok"])
                        kb.dma(sm[:, t, :], st_sm[2 * i + t], w=["sm"])
                    kb.e("vector", "tensor_scalar", out=nacs[:], in0=sm[:, :, 16:32], scalar1=-1.0, scalar2=None,
                         op0=ALU.mult, r=["sm"], w=["nacs"])
                    kb.act(ea[:], sm[:, :, 16:32], AF.Exp, r=["sm"], w=["ea"])
                    for g_ in range(4):
                        for k in range(16):
                            kb.mm(p_w[:, 0:259], wZC[:, k, 1024 + g_ * 128:1024 + (g_ + 1) * 128], xTo[:, k, 1:260],
                                  k == 0, k == 15, r=["wZC", "xTo"], w=["p_w"])
                        kb.act(rawC[:, g_, :], p_w[:, 0:259], AF.Copy, r=["p_w"], w=["rawC"])
                        kb.e("vector", "tensor_scalar", out=caccC[:, g_, :], in0=rawC[:, g_, 0:256], scalar1=cw[:, g_, 0:1],
                             scalar2=cb_[:, g_:g_ + 1], op0=ALU.mult, op1=ALU.add, r=["rawC", "cw", "cb"], w=["caccC"])
                        for tp in range(1, 4):
                            kb.e("vector", "scalar_tensor_tensor", out=caccC[:, g_, :], in0=rawC[:, g_, tp:tp + 256],
                                 scalar=cw[:, g_, tp:tp + 1], in1=caccC[:, g_, :], op0=ALU.mult, op1=ALU.add,
                                 r=["rawC", "cw", "caccC"], w=["caccC"])
                    kb.act(CT[:], caccC[:], AF.Silu, r=["caccC"], w=["CT"])
                    for g_ in range(4):
                        for s_ in range(2):
                            kb.mm(p_w[:, 512:768], BT[:, g_, s_ * 128:(s_ + 1) * 128], CT[:, g_, :], True, True,
                                  r=["BT", "CT"], w=["p_w"])
                            kb.act(cbT[:, g_, s_, :], p_w[:, 512:768], AF.Copy, r=["p_w"], w=["cbT"])
                    for h in range(16):
                        g_ = h // 4
                        dr = drep[h % 2]
                        pa = p_a[h % 2]
                        for s_ in range(2):
                            kb.act(dr[:, s_, :], C["ones"][:], AF.Identity, scale=sm[:, s_, 32 + h:33 + h],
                                   r=["sm", "c_ones"], w=[("drep", h % 2)])
                        kb.mm(pa[:, 0:256], dr[:, 0, :], C["triA"][:], True, False, r=[("drep", h % 2), "c_triA"], w=[("p_a", h % 2)])
                        kb.mm(pa[:, 0:256], dr[:, 1, :], C["triB"][:], False, True, r=[("drep", h % 2), "c_triB"], w=[("p_a", h % 2)])
                        for s_ in range(2):
                            dtm = dtmp[2 * (h % 2) + s_]
                            wt = WT[2 * (h % 2) + s_]
                            kb.e("vector", "tensor_tensor", out=dtm[:], in0=pa[:, 0:256], in1=C["negA" if s_ == 0 else "negB"][:],
                                 op=ALU.add, r=[("p_a", h % 2), "c_negA", "c_negB"], w=[("dtmp", 2 * (h % 2) + s_)])
                            kb.act(dtm[:], dtm[:], AF.Exp, bias=nacs[:, s_, h:h + 1], r=[("dtmp", 2 * (h % 2) + s_), "nacs"], w=[("dtmp", 2 * (h % 2) + s_)])
                            kb.e("vector", "scalar_tensor_tensor", out=wt[:], in0=dtm[:], scalar=sm[:, s_, h:h + 1],
                                 in1=cbT[:, g_, s_, :], op0=ALU.mult, op1=ALU.mult, r=[("dtmp", 2 * (h % 2) + s_), "sm", "cbT"], w=[("WT", 2 * (h % 2) + s_)])
                        first0 = (h % 8 == 0)
                        kb.mm(p_y[:, h * 64:(h + 1) * 64], WT[2 * (h % 2)][:, 0:128], xs_tok[:, 0, h * 64:(h + 1) * 64], first0, True,
                              r=[("WT", 2 * (h % 2)), "xs_tok"], w=["p_y"])
                        kb.mm(p_y[:, 1024 + h * 64:1024 + (h + 1) * 64], WT[2 * (h % 2)][:, 128:256], xs_tok[:, 0, h * 64:(h + 1) * 64],
                              first0, False, r=[("WT", 2 * (h % 2)), "xs_tok"], w=["p_y"])
                        kb.mm(p_y[:, 1024 + h * 64:1024 + (h + 1) * 64], WT[2 * (h % 2) + 1][:, 128:256], xs_tok[:, 1, h * 64:(h + 1) * 64],
                              False, True, r=[("WT", 2 * (h % 2) + 1), "xs_tok"], w=["p_y"])
                    for lt in range(2):
                        for cc in range(2):
                            for k in range(16):
                                kb.mm(p_w[:, cc * 512:(cc + 1) * 512], xTo[:, k, 4 + lt * 128:4 + (lt + 1) * 128],
                                      wZC[:, k, cc * 512:(cc + 1) * 512], k == 0, k == 15, r=["wZC", "xTo"], w=["p_w"])
                        kb.act(sz[:], p_w[:], AF.Silu, r=["p_w"], w=["sz"])
                        for g_ in range(4):
                            kb.mm(p_w[:, g_ * 256:(g_ + 1) * 256], CT[:, g_, lt * 128:(lt + 1) * 128], Hbf[:, g_ * 256:(g_ + 1) * 256],
                                  g_ % 2 == 0, True, r=["CT", "Hbf", "sz"], w=["p_w"])
                        v3 = lambda ap: ap.rearrange("p (h d) -> p h d", h=16)
                        kb.e("vector", "tensor_tensor", out=v3(yv[:]), in0=v3(p_w[:]), in1=bc(ea[:, lt, :].unsqueeze(2), [128, 16, 64]),
                             op=ALU.mult, r=["p_w", "ea"], w=["yv"])
                        kb.e("vector", "tensor_tensor", out=yv[:], in0=yv[:], in1=p_y[:, lt * 1024:(lt + 1) * 1024], op=ALU.add,
                             r=["yv", "p_y"], w=["yv"])
                        kb.e("vector", "tensor_tensor", out=v3(y2[:]), in0=v3(xs_tok[:, lt, :]), in1=bc(dsk[:].unsqueeze(2), [128, 16, 64]),
                             op=ALU.mult, r=["xs_tok", "dsk"], w=["y2"])
                        kb.e("vector", "tensor_tensor", out=yv[:], in0=yv[:], in1=y2[:], op=ALU.add, r=["yv", "y2"], w=["yv"])
                        kb.e("vector", "tensor_tensor", out=yv[:], in0=yv[:], in1=sz[:], op=ALU.mult, r=["yv", "sz"], w=["yv"])
                        kb.e("vector", "tensor_tensor", out=y2[:], in0=yv[:], in1=yv[:], op=ALU.mult, r=["yv"], w=["y2"])
                        kb.e("vector", "tensor_reduce", out=ss[:], in_=y2[:].rearrange("p (g d) -> p g d", g=4), axis=AX.X,
                             op=ALU.add, r=["y2"], w=["ss"])
                        kb.e("vector", "tensor_scalar", out=ss[:], in0=ss[:], scalar1=1.0 / 256.0, scalar2=EPS, op0=ALU.mult,
                             op1=ALU.add, r=["ss"], w=["ss"])
                        kb.act(ss[:], ss[:], AF.Sqrt, r=["ss"], w=["ss"])
                        kb.e("vector", "reciprocal", out=ss[:], in_=ss[:], r=["ss"], w=["ss"])
                        kb.e("vector", "tensor_tensor", out=yv[:].rearrange("p (g d) -> p g d", g=4),
                             in0=yv[:].rearrange("p (g d) -> p g d", g=4), in1=bc(ss[:].unsqueeze(2), [128, 4, 256]),
                             op=ALU.mult, r=["yv", "ss"], w=["yv"])
                        kb.e("vector", "tensor_tensor", out=ybf[:], in0=yv[:], in1=normw[:], op=ALU.mult, r=["yv", "normw"], w=["ybf"])
                        row0 = (2 * i + lt) * 128
                        kb.dma(mix_d[row0:row0 + 128, 1024:2048], ybf[:], r=["ybf"], w=["mix_d"])
                P.end_phase()


        TWO_PI = 6.283185307179586
        C1 = 6.28125
        C2 = TWO_PI - C1

        def rope_tables(st, pos_i, n, sin_t, cos_t, invf, tag):
            posf = sb(st, "posf" + tag, [128, n])
            ang = sb(st, "ang" + tag, [128, n, 8])
            kf = sb(st, "kf" + tag, [128, n, 8])
            ki = sb(st, "ki" + tag, [128, n, 8], I32)
            rr = sb(st, "rr" + tag, [128, n, 8])
            m1 = sb(st, "m1" + tag, [128, n, 8])
            R = ["rope" + tag]
            kb.e("vector", "tensor_copy", out=posf[:], in_=pos_i, r=R, w=R)
            kb.e("vector", "tensor_tensor", out=ang[:], in0=bc(posf[:].unsqueeze(2), [128, n, 8]),
                 in1=bc(invf.unsqueeze(1), [128, n, 8]), op=ALU.mult, r=R + ["c_invfreq"], w=R)
            kb.e("vector", "tensor_scalar", out=kf[:], in0=ang[:], scalar1=1.0 / TWO_PI, scalar2=None, op0=ALU.mult, r=R, w=R)
            kb.e("vector", "tensor_copy", out=ki[:], in_=kf[:], r=R, w=R)
            kb.e("vector", "tensor_copy", out=kf[:], in_=ki[:], r=R, w=R)
            kb.e("vector", "scalar_tensor_tensor", out=rr[:], in0=kf[:], scalar=-C1, in1=ang[:], op0=ALU.mult, op1=ALU.add, r=R, w=R)
            kb.e("vector", "scalar_tensor_tensor", out=rr[:], in0=kf[:], scalar=-C2, in1=rr[:], op0=ALU.mult, op1=ALU.add, r=R, w=R)
            kb.e("vector", "tensor_scalar", out=m1[:], in0=rr[:], scalar1=float(np.pi), scalar2=None, op0=ALU.is_gt, r=R, w=R)
            kb.e("vector", "scalar_tensor_tensor", out=rr[:], in0=m1[:], scalar=-TWO_PI, in1=rr[:], op0=ALU.mult, op1=ALU.add, r=R, w=R)
            kb.e("vector", "tensor_scalar", out=m1[:], in0=rr[:], scalar1=-float(np.pi), scalar2=None, op0=ALU.is_lt, r=R, w=R)
            kb.e("vector", "scalar_tensor_tensor", out=rr[:], in0=m1[:], scalar=TWO_PI, in1=rr[:], op0=ALU.mult, op1=ALU.add, r=R, w=R)
            kb.e("vector", "tensor_scalar", out=rr[:], in0=rr[:], scalar1=float(np.pi), scalar2=-float(np.pi), op0=ALU.min, op1=ALU.max, r=R, w=R)
            kb.act(sin_t, rr[:], AF.Sin, r=R, w=R + ["ropetab" + tag])
            kb.e("vector", "tensor_scalar", out=m1[:], in0=rr[:], scalar1=-1.0, scalar2=None, op0=ALU.mult, r=R, w=R)
            kb.e("vector", "tensor_tensor", out=m1[:], in0=m1[:], in1=rr[:], op=ALU.max, r=R, w=R)
            kb.e("vector", "tensor_scalar", out=m1[:], in0=m1[:], scalar1=-1.0, scalar2=float(np.pi / 2), op0=ALU.mult, op1=ALU.add, r=R, w=R)
            kb.act(cos_t, m1[:], AF.Sin, r=R, w=R + ["ropetab" + tag])

        def rope_apply(xin, out_view, cos_b, sin_b, tmps, shape, rin, rout, rtmp):
            x1 = xin[..., 0:8] if False else None

        def nsa_all():
            with ExitStack() as nsa:
                KselT = sb(nsa, "KselT", [128, NPOS], BF16)
                KwinT = sb(nsa, "KwinT", [128, NPOS], BF16)
                Vsel = sb(nsa, "Vsel", [128, NT, 2, 65], BF16)
                Vwin = sb(nsa, "Vwin", [128, NT, 2, 65], BF16)
                cosT = sb(nsa, "cosT", [128, NT, 8])
                sinT = sb(nsa, "sinT", [128, NT, 8])
                NCB = 128 * NCT
                kcT = sb(nsa, "kcT", [128, NCB], BF16)
                vc_ext = sb(nsa, "vc_ext", [128, NCT, 2, 193], BF16)
                with ExitStack() as cmpst:
                    KcT = sb(cmpst, "KcT", [128, NPOS], BF16)
                    VcT = sb(cmpst, "VcT", [128, NPOS], BF16)
                    with ExitStack() as st:
                        C = load_consts(st, ["ident", "invfreq"], bf=["ident"])
                        wK = sb(st, "wK", [128, 16, 768], BF16)
                        stg = [sb(st, "stgA", [128, 2048]), sb(st, "stgB", [128, 2048])]
                        xTc = [sb(st, "xTc0", [128, 16, 256], BF16), sb(st, "xTc1", [128, 16, 256], BF16)]
                        kval = sb(st, "kval", [128, NT])
                        posi = sb(st, "posi", [128, NT], I32)
                        ktok = sb(st, "ktok", [128, 2, 128], BF16)
                        tt = [sb(st, "ropet%d" % q_, [128, 2, 2, 8]) for q_ in range(4)]
                        p_fm = [ps(st, "p_fm0", [128, 512]), ps(st, "p_fm1", [128, 512])]
                        p_kv = [ps(st, "p_kv%d" % q_, [128, 512]) for q_ in range(4)]
                        p_tk = ps(st, "p_tk", [128, 1024], BF16)
                        kb.dma(kval[:], kvalid_d, w=["kval"])
                        kb.dma(posi[:], pos_tm, w=["ropeK"])
                        rope_tables(st, posi[:], NT, sinT[:], cosT[:], C["invfreq"][:], "K")
                        for k4 in range(0, 16, 2):
                            src = w_in[k4 * 128:(k4 + 2) * 128, C_KV:C_KV + 768].rearrange("(k p) n -> p k n", p=128)
                            load_cast(stg, wK[:, k4:k4 + 2, :], src, [128, 2, 768], ["wK"])
                        def stage_KA(c):
                            xb = xTc[c % 2]
                            rxb = ("xTc", c % 2)
                            for k4 in range(0, 16, 4):
                                src = xT[k4 * 128:(k4 + 4) * 128, c * 256:(c + 1) * 256].rearrange("(k p) n -> p k n", p=128)
                                load_cast(stg, xb[:, k4:k4 + 4, :], src, [128, 4, 256], [rxb], eng_cast="scalar")
                            for m in range(2):
                                pf = p_fm[m]
                                for k in range(16):
                                    kb.mm(pf[:, 0:256], wK[:, k, m * 128:(m + 1) * 128], xb[:, k, :], k == 0, k == 15,
                                          r=["wK", rxb], w=[("p_fm", m)])
                                dst = (KcT if m == 0 else VcT)
                                kb.act(dst[:, c * 256:(c + 1) * 256], pf[:, 0:256], AF.Copy, r=[("p_fm", m)], w=["KcT" if m == 0 else "VcT"])
                            for t in range(2):
                                T = 2 * c + t
                                pk = p_kv[2 * (c % 2) + t]
                                rpk = ("p_kv", 2 * (c % 2) + t)
                                for k in range(16):
                                    kb.mm(pk[:], xb[:, k, t * 128:(t + 1) * 128], wK[:, k, 256:768], k == 0, k == 15,
                                          r=["wK", rxb], w=[rpk])
                        def stage_KB(c):
                            for t in range(2):
                                T = 2 * c + t
                                pk = p_kv[2 * (c % 2) + t]
                                rpk = ("p_kv", 2 * (c % 2) + t)
                                pk4 = pk[:].rearrange("p (a g d) -> p a g d", a=4, g=2)
                                kt4 = ktok[:].rearrange("p a (g d) -> p a g d", g=2)
                                kb.act(kt4, pk4[:, 0:4:2, :, :], AF.Copy, r=[rpk], w=["ktok"])
                                x1 = pk4[:, 0:4:2, :, 0:8]
                                x2 = pk4[:, 0:4:2, :, 8:16]
                                cb4 = bc(cosT[:, T, :].unsqueeze(1).unsqueeze(1), [128, 2, 2, 8])
                                sb4 = bc(sinT[:, T, :].unsqueeze(1).unsqueeze(1), [128, 2, 2, 8])
                                RT = ["ropetabK"]
                                kb.e("vector", "tensor_tensor", out=tt[0][:], in0=x1, in1=cb4, op=ALU.mult, r=[rpk] + RT, w=["tt0"])
                                kb.e("vector", "tensor_tensor", out=tt[1][:], in0=x2, in1=sb4, op=ALU.mult, r=[rpk] + RT, w=["tt1"])
                                kb.e("vector", "tensor_tensor", out=tt[2][:], in0=x2, in1=cb4, op=ALU.mult, r=[rpk] + RT, w=["tt2"])
                                kb.e("vector", "tensor_tensor", out=tt[3][:], in0=x1, in1=sb4, op=ALU.mult, r=[rpk] + RT, w=["tt3"])
                                kb.e("vector", "tensor_tensor", out=kt4[:, :, :, 0:8], in0=tt[0][:], in1=tt[1][:], op=ALU.subtract,
                                     r=["tt0", "tt1"], w=["ktok"])
                                kb.e("vector", "tensor_tensor", out=kt4[:, :, :, 8:16], in0=tt[2][:], in1=tt[3][:], op=ALU.add,
                                     r=["tt2", "tt3"], w=["ktok"])
                                for s_ in range(2):
                                    kb.tr(p_tk[:, s_ * 128:(s_ + 1) * 128], ktok[:, s_, :], C["ident_bf"][:], r=["ktok", "cb_ident"], w=["p_tk"])
                                kb.act(KselT[:, T * 128:(T + 1) * 128], p_tk[:, 0:128], AF.Copy, r=["p_tk"], w=["KselT"])
                                kb.act(KwinT[:, T * 128:(T + 1) * 128], p_tk[:, 128:256], AF.Copy, r=["p_tk"], w=["KwinT"])
                                kb.e("vector", "tensor_copy", out=Vsel[:, T, :, 0:64], in_=pk4[:, 1, :, :], r=[rpk], w=["Vsel"])
                                kb.e("vector", "tensor_copy", out=Vwin[:, T, :, 0:64], in_=pk4[:, 3, :, :], r=[rpk], w=["Vwin"])
                                kvb = bc(kval[:, T:T + 1].unsqueeze(2), [128, 2, 1])
                                kb.e("vector", "tensor_copy", out=Vsel[:, T, :, 64:65], in_=kvb, r=["kval"], w=["Vsel"])
                                kb.e("vector", "tensor_copy", out=Vwin[:, T, :, 64:65], in_=kvb, r=["kval"], w=["Vwin"])
                        stage_KA(0)
                        for c in range(NBC):
                            if c + 1 < NBC:
                                stage_KA(c + 1)
                            stage_KB(c)
                        P.end_phase()
                    with ExitStack() as st:
                        C = load_consts(st, ["ident", "invfreq", "cover"], bf=["ident"])
                        stg = [sb(st, "stgA", [128, 4096]), sb(st, "stgB", [128, 4096])]
                        W1 = sb(st, "W1", [128, 32, 256], BF16)
                        peT = sb(st, "peT", [128, 32], BF16)
                        b1 = sb(st, "b1", [128, 2])
                        bias = sb(st, "bias", [128, 2])
                        w2 = sb(st, "w2", [128, 2, 64], BF16)
                        hidT = sb(st, "hidT", [128, 2, NCB], BF16)
                        gx = sb(st, "gx", [128, NCB])
                        gu = sb(st, "gu", [128, NCB])
                        cval = sb(st, "cval", [128, NCT])
                        cposi = sb(st, "cposi", [128, NCT], I32)
                        ccos = sb(st, "ccos", [128, NCT, 8])
                        csin = sb(st, "csin", [128, NCT, 8])
                        kctok = sb(st, "kctok", [128, NCT, 128], BF16)
                        tt = [sb(st, "ropec%d" % q_, [128, 8]) for q_ in range(4)]
                        p_h = [ps(st, "p_h0", [128, 512]), ps(st, "p_h1", [128, 512])]
                        p_b = ps(st, "p_b", [128, 512])
                        p_o = ps(st, "p_o", [128, 512])
                        p_t = ps(st, "p_t", [128, 1024], BF16)
                        kb.dma(cval[:], cvalid_d, w=["cval"])
                        kb.dma(cposi[:], cpos, w=["ropeC"])
                        rope_tables(st, cposi[:], NCT, csin[:], ccos[:], C["invfreq"][:], "C")
                        for kvn, Xc in (("k", KcT), ("v", VcT)):
                            cw_ = cmpw[kvn]
                            for half in range(2):
                                for jh in range(2):
                                    src = cw_["w1"].rearrange("(j d) h -> d j h", d=64)[:, jh * 16:(jh + 1) * 16, :]
                                    load_cast(stg, W1[half * 64:(half + 1) * 64, jh * 16:(jh + 1) * 16, :], src, [64, 16, 256], ["W1"],
                                              pr=(half * 64, (half + 1) * 64))
                                load_cast(stg, peT[half * 64:(half + 1) * 64, :], cw_["peT"], [64, 32], ["peT"], pr=(half * 64, (half + 1) * 64))
                            load_cast(stg, w2[:], cw_["w2"].rearrange("(c p) n -> p c n", p=128), [128, 2, 64], ["w2"])
                            kb.dma(b1[:], cw_["b1"], w=["b1"])
                            for hc in range(2):
                                for j in range(32):
                                    kb.mm(p_b[:, hc:hc + 1], W1[0:64, j, hc * 128:(hc + 1) * 128], peT[0:64, j:j + 1], j == 0, j == 31,
                                          r=["W1", "peT"], w=["p_b"])
                            kb.e("vector", "tensor_tensor", out=bias[:], in0=p_b[:, 0:2], in1=b1[:], op=ALU.add, r=["p_b", "b1"], w=["bias"])
                            for g_ in range(2):
                                for hc in range(2):
                                    for j in range(32):
                                        kb.mm(p_h[hc][:, 0:NCB], W1[g_ * 64:(g_ + 1) * 64, j, hc * 128:(hc + 1) * 128],
                                              Xc[g_ * 64:(g_ + 1) * 64, j:j + 16 * NCB:16], j == 0, j == 31,
                                              r=["W1", "KcT", "VcT"], w=[("p_h", hc)])
                                    kb.act(gx[:], p_h[hc][:, 0:NCB], AF.Identity, bias=bias[:, hc:hc + 1], r=[("p_h", hc), "bias"], w=["gx"])
                                    kb.e("vector", "tensor_tensor", out=gu[:], in0=gx[:], in1=gx[:], op=ALU.mult, r=["gx"], w=["gu"])
                                    kb.e("vector", "tensor_scalar", out=gu[:], in0=gu[:], scalar1=0.044715, scalar2=1.0, op0=ALU.mult, op1=ALU.add, r=["gu"], w=["gu"])
                                    kb.e("vector", "tensor_tensor", out=gu[:], in0=gu[:], in1=gx[:], op=ALU.mult, r=["gu", "gx"], w=["gu"])
                                    kb.act(gu[:], gu[:], AF.Tanh, scale=0.7978845608028654, r=["gu"], w=["gu"])
                                    kb.e("vector", "tensor_scalar", out=gu[:], in0=gu[:], scalar1=1.0, scalar2=0.5, op0=ALU.add, op1=ALU.mult, r=["gu"], w=["gu"])
                                    kb.e("vector", "tensor_tensor", out=hidT[:, hc, :], in0=gu[:], in1=gx[:], op=ALU.mult, r=["gu", "gx"], w=["hidT"])
                                for nt in range(NCT):
                                    for hc in range(2):
                                        kb.mm(p_o[:, 0:64], hidT[:, hc, nt * 128:(nt + 1) * 128], w2[:, hc, :], hc == 0, hc == 1,
                                              r=["hidT", "w2"], w=["p_o"])
                                    if kvn == "k":
                                        ko = kctok[:, nt, g_ * 64:(g_ + 1) * 64]
                                        kb.act(ko, p_o[:, 0:64], AF.Copy, r=["p_o"], w=["kctok"])
                                        x1 = p_o[:, 0:8]
                                        x2 = p_o[:, 8:16]
                                        cb_ = ccos[:, nt, :]
                                        sb_ = csin[:, nt, :]
                                        RT = ["ropetabC"]
                                        kb.e("vector", "tensor_tensor", out=tt[0][:], in0=x1, in1=cb_, op=ALU.mult, r=["p_o"] + RT, w=["tt0"])
                                        kb.e("vector", "tensor_tensor", out=tt[1][:], in0=x2, in1=sb_, op=ALU.mult, r=["p_o"] + RT, w=["tt1"])
                                        kb.e("vector", "tensor_tensor", out=tt[2][:], in0=x2, in1=cb_, op=ALU.mult, r=["p_o"] + RT, w=["tt2"])
                                        kb.e("vector", "tensor_tensor", out=tt[3][:], in0=x1, in1=sb_, op=ALU.mult, r=["p_o"] + RT, w=["tt3"])
                                        kb.e("vector", "tensor_tensor", out=ko[:, 0:8], in0=tt[0][:], in1=tt[1][:], op=ALU.subtract, r=["tt0", "tt1"], w=["kctok"])
                                        kb.e("vector", "tensor_tensor", out=ko[:, 8:16], in0=tt[2][:], in1=tt[3][:], op=ALU.add, r=["tt2", "tt3"], w=["kctok"])
                                    else:
                                        kb.e("vector", "tensor_scalar", out=vc_ext[:, nt, g_, 0:64], in0=p_o[:, 0:64], scalar1=cval[:, nt:nt + 1],
                                             scalar2=None, op0=ALU.mult, r=["p_o", "cval"], w=["vc_ext"])
                        for nt in range(NCT):
                            kb.tr(p_t[:, nt * 128:(nt + 1) * 128], kctok[:, nt, :], C["ident_bf"][:], r=["kctok", "cb_ident"], w=["p_t"])
                            kb.act(kcT[:, nt * 128:(nt + 1) * 128], p_t[:, nt * 128:(nt + 1) * 128], AF.Copy, r=["p_t"], w=["kcT"])
                            for g_ in range(2):
                                kb.e("vector", "tensor_copy", out=vc_ext[:, nt, g_, 64:65], in_=cval[:, nt:nt + 1], r=["cval"], w=["vc_ext"])
                                kb.e("vector", "tensor_scalar", out=vc_ext[:, nt, g_, 65:193], in0=C["cover"][:, nt, :], scalar1=cval[:, nt:nt + 1],
                                     scalar2=None, op0=ALU.mult, r=["c_cover", "cval"], w=["vc_ext"])
                        P.end_phase()
                QT = sb(nsa, "QT", [128, 8, NTOK], BF16)
                gates = sb(nsa, "gates", [128, NOT, 48])
                with ExitStack() as st:
                    C = load_consts(st, ["ident"], bf=["ident"])
                    stg = [sb(st, "stgA", [128, 2048]), sb(st, "stgB", [128, 2048])]
                    wQ = sb(st, "wQ", [128, 16, 1072], BF16)
                    xTo = [sb(st, "xTq0", [128, 16, 128], BF16), sb(st, "xTq1", [128, 16, 128], BF16)]
                    qtok = sb(st, "qtok", [128, 1024], BF16)
                    tt = [sb(st, "ropeq%d" % q_, [128, 16, 8]) for q_ in range(4)]
                    p_q = ps(st, "p_q", [128, 1024])
                    p_g = ps(st, "p_g", [128, 512])
                    p_t = ps(st, "p_t", [128, 1024], BF16)
                    for k2 in range(0, 16, 2):
                        src = w_in[k2 * 128:(k2 + 2) * 128, C_Q:C_Q + 1024].rearrange("(k p) n -> p k n", p=128)
                        load_cast(stg, wQ[:, k2:k2 + 2, 0:1024], src, [128, 2, 1024], ["wQ"])
                    srcg = w_in[:, C_GATE:C_GATE + 48].rearrange("(k p) n -> p k n", p=128)
                    load_cast(stg, wQ[:, :, 1024:1072], srcg, [128, 16, 48], ["wQ"])
                    for ot in range(NOT):
                        i, t = ot // 2, ot % 2
                        T = 6 + 8 * i + t
                        xb = xTo[ot % 2]
                        rxb = ("xTq", ot % 2)
                        for k8 in range(0, 16, 8):
                            src = xT[k8 * 128:(k8 + 8) * 128, T * 128:(T + 1) * 128].rearrange("(k p) n -> p k n", p=128)
                            load_cast(stg, xb[:, k8:k8 + 8, :], src, [128, 8, 128], [rxb], eng_cast="scalar")
                        for cc in range(2):
                            for k in range(16):
                                kb.mm(p_q[:, cc * 512:(cc + 1) * 512], xb[:, k, :], wQ[:, k, cc * 512:(cc + 1) * 512], k == 0, k == 15,
                                      r=["wQ", rxb], w=["p_q"])
                        for k in range(16):
                            kb.mm(p_g[:, 0:48], xb[:, k, :], wQ[:, k, 1024:1072], k == 0, k == 15, r=["wQ", rxb], w=["p_g"])
                        kb.act(gates[:, ot, :], p_g[:, 0:48], AF.Tanh, scale=0.5, r=["p_g"], w=["gates"])
                        kb.e("vector", "tensor_scalar", out=gates[:, ot, :], in0=gates[:, ot, :], scalar1=0.5, scalar2=0.5, op0=ALU.mult, op1=ALU.add,
                             r=["gates"], w=["gates"])
                        q4o = qtok[:].rearrange("p (r g d) -> p g r d", r=8, g=2)
                        pq4 = p_q[:].rearrange("p (g r d) -> p g r d", g=2, r=8)
                        kb.act(q4o, pq4, AF.Copy, r=["p_q"], w=["qtok"])
                        x1 = pq4[:, :, :, 0:8]
                        x2 = pq4[:, :, :, 8:16]
                        cb3 = bc(cosT[:, T, :].unsqueeze(1).unsqueeze(1), [128, 2, 8, 8])
                        sb3 = bc(sinT[:, T, :].unsqueeze(1).unsqueeze(1), [128, 2, 8, 8])
                        tv = [t_[:].rearrange("p (g r) e -> p g r e", g=2) for t_ in tt]
                        kb.e("vector", "tensor_tensor", out=tv[0], in0=x1, in1=cb3, op=ALU.mult, r=["p_q"], w=["tt0"])
                        kb.e("vector", "tensor_tensor", out=tv[1], in0=x2, in1=sb3, op=ALU.mult, r=["p_q"], w=["tt1"])
                        kb.e("vector", "tensor_tensor", out=tv[2], in0=x2, in1=cb3, op=ALU.mult, r=["p_q"], w=["tt2"])
                        kb.e("vector", "tensor_tensor", out=tv[3], in0=x1, in1=sb3, op=ALU.mult, r=["p_q"], w=["tt3"])
                        kb.e("vector", "tensor_tensor", out=q4o[:, :, :, 0:8], in0=tv[0], in1=tv[1], op=ALU.subtract, r=["tt0", "tt1"], w=["qtok"])
                        kb.e("vector", "tensor_tensor", out=q4o[:, :, :, 8:16], in0=tv[2], in1=tv[3], op=ALU.add, r=["tt2", "tt3"], w=["qtok"])
                        for r_ in range(8):
                            kb.tr(p_t[:, r_ * 128:(r_ + 1) * 128], qtok[:, r_ * 128:(r_ + 1) * 128], C["ident_bf"][:], r=["qtok", "cb_ident"], w=["p_t"])
                        kb.act(QT[:, :, ot * 128:(ot + 1) * 128], p_t[:].rearrange("p (r q) -> p r q", r=8), AF.Copy, r=["p_t"], w=["QT"])
                    P.end_phase()
                with ExitStack() as st:
                    C = load_consts(st, ["ident", "triL", "triU", "cmpmask", "iota_row", "qhalf"],
                                    bf=["ident", "triL", "triU", "cmpmask"])
                    stgN = [sb(st, "stgA", [128, 2048]), sb(st, "stgB", [128, 2048])]
                    C["ebig_bf"] = sb(st, "ebig_bf", [128, 8192], BF16)
                    for q_ in range(4):
                        load_cast(stgN, C["ebig_bf"][:, q_ * 2048:(q_ + 1) * 2048], cd["ebig"][:, q_ * 2048:(q_ + 1) * 2048], [128, 2048], ["cb_ebig"])
                    f0 = sb(st, "f0", [128, 128])
                    kb.dma(f0[:], forced0_d, w=["f0"])
                    pT = [sb(st, "pT%d" % q_, [128, 1024], BF16) for q_ in range(4)]
                    negc = sb(st, "negc", [128, 6, 4, 128], BF16)
                    negsel = sb(st, "negsel", [128, 4, 128], BF16)
                    selTb = sb(st, "selTb", [128, 128], BF16)
                    for mi, msrc in enumerate([C["triL"][:], C["triU"][:]] + [C["cmpmask"][:, v_, :] for v_ in range(4)]):
                        kb.e("vector", "tensor_scalar", out=negc[:, mi, :, :], in0=bc(msrc.unsqueeze(1), [128, 4, 128]), scalar1=-1.0, scalar2=-NEG,
                             op0=ALU.add, op1=ALU.mult, r=["c_triL", "c_triU", "c_cmpmask"], w=["negc"])
                    accC = sb(st, "accC", [128, 8, 193])
                    accS = sb(st, "accS", [128, 8, 65])
                    accW = sb(st, "accW", [128, 8, 65])
                    rz = sb(st, "rz", [128, 3, 8])
                    imp = sb(st, "imp", [128, 128])
                    imp2 = sb(st, "imp2", [128, 128])
                    vmask = sb(st, "vmask", [128, 128])
                    fmask = sb(st, "fmask", [128, 128])
                    f2 = sb(st, "f2", [128, 128])
                    cur = sb(st, "cur", [128, 2])
                    m8 = sb(st, "m8", [128, 16])
                    selb = sb(st, "selb", [128, 128], BF16)
                    selT = sb(st, "selT", [128, 128], BF16)
                    ynsa = sb(st, "ynsa", [128, 8, 64])
                    ytmp = sb(st, "ytmp", [128, 8, 64])
                    ybf = sb(st, "ybf", [128, 512], BF16)
                    p_s = [ps(st, "p_s0", [128, 1024]), ps(st, "p_s1", [128, 1024])]
                    p_acc = ps(st, "p_acc", [128, 2048])

                    DVE_MASK = lambda u: True

                    def attn_pass(qh, units, vcols, per_bank):
                        nu = len(units)

                        def emit_S(u):
                            kT, vext, mask, rk, rv = units[u]
                            sp = p_s[u % 2]
                            rsp = ("p_s", u % 2)
                            for hh in range(2):
                                dve_mask = (mask is not None) and mask[0] == "expand" and DVE_MASK(u)
                                kb.mm(sp[:, hh * 512:(hh + 1) * 512], kT, qh[hh], True, (mask is None) or dve_mask, r=[rk, "QT"], w=[rsp])
                                if mask is not None:
                                    kind, mval, rm = mask
                                    if kind == "expand" and DVE_MASK(u):
                                        pass
                                    elif kind == "expand":
                                        kb.mm(sp[:, hh * 512:(hh + 1) * 512], mval, negsel[:].rearrange("p r q -> p (r q)"), False, True,
                                              r=["cb_ebig", "negsel"], w=[rsp])
                                    else:
                                        kb.mm(sp[:, hh * 512:(hh + 1) * 512], C["ident_bf"][:], negc[:, mval, :, :].rearrange("p r q -> p (r q)"),
                                              False, True, r=["cb_ident", "negc"], w=[rsp])

                        def emit_PV(u):
                            kT, vext, mask, rk, rv = units[u]
                            pt = pT[u % 4]
                            rpt = ("pT", u % 4)
                            for r_ in range(8):
                                bank = r_ // per_bank
                                col = bank * 512 + (r_ % per_bank) * vcols
                                kb.mm(p_acc[:, col:col + vcols], pt[:, r_ * 128:(r_ + 1) * 128], vext,
                                      (u == 0) and (r_ % per_bank == 0), u == nu - 1, r=[rpt, rv], w=[("p_acc", bank)])

                        emit_S(0)
                        for u in range(nu):
                            kT, vext, mask, rk, rv = units[u]
                            if u + 1 < nu:
                                emit_S(u + 1)
                            pt = pT[u % 4]
                            rpt = ("pT", u % 4)
                            kb.act(pt[:], p_s[u % 2][:], AF.Exp, scale=0.125, r=[("p_s", u % 2)], w=[rpt])
                            if (mask is not None) and mask[0] == "expand" and DVE_MASK(u):
                                mps = p_acc[:, 1024 + (u % 2) * 128:1152 + (u % 2) * 128]
                                kb.mm(mps, mask[1], selTb[:], True, True, r=["cb_ebig", "selTb"], w=[("p_acc", 2)])
                                kb.e("vector", "tensor_tensor", out=pt[:].rearrange("p (r q) -> p r q", r=8),
                                     in0=pt[:].rearrange("p (r q) -> p r q", r=8), in1=bc(mps.unsqueeze(1), [128, 8, 128]),
                                     op=ALU.mult, r=[rpt, ("p_acc", 2)], w=[rpt])
                            if u >= 1:
                                emit_PV(u - 1)
                        emit_PV(nu - 1)

                    for ot in range(NOT):
                        i, t = ot // 2, ot % 2
                        T = 6 + 8 * i + t
                        gv = gates[:, ot, :].rearrange("p (h b) -> p h b", b=3)
                        for g_ in range(2):
                            gs = slice(g_ * 64, (g_ + 1) * 64)
                            qh = [QT[gs, 0:4, ot * 128:(ot + 1) * 128], QT[gs, 4:8, ot * 128:(ot + 1) * 128]]
                            ntl = (8 * T) // 128
                            units = []
                            for nt in range(ntl + 1):
                                mk = None
                                if nt == ntl:
                                    v = 2 * (i % 2) + t
                                    mk = ("const", 2 + v, None)
                                units.append((kcT[gs, nt * 128:(nt + 1) * 128], vc_ext[:, nt, g_, :], mk, "kcT", "vc_ext"))
                            attn_pass(qh, units, 193, 2)
                            for b_ in range(4):
                                kb.act(accC[:, 2 * b_:2 * b_ + 2, :], p_acc[:, b_ * 512:b_ * 512 + 386].rearrange("p (r c) -> p r c", r=2),
                                       AF.Copy, r=[("p_acc", b_)], w=["accC"])
                            kb.e("vector", "tensor_scalar", out=rz[:, 0, :], in0=accC[:, :, 64], scalar1=1e-30, scalar2=None, op0=ALU.max,
                                 r=["accC"], w=["rz"])
                            kb.e("vector", "reciprocal", out=rz[:, 0, :], in_=rz[:, 0, :], r=["rz"], w=["rz"])
                            kb.e("vector", "tensor_scalar", out=imp[:], in0=accC[:, 0, 65:193], scalar1=rz[:, 0, 0:1], scalar2=None, op0=ALU.mult,
                                 r=["accC", "rz"], w=["imp"])
                            for r_ in range(1, 8):
                                kb.e("vector", "scalar_tensor_tensor", out=imp[:], in0=accC[:, r_, 65:193], scalar=rz[:, 0, r_:r_ + 1], in1=imp[:],
                                     op0=ALU.mult, op1=ALU.add, r=["accC", "rz", "imp"], w=["imp"])
                            kb.e("vector", "tensor_scalar", out=cur[:, 0:1], in0=C["qhalf"][:], scalar1=float(2 * T), scalar2=None, op0=ALU.add,
                                 r=["c_qhalf"], w=["cur"])
                            kb.e("vector", "tensor_scalar", out=cur[:, 1:2], in0=C["qhalf"][:], scalar1=float(2 * T - 1), scalar2=None, op0=ALU.add,
                                 r=["c_qhalf"], w=["cur"])
                            kb.e("vector", "tensor_scalar", out=vmask[:], in0=C["iota_row"][:], scalar1=cur[:, 0:1], scalar2=None, op0=ALU.is_le,
                                 r=["c_iota_row", "cur"], w=["vmask"])
                            kb.e("vector", "tensor_scalar", out=fmask[:], in0=C["iota_row"][:], scalar1=cur[:, 0:1], scalar2=None, op0=ALU.is_equal,
                                 r=["c_iota_row", "cur"], w=["fmask"])
                            kb.e("vector", "tensor_scalar", out=f2[:], in0=C["iota_row"][:], scalar1=cur[:, 1:2], scalar2=None, op0=ALU.is_equal,
                                 r=["c_iota_row", "cur"], w=["f2"])
                            kb.e("vector", "tensor_tensor", out=fmask[:], in0=fmask[:], in1=f2[:], op=ALU.add, r=["fmask", "f2"], w=["fmask"])
                            kb.e("vector", "tensor_tensor", out=fmask[:], in0=fmask[:], in1=f0[:], op=ALU.add, r=["fmask", "f0"], w=["fmask"])
                            kb.e("vector", "tensor_tensor", out=fmask[:], in0=fmask[:], in1=vmask[:], op=ALU.add, r=["fmask", "vmask"], w=["fmask"])
                            kb.e("vector", "tensor_scalar", out=fmask[:], in0=fmask[:], scalar1=-1.0, scalar2=1.0e4, op0=ALU.add, op1=ALU.mult,
                                 r=["fmask"], w=["fmask"])
                            kb.e("vector", "tensor_tensor", out=imp[:], in0=imp[:], in1=vmask[:], op=ALU.mult, r=["imp", "vmask"], w=["imp"])
                            kb.e("vector", "tensor_tensor", out=imp[:], in0=imp[:], in1=fmask[:], op=ALU.add, r=["imp", "fmask"], w=["imp"])
                            kb.e("vector", "max", out=m8[:, 0:8], in_=imp[:], r=["imp"], w=["m8"])
                            kb.e("vector", "match_replace", out=imp2[:], in_to_replace=m8[:, 0:8], in_values=imp[:], imm_value=-1.0e30,
                                 r=["imp", "m8"], w=["imp2"])
                            kb.e("vector", "max", out=m8[:, 8:16], in_=imp2[:], r=["imp2"], w=["m8"])
                            kb.e("vector", "tensor_scalar", out=selb[:], in0=imp[:], scalar1=m8[:, 15:16], scalar2=None, op0=ALU.is_ge,
                                 r=["imp", "m8"], w=["selb"])
                            pst = p_acc[:, 1536:2048].bitcast(BF16)
                            kb.tr(pst[:, 0:128], selb[:], C["ident_bf"][:], r=["selb", "cb_ident"], w=[("p_acc", 3)])
                            kb.act(selTb[:], pst[:, 0:128], AF.Copy, r=[("p_acc", 3)], w=["selTb"])
                            kb.e("vector", "tensor_scalar", out=negsel[:], in0=bc(pst[:, 0:128].unsqueeze(1), [128, 4, 128]), scalar1=-1.0, scalar2=-NEG,
                                 op0=ALU.add, op1=ALU.mult, r=[("p_acc", 3)], w=["negsel"])
                            kb.e("vector", "tensor_tensor", out=rz[:, 0, :], in0=rz[:, 0, :], in1=gv[:, g_ * 8:(g_ + 1) * 8, 0], op=ALU.mult,
                                 r=["rz", "gates"], w=["rz"])
                            kb.e("vector", "tensor_tensor", out=ynsa[:], in0=accC[:, :, 0:64], in1=bc(rz[:, 0, :].unsqueeze(2), [128, 8, 64]),
                                 op=ALU.mult, r=["accC", "rz"], w=["ynsa"])
                            for br, (Kt, Vt, acc_sb, kts) in ((1, (KwinT, Vwin, accW, list(range(T - 4, T + 1)))),
                                                              (0, (KselT, Vsel, accS, list(range(0, T + 1))))):
                                units = []
                                for kt in kts:
                                    if br == 0:
                                        mk = ("expand", C["ebig_bf"][:, kt * 128:(kt + 1) * 128], None) if kt < T else ("const", 0, None)
                                    else:
                                        mk = ("const", 1, None) if kt == T - 4 else (("const", 0, None) if kt == T else None)
                                    units.append((Kt[gs, kt * 128:(kt + 1) * 128], Vt[:, kt, g_, :], mk, "KselT" if br == 0 else "KwinT",
                                                  "Vsel" if br == 0 else "Vwin"))
                                attn_pass(qh, units, 65, 4)
                                for b_ in range(2):
                                    kb.act(acc_sb[:, 4 * b_:4 * b_ + 4, :], p_acc[:, b_ * 512:b_ * 512 + 260].rearrange("p (r c) -> p r c", r=4),
                                           AF.Copy, r=[("p_acc", b_)], w=["acc%d" % br])
                                kb.e("vector", "tensor_scalar", out=rz[:, 1 + br, :], in0=acc_sb[:, :, 64], scalar1=1e-30, scalar2=None, op0=ALU.max,
                                     r=["acc%d" % br], w=["rz"])
                                kb.e("vector", "reciprocal", out=rz[:, 1 + br, :], in_=rz[:, 1 + br, :], r=["rz"], w=["rz"])
                                kb.e("vector", "tensor_tensor", out=rz[:, 1 + br, :], in0=rz[:, 1 + br, :], in1=gv[:, g_ * 8:(g_ + 1) * 8, 1 + br], op=ALU.mult,
                                     r=["rz", "gates"], w=["rz"])
                                kb.e("vector", "tensor_tensor", out=ytmp[:], in0=acc_sb[:, :, 0:64], in1=bc(rz[:, 1 + br, :].unsqueeze(2), [128, 8, 64]),
                                     op=ALU.mult, r=["acc%d" % br, "rz"], w=["ytmp"])
                                kb.e("vector", "tensor_tensor", out=ynsa[:], in0=ynsa[:], in1=ytmp[:], op=ALU.add, r=["ynsa", "ytmp"], w=["ynsa"])
                            kb.e("vector", "tensor_copy", out=ybf[:], in_=ynsa[:].rearrange("p r d -> p (r d)"), r=["ynsa"], w=["ybf"])
                            kb.dma(mix_d[ot * 128:(ot + 1) * 128, g_ * 512:(g_ + 1) * 512], ybf[:], r=["ybf"], w=["mix_d"])
                    P.end_phase()

        def dump_mix():
            with ExitStack() as st:
                t32 = sb(st, "t32", [128, 2048])
                tb = sb(st, "tb", [128, 2048], BF16)
                for tt_ in range(NOT):
                    kb.dma(tb[:], mix_d[tt_ * 128:(tt_ + 1) * 128, :], w=["tb"])
                    kb.e("vector", "tensor_copy", out=t32[:], in_=tb[:], r=["tb"], w=["t32"])
                    kb.dma(out_d[tt_ * 128:(tt_ + 1) * 128, :], t32[:], r=["t32"], w=["out"])
                P.end_phase()


        def layer_norm_tile(rin, outt, gt, bt, stats, mv, rres, wres):
            for c4 in range(4):
                kb.e("vector", "bn_stats", out=stats[:, c4, :], in_=rin[:, c4 * 512:(c4 + 1) * 512], r=[rres], w=["lnstats"])
            kb.e("vector", "bn_aggr", out=mv[:, 0:2], in_=stats[:].rearrange("p a b -> p (a b)"), r=["lnstats"], w=["lnmv"])
            kb.e("vector", "tensor_scalar", out=mv[:, 2:3], in0=mv[:, 1:2], scalar1=EPS, scalar2=None, op0=ALU.add, r=["lnmv"], w=["lnmv"])
            kb.act(mv[:, 2:3], mv[:, 2:3], AF.Sqrt, r=["lnmv"], w=["lnmv"])
            kb.e("vector", "reciprocal", out=mv[:, 2:3], in_=mv[:, 2:3], r=["lnmv"], w=["lnmv"])
            kb.e("vector", "tensor_scalar", out=rin, in0=rin, scalar1=mv[:, 0:1], scalar2=mv[:, 2:3], op0=ALU.subtract, op1=ALU.mult,
                 r=[rres, "lnmv"], w=[rres])
            kb.e("vector", "tensor_tensor", out=rin, in0=rin, in1=gt, op=ALU.mult, r=[rres, "lng"], w=[rres])
            kb.e("vector", "tensor_tensor", out=outt, in0=rin, in1=bt, op=ALU.add, r=[rres, "lnb"], w=[wres])

        def moe_all():
            HT = NOT // 2
            with ExitStack() as outer:
                slotidx = sb(outer, "slotidx", [128, NOT, 32])
                Wsp = sb(outer, "Wsp", [128, NOT, 32, 2], BF16)
                with ExitStack() as st:
                    C = load_consts(st, ["ident", "ones", "triS"], bf=["ident", "ones", "triS"])
                    stg = [sb(st, "stgA", [128, 4096]), sb(st, "stgB", [128, 4096])]
                    wo = sb(st, "wo", [128, 16, 2048], BF16)
                    wr = sb(st, "wr", [128, 16, 36])
                    brt = sb(st, "brt", [128, 36])
                    g1 = sb(st, "g1", [128, 2048])
                    b1_ = sb(st, "b1_", [128, 2048])
                    mixb2 = [sb(st, "mixb0", [128, 2048], BF16), sb(st, "mixb1", [128, 2048], BF16)]
                    mixT2 = [sb(st, "mixT0", [128, 16, 128], BF16), sb(st, "mixT1", [128, 16, 128], BF16)]
                    xt2 = [sb(st, "xt0", [128, 2048]), sb(st, "xt1", [128, 2048])]
                    rr2 = [sb(st, "rr0", [128, 2048]), sb(st, "rr1", [128, 2048])]
                    hh = sb(st, "hh", [128, 2048])
                    hb = sb(st, "hb", [128, 2048], BF16)
                    hT32 = sb(st, "hT32", [128, 16, 128])
                    stats = sb(st, "stats", [128, 4, 6])
                    mv = sb(st, "mv", [128, 4])
                    lg = sb(st, "lg", [128, 36])
                    sm_ = sb(st, "smr", [128, 16])
                    ohg = sb(st, "ohg", [128, 4])
                    eg = sb(st, "eg", [128, 4])
                    les = sb(st, "les", [128, 8])
                    m8 = sb(st, "m8", [128, 8])
                    oh1 = sb(st, "oh1", [128, 8])
                    oh2 = sb(st, "oh2", [128, 8])
                    A8 = sb(st, "A8", [128, 8])
                    W8 = sb(st, "W8", [128, 8])
                    Aall = sb(st, "Aall", [128, NOT, 32])
                    Abf = sb(st, "Abf", [128, NOT, 32], BF16)
                    Wall = sb(st, "Wall", [128, NOT, 32])
                    Whi = sb(st, "Whi", [128, NOT, 32], BF16)
                    Wtmp = sb(st, "Wtmp", [128, NOT, 32])
                    p_o = ps(st, "p_o", [128, 2048])
                    p_t = ps(st, "p_t", [128, 1024], BF16)
                    p_r = ps(st, "p_r", [128, 512])
                    p_l = ps(st, "p_l", [128, 512])
                    for k2 in range(0, 16, 2):
                        src = w_out_d[k2 * 128:(k2 + 2) * 128, :].rearrange("(k p) n -> p k n", p=128)
                        load_cast(stg, wo[:, k2:k2 + 2, :], src, [128, 2, 2048], ["wo"])
                    kb.dma(wr[:], w_router_d.rearrange("(k p) n -> p k n", p=128), w=["wr"])
                    kb.dma(brt[:], b_router_d, w=["brt"])
                    kb.dma(g1[:], ln1g_d, w=["lng"])
                    kb.dma(b1_[:], ln1b_d, w=["lnb"])
                    def stage_OA(ot):
                        rows = slice(ot * 128, (ot + 1) * 128)
                        mixb, mixT, xt, rr = mixb2[ot % 2], mixT2[ot % 2], xt2[ot % 2], rr2[ot % 2]
                        RO = lambda n_: (n_, ot % 2)
                        kb.dma(mixb[:], mix_d[rows, :], w=[RO("mixb")])
                        kb.dma(xt[:], x_own[rows, :], w=[RO("xt")])
                        for k8 in range(2):
                            for k in range(8):
                                kk = k8 * 8 + k
                                kb.tr(p_t[:, k * 128:(k + 1) * 128], mixb[:, kk * 128:(kk + 1) * 128], C["ident_bf"][:], r=[RO("mixb"), "cb_ident"], w=["p_t"])
                            kb.act(mixT[:, k8 * 8:(k8 + 1) * 8, :], p_t[:].rearrange("p (k q) -> p k q", k=8), AF.Copy, r=["p_t"], w=[RO("mixT")])
                        for dc in range(4):
                            for k in range(16):
                                kb.mm(p_o[:, dc * 512:(dc + 1) * 512], mixT[:, k, :], wo[:, k, dc * 512:(dc + 1) * 512], k == 0, k == 15,
                                      r=[RO("mixT"), "wo"], w=[("p_o", dc)])
                        kb.e("vector", "scalar_tensor_tensor", out=rr[:], in0=xt[:], scalar=ALPHA, in1=p_o[:], op0=ALU.mult, op1=ALU.add,
                             r=[RO("xt")] + [("p_o", dc) for dc in range(4)], w=[RO("rr")])
                    def stage_OB(ot):
                        rows = slice(ot * 128, (ot + 1) * 128)
                        mixb, mixT, xt, rr = mixb2[ot % 2], mixT2[ot % 2], xt2[ot % 2], rr2[ot % 2]
                        RO = lambda n_: (n_, ot % 2)
                        layer_norm_tile(rr[:], hh[:], g1[:], b1_[:], stats, mv, RO("rr"), "hh")
                        kb.dma(h32_d[rows, :], hh[:], r=["hh"], w=["h32_d"])
                        kb.act(hb[:], hh[:], AF.Copy, r=["hh"], w=["hb"])
                        kb.dma(hbf_d[rows, :], hb[:], r=["hb"], w=["hbf_d"])
                        for k4 in range(4):
                            for k in range(4):
                                kk = k4 * 4 + k
                                kb.tr(p_r[:, k * 128:(k + 1) * 128], hh[:, kk * 128:(kk + 1) * 128], C["ident"][:], r=["hh", "c_ident"], w=["p_r"])
                            kb.act(hT32[:, k4 * 4:(k4 + 1) * 4, :], p_r[:].rearrange("p (k q) -> p k q", k=4), AF.Copy, r=["p_r"], w=["hT32"])
                        for k in range(16):
                            kb.mm(p_l[:, 0:36], hT32[:, k, :], wr[:, k, :], k == 0, k == 15, r=["hT32", "wr"], w=["p_l"])
                        R = ["route"]
                        kb.e("vector", "tensor_tensor", out=lg[:], in0=p_l[:, 0:36], in1=brt[:], op=ALU.add, r=["p_l", "brt"], w=R)
                        kb.e("vector", "tensor_reduce", out=sm_[:, 0:1], in_=lg[:, 0:4], axis=AX.X, op=ALU.max, r=R, w=R)
                        kb.e("vector", "tensor_scalar", out=sm_[:, 1:2], in0=sm_[:, 0:1], scalar1=-1.0, scalar2=None, op0=ALU.mult, r=R, w=R)
                        kb.act(eg[:], lg[:, 0:4], AF.Exp, bias=sm_[:, 1:2], r=R, w=R)
                        kb.e("vector", "tensor_reduce", out=sm_[:, 2:3], in_=eg[:], axis=AX.X, op=ALU.add, r=R, w=R)
                        kb.e("vector", "reciprocal", out=sm_[:, 3:4], in_=sm_[:, 2:3], r=R, w=R)
                        kb.e("vector", "tensor_scalar", out=ohg[:], in0=lg[:, 0:4], scalar1=sm_[:, 0:1], scalar2=None, op0=ALU.is_equal, r=R, w=R)
                        kb.e("vector", "tensor_scalar", out=les[:], in0=lg[:, 4:12], scalar1=ohg[:, 0:1], scalar2=None, op0=ALU.mult, r=R, w=R)
                        for g_ in range(1, 4):
                            kb.e("vector", "scalar_tensor_tensor", out=les[:], in0=lg[:, 4 + 8 * g_:12 + 8 * g_], scalar=ohg[:, g_:g_ + 1], in1=les[:],
                                 op0=ALU.mult, op1=ALU.add, r=R, w=R)
                        kb.e("vector", "max", out=m8[:], in_=les[:], r=R, w=R)
                        kb.e("vector", "tensor_tensor", out=sm_[:, 4:5], in0=m8[:, 1:2], in1=m8[:, 0:1], op=ALU.subtract, r=R, w=R)
                        kb.act(sm_[:, 5:6], sm_[:, 4:5], AF.Exp, r=R, w=R)
                        kb.e("vector", "tensor_scalar", out=sm_[:, 6:7], in0=sm_[:, 5:6], scalar1=1.0, scalar2=None, op0=ALU.add, r=R, w=R)
                        kb.e("vector", "reciprocal", out=sm_[:, 6:7], in_=sm_[:, 6:7], r=R, w=R)
                        kb.e("vector", "tensor_tensor", out=sm_[:, 7:8], in0=sm_[:, 5:6], in1=sm_[:, 6:7], op=ALU.mult, r=R, w=R)
                        kb.e("vector", "tensor_tensor", out=sm_[:, 8:9], in0=sm_[:, 6:7], in1=sm_[:, 3:4], op=ALU.mult, r=R, w=R)
                        kb.e("vector", "tensor_tensor", out=sm_[:, 9:10], in0=sm_[:, 7:8], in1=sm_[:, 3:4], op=ALU.mult, r=R, w=R)
                        kb.e("vector", "tensor_scalar", out=oh1[:], in0=les[:], scalar1=m8[:, 0:1], scalar2=None, op0=ALU.is_equal, r=R, w=R)
                        kb.e("vector", "tensor_scalar", out=oh2[:], in0=les[:], scalar1=m8[:, 1:2], scalar2=None, op0=ALU.is_equal, r=R, w=R)
                        kb.e("vector", "tensor_tensor", out=A8[:], in0=oh1[:], in1=oh2[:], op=ALU.add, r=R, w=R)
                        kb.e("vector", "tensor_scalar", out=W8[:], in0=oh1[:], scalar1=sm_[:, 8:9], scalar2=None, op0=ALU.mult, r=R, w=R)
                        kb.e("vector", "scalar_tensor_tensor", out=W8[:], in0=oh2[:], scalar=sm_[:, 9:10], in1=W8[:], op0=ALU.mult, op1=ALU.add, r=R, w=R)
                        o3 = bc(ohg[:].unsqueeze(2), [128, 4, 8])
                        kb.e("vector", "tensor_tensor", out=Aall[:, ot, :].rearrange("p (g e) -> p g e", g=4), in0=o3,
                             in1=bc(A8[:].unsqueeze(1), [128, 4, 8]), op=ALU.mult, r=R, w=["Aall"])
                        kb.e("vector", "tensor_tensor", out=Wall[:, ot, :].rearrange("p (g e) -> p g e", g=4), in0=o3,
                             in1=bc(W8[:].unsqueeze(1), [128, 4, 8]), op=ALU.mult, r=R, w=["Wall"])
                    stage_OA(0)
                    for ot in range(NOT):
                        if ot + 1 < NOT:
                            stage_OA(ot + 1)
                        stage_OB(ot)
                    kb.e("vector", "tensor_copy", out=Abf[:], in_=Aall[:], r=["Aall"], w=["Abf"])
                    for tt in range(NOT):
                        hf = tt // HT
                        prev = list(range(hf * HT, tt))
                        for n_, tp in enumerate(prev):
                            kb.mm(p_l[:, 64:96], C["ones_bf"][:], Abf[:, tp, :], n_ == 0, False, r=["Abf", "cb_ones"], w=["p_l"])
                        kb.mm(p_l[:, 64:96], C["triS_bf"][:], Abf[:, tt, :], len(prev) == 0, True, r=["Abf", "cb_triS"], w=["p_l"])
                        kb.e("vector", "scalar_tensor_tensor", out=slotidx[:, tt, :], in0=p_l[:, 64:96], scalar=1.0, in1=Aall[:, tt, :],
                             op0=ALU.add, op1=ALU.mult, r=["p_l", "Aall"], w=["slotidx"])
                    kb.e("vector", "tensor_scalar", out=slotidx[:], in0=slotidx[:], scalar1=-1.0, scalar2=None, op0=ALU.add, r=["slotidx"], w=["slotidx"])
                    kb.e("vector", "tensor_copy", out=Whi[:], in_=Wall[:], r=["Wall"], w=["Whi"])
                    kb.e("vector", "tensor_copy", out=Wsp[:, :, :, 0], in_=Whi[:], r=["Whi"], w=["Wsp"])
                    kb.e("vector", "tensor_tensor", out=Wtmp[:], in0=Wall[:], in1=Whi[:], op=ALU.subtract, r=["Wall", "Whi"], w=["Wtmp"])
                    kb.e("vector", "tensor_copy", out=Wsp[:, :, :, 1], in_=Wtmp[:], r=["Wtmp"], w=["Wsp"])
                    P.end_phase()
                if stop == "h":
                    with ExitStack() as st:
                        t32 = sb(st, "t32", [128, 2048])
                        for tt_ in range(NOT):
                            kb.dma(t32[:], h32_d[tt_ * 128:(tt_ + 1) * 128, :], w=["t32"])
                            kb.dma(out_d[tt_ * 128:(tt_ + 1) * 128, :], t32[:], r=["t32"], w=["out"])
                        P.end_phase()
                    return
                for hf in range(2):
                    with ExitStack() as accst:
                        acc = sb(accst, "acc", [128, HT, 2048])
                        with ExitStack() as st:
                            C = load_consts(st, ["ident", "iota_row"], bf=["ident"])
                            stg = [sb(st, "stgA", [128, 2048]), sb(st, "stgB", [128, 2048]), sb(st, "stgC", [128, 2048]), sb(st, "stgD", [128, 2048])]
                            hbf = sb(st, "hbf", [128, HT, 2048], BF16)
                            wg = sb(st, "wg", [128, 16, 512], BF16)
                            wu = sb(st, "wu", [128, 16, 512], BF16)
                            wd = sb(st, "wd", [128, 4, 2048], BF16)
                            S = sb(st, "S", [128, HT, 128], BF16)
                            ST2 = [sb(st, "ST0", [128, HT * 128], BF16), sb(st, "ST1", [128, HT * 128], BF16)]
                            XeT = sb(st, "XeT", [128, 16, 128], BF16)
                            sgl = sb(st, "sgl", [128, 512])
                            aT2 = [sb(st, "aT0", [128, 512], BF16), sb(st, "aT1", [128, 512], BF16)]
                            wsl2 = [sb(st, "wsl0", [128, 4]), sb(st, "wsl1", [128, 4])]
                            Yw = sb(st, "Yw", [128, 2048], BF16)
                            pg = [ps(st, "p_g0", [128, 512]), ps(st, "p_g1", [128, 512])]
                            pG = ps(st, "p_G", [128, 512])
                            pU = ps(st, "p_U", [128, 512])
                            pY = ps(st, "p_Y", [128, 2048])
                            pUb = pU[:, :].bitcast(BF16)
                            kb.e("vector", "memset", acc[:].rearrange("p a b -> p (a b)"), 0.0, w=[("acc", 0), ("acc", 1)])
                            for tt in range(HT):
                                kb.dma(hbf[:, tt, :], hbf_d[(hf * HT + tt) * 128:(hf * HT + tt + 1) * 128, :], w=["hbf"])
                            def load_gu(e_):
                                for q in range(4):
                                    srcg = w_gate_d[e_, q * 512:(q + 1) * 512, :].rearrange("(k p) n -> p k n", p=128)
                                    load_cast(stg, wg[:, 4 * q:4 * q + 4, :], srcg, [128, 4, 512], ["wg"], eng_cast="scalar")
                                for q in range(4):
                                    srcu = w_up_d[e_, q * 512:(q + 1) * 512, :].rearrange("(k p) n -> p k n", p=128)
                                    load_cast(stg, wu[:, 4 * q:4 * q + 4, :], srcu, [128, 4, 512], ["wu"], eng_cast="scalar")

                            def load_d(e_):
                                for q in range(4):
                                    srcd = w_down_d[e_, q * 128:(q + 1) * 128, :]
                                    load_cast(stg, wd[:, q, :], srcd, [128, 2048], ["wd"], eng_cast="vector")

                            def stage_A(e_):
                                par = e_ % 2
                                STp, aTp, wslp = ST2[par], aT2[par], wsl2[par]
                                for tt in range(HT):
                                    kb.e("vector", "tensor_scalar", out=S[:, tt, :], in0=C["iota_row"][:], scalar1=slotidx[:, hf * HT + tt, e_:e_ + 1],
                                         scalar2=None, op0=ALU.is_equal, r=["c_iota_row", "slotidx"], w=["S"])
                                for tt in range(HT):
                                    kb.tr(pUb[:, tt * 128:(tt + 1) * 128], S[:, tt, :], C["ident_bf"][:], r=["S", "cb_ident"], w=["p_U"])
                                kb.act(STp[:], pUb[:, 0:HT * 128], AF.Copy, r=["p_U"], w=[("ST", par)])
                                for tt in range(HT):
                                    kb.mm(pG[:, 0:2], S[:, tt, :], Wsp[:, hf * HT + tt, e_, :], tt == 0, tt == HT - 1, r=["S", "Wsp"], w=["p_G"])
                                kb.e("vector", "tensor_reduce", out=wslp[:, 0:1], in_=pG[:, 0:2], axis=AX.X, op=ALU.add, r=["p_G"], w=[("wsl", par)])
                                for q in range(4):
                                    pgq = pg[q % 2]
                                    for d4 in range(4):
                                        dk = 4 * q + d4
                                        for tt in range(HT):
                                            kb.mm(pgq[:, d4 * 128:(d4 + 1) * 128], hbf[:, tt, dk * 128:(dk + 1) * 128], S[:, tt, :],
                                                  (tt == 0) and (d4 == 0), tt == HT - 1, r=["hbf", "S"], w=[("p_g", q % 2)])
                                    kb.act(XeT[:, 4 * q:4 * q + 4, :], pgq[:].rearrange("p (a b) -> p a b", a=4), AF.Copy, r=[("p_g", q % 2)], w=["XeT"])
                                for (pp, ww, rp, rw) in ((pG, wg, "p_G", "wg"), (pU, wu, "p_U", "wu")):
                                    for hc in range(4):
                                        for dk in range(16):
                                            kb.mm(pp[:, hc * 128:(hc + 1) * 128], ww[:, dk, hc * 128:(hc + 1) * 128], XeT[:, dk, :],
                                                  (dk == 0) and (hc == 0), dk == 15, r=[rw, "XeT"], w=[rp])
                                kb.act(sgl[:], pG[:], AF.Silu, r=["p_G"], w=["sgl"])
                                kb.e("vector", "tensor_tensor", out=aTp[:], in0=sgl[:], in1=pU[:], op=ALU.mult, r=["sgl", "p_U"], w=[("aT", par)])

                            def stage_B(e_):
                                par = e_ % 2
                                STp, aTp, wslp = ST2[par], aT2[par], wsl2[par]
                                for dc in range(4):
                                    for hc in range(4):
                                        kb.mm(pY[:, dc * 512:(dc + 1) * 512], aTp[:, hc * 128:(hc + 1) * 128], wd[:, hc, dc * 512:(dc + 1) * 512],
                                              hc == 0, hc == 3, r=[("aT", par), "wd"], w=[("p_Y", dc)])
                                if e_ + 1 < E_N:
                                    load_d(e_ + 1)
                                for h2 in range(2):
                                    kb.act(Yw[:, h2 * 1024:(h2 + 1) * 1024], pY[:, h2 * 1024:(h2 + 1) * 1024], AF.Identity, scale=wslp[:, 0:1],
                                           r=[("p_Y", 2 * h2), ("p_Y", 2 * h2 + 1), ("wsl", par)], w=[("Yw", h2)])
                                for tt in range(HT):
                                    for h2 in range(2):
                                        for dc in range(2 * h2, 2 * h2 + 2):
                                            kb.mm(pY[:, dc * 512:(dc + 1) * 512], STp[:, tt * 128:(tt + 1) * 128], Yw[:, dc * 512:(dc + 1) * 512],
                                                  True, True, r=[("ST", par), ("Yw", h2)], w=[("p_Y", dc)])
                                        kb.e("vector", "tensor_tensor", out=acc[:, tt, h2 * 1024:(h2 + 1) * 1024], in0=acc[:, tt, h2 * 1024:(h2 + 1) * 1024],
                                             in1=pY[:, h2 * 1024:(h2 + 1) * 1024], op=ALU.add,
                                             r=[("acc", h2), ("p_Y", 2 * h2), ("p_Y", 2 * h2 + 1)], w=[("acc", h2)])

                            load_gu(0)
                            load_d(0)
                            stage_A(0)
                            for e_ in range(E_N):
                                if e_ + 1 < E_N:
                                    load_gu(e_ + 1)
                                    stage_A(e_ + 1)
                                stage_B(e_)
                            P.end_phase()
                        with ExitStack() as st:
                            g2 = sb(st, "g2", [128, 2048])
                            b2_ = sb(st, "b2_", [128, 2048])
                            ht = [sb(st, "ht0", [128, 2048]), sb(st, "ht1", [128, 2048])]
                            oo = [sb(st, "oo0", [128, 2048]), sb(st, "oo1", [128, 2048])]
                            stats = sb(st, "stats", [128, 4, 6])
                            mv = sb(st, "mv", [128, 4])
                            kb.dma(g2[:], ln2g_d, w=["lng"])
                            kb.dma(b2_[:], ln2b_d, w=["lnb"])
                            for tt in range(HT):
                                rows = slice((hf * HT + tt) * 128, (hf * HT + tt + 1) * 128)
                                hx = ht[tt % 2]
                                ox = oo[tt % 2]
                                kb.dma(hx[:], h32_d[rows, :], w=[("ht", tt % 2)])
                                kb.e("vector", "scalar_tensor_tensor", out=hx[:], in0=hx[:], scalar=ALPHA, in1=acc[:, tt, :], op0=ALU.mult, op1=ALU.add,
                                     r=[("ht", tt % 2), "acc"], w=[("ht", tt % 2)])
                                layer_norm_tile(hx[:], ox[:], g2[:], b2_[:], stats, mv, ("ht", tt % 2), ("oo", tt % 2))
                                kb.dma(out_d[rows, :], ox[:], r=[("oo", tt % 2)], w=["out"])
                            P.end_phase()

        phase_S1()
        phase_S2()
        if stop == "mix":
            nsa_all()
            dump_mix()
            return nc
        if stop == "ssm":
            with ExitStack() as st:
                t32 = sb(st, "t32", [128, 2048])
                tb = sb(st, "tb", [128, 2048], BF16)
                for tt in range(NOT):
                    kb.dma(tb[:, 1024:2048], mix_d[tt * 128:(tt + 1) * 128, 1024:2048], w=["tb"])
                    kb.e("vector", "memset", t32[:, 0:1024], 0.0, w=["t32"])
                    kb.e("vector", "tensor_copy", out=t32[:, 1024:2048], in_=tb[:, 1024:2048], r=["tb"], w=["t32"])
                    kb.dma(out_d[tt * 128:(tt + 1) * 128, :], t32[:], r=["t32"], w=["out"])
                P.end_phase()
            return nc
        nsa_all()
        moe_all()
    return nc


def run(inputs, SEQ, B, stop="all", debug=False):
    maps = prepare_inputs(inputs, SEQ, B)
    nc = build(SEQ, stop=stop, debug=debug)
    if stop != "all":
        used = None
    res = run_bass_kernel_spmd(nc, maps, core_ids=list(range(4 * B)))
    NOWN = SEQ // 1024
    out = np.zeros((B, SEQ, D), np.float32)
    for b in range(B):
        for j in range(4):
            o = np.asarray(res.results[b * 4 + j]["out"])
            for i in range(NOWN):
                c = 4 * i + j
                out[b, c * 256:(c + 1) * 256] = o[i * 256:(i + 1) * 256]
    return out, res


def kernel(**inputs):
    out, _ = run(inputs, 8192, 2)
    return out
```

```python
import numpy as np
from contextlib import ExitStack
import concourse.bass as bass
import concourse.mybir as mybir
from concourse.bass_utils import run_bass_kernel_spmd

F32 = mybir.dt.float32
BF16 = mybir.dt.bfloat16
I32 = mybir.dt.int32
AF = mybir.ActivationFunctionType
ALU = mybir.AluOpType
AX = mybir.AxisListType

D = 2048
NEG = -30000.0
ALPHA = 2.0 ** 0.25
EPS = 1e-5
C_Q, C_KV, C_GATE, C_Z, C_XS, C_B, C_C, C_DT = 0, 1024, 1792, 1840, 2864, 3888, 4400, 4912


class Prog:
    ENGS = ("tensor", "vector", "scalar", "gpsimd", "sync")

    def __init__(self, nc, stack):
        self.nc = nc
        self.ops = {e: [] for e in self.ENGS}
        self.cnt = {e: 0 for e in self.ENGS}
        self.seen = {e: {} for e in self.ENGS}
        self.last_w = {}
        self.readers = {}
        self.ndma = 0
        self.DMAK = 32
        self.dma_cnt = [0] * self.DMAK
        self.sems = {}
        for e in self.ENGS:
            self.sems[e] = stack.enter_context(nc.semaphore("s_" + e))
        for i in range(self.DMAK):
            self.sems[("dma", i)] = stack.enter_context(nc.semaphore("s_dma%d" % i))
        self.nphase = 0

    def _need(self, eng, dep):
        key, val = dep
        if key == eng and eng == "tensor":
            return
        if self.seen[eng].get(key, 0) >= val:
            return
        self.seen[eng][key] = val
        self.ops[eng].append(("wait", (key, val)))

    def _deps(self, eng, reads, writes):
        for r in reads:
            if r in self.last_w:
                self._need(eng, self.last_w[r])
        for w in writes:
            if w in self.last_w:
                self._need(eng, self.last_w[w])
            for k, v in self.readers.get(w, {}).items():
                self._need(eng, (k, v))

    def _mark(self, me, reads, writes):
        for r in reads:
            d = self.readers.setdefault(r, {})
            if d.get(me[0], 0) < me[1]:
                d[me[0]] = me[1]
        for w in writes:
            self.last_w[w] = me
            self.readers[w] = {}

    @staticmethod
    def _excl(reads, writes):
        def isp(r):
            n = r[0] if isinstance(r, tuple) else r
            return isinstance(n, str) and n.startswith("p_")
        ex = [r for r in reads if isp(r)]
        if ex:
            reads = [r for r in reads if not isp(r)]
            writes = list(writes) + ex
        return reads, writes

    def op(self, eng, fn, reads=(), writes=()):
        reads, writes = self._excl(reads, writes)
        self._deps(eng, reads, writes)
        self.cnt[eng] += 1
        me = (eng, self.cnt[eng])
        self.ops[eng].append(("op", fn))
        self._mark(me, reads, writes)

    def dma(self, eng, out, in_, reads=(), writes=()):
        slot = self.ndma % self.DMAK
        self.ndma += 1
        key = ("dma", slot)
        if self.dma_cnt[slot] > 0:
            self._need(eng, (key, 16 * self.dma_cnt[slot]))
        self._deps(eng, reads, writes)
        self.dma_cnt[slot] += 1
        me = (key, 16 * self.dma_cnt[slot])
        self.ops[eng].append(("dma", (out, in_, slot)))
        self._mark(me, reads, writes)

    def end_phase(self):
        for slot in range(self.DMAK):
            if self.dma_cnt[slot] > 0:
                self._need("sync", (("dma", slot), 16 * self.dma_cnt[slot]))
        nc = self.nc
        sems = self.sems
        ops = self.ops
        with nc.Block() as block:
            def mk(e):
                def body(engine):
                    for kind, p in ops[e]:
                        if kind == "wait":
                            engine.wait_ge(sems[p[0]], p[1])
                        elif kind == "op":
                            p(engine).then_inc(sems[e], 1)
                        else:
                            out, in_, slot = p
                            engine.dma_start(out=out, in_=in_).then_inc(sems[("dma", slot)], 16)
                return body
            for e in self.ENGS:
                if ops[e]:
                    getattr(block, e)(mk(e))
        self.ops = {e: [] for e in self.ENGS}
        self.last_w = {}
        self.readers = {}
        self.nphase += 1


class KB:
    def __init__(self, nc, P):
        self.nc = nc
        self.P = P

    def e(self, eng, method, *args, r=(), w=(), **kw):
        self.P.op(eng, lambda en: getattr(en, method)(*args, **kw), r, w)

    def mm(self, out, lhsT, rhs, start, stop, r=(), w=()):
        self.P.op("tensor", lambda en: en.matmul(out, lhsT=lhsT, rhs=rhs, start=start, stop=stop,
                                                 skip_group_check=True), r, w)

    def tr(self, out, in_, ident, r=(), w=()):
        self.P.op("tensor", lambda en: en.transpose(out, in_, ident), r, w)

    def act(self, out, in_, func, r=(), w=(), **kw):
        self.P.op("scalar", lambda en: en.activation(out=out, in_=in_, func=func, **kw), r, w)

    def dma(self, out, in_, r=(), w=(), eng="sync"):
        self.P.dma(eng, out, in_, r, w)


def bc(ap, shape):
    return ap.broadcast_to(list(shape))


def make_consts():
    c = {}
    k = np.arange(128)[:, None]
    q = np.arange(128)[None, :]
    c["ident"] = np.eye(128, dtype=np.float32)
    c["triL"] = (k <= q).astype(np.float32)
    c["triU"] = (k > q).astype(np.float32)
    c["triS"] = (k < q).astype(np.float32)
    l = np.arange(256)[None, :]
    c["triA"] = (k <= l).astype(np.float32)
    c["triB"] = ((k + 128) <= l).astype(np.float32)
    c["negA"] = np.where(k <= l, 0.0, NEG).astype(np.float32)
    c["negB"] = np.where((k + 128) <= l, 0.0, NEG).astype(np.float32)
    c["ones"] = np.ones((128, 128), np.float32)
    c["iota_row"] = np.broadcast_to(np.arange(128, dtype=np.float32)[None, :], (128, 128)).copy()
    c["iota_col"] = np.arange(128, dtype=np.float32)[:, None].copy()
    c["qhalf"] = (np.arange(128) >= 64).astype(np.float32)[:, None].copy()
    key = np.arange(8192)[None, :]
    c["ebig"] = ((key // 64) == k).astype(np.float32)
    n = np.arange(512)[:, None]
    b = np.arange(128)[None, :]
    cover = ((n >= 4 * b - 1) & (n <= 4 * b + 3)).astype(np.float32)
    c["cover"] = cover.reshape(4, 128, 128).transpose(1, 0, 2).copy()
    cm = np.zeros((128, 4, 128), np.float32)
    for v in range(4):
        o = 48 + 64 * (v // 2) + 8 * (v % 2)
        cm[:, v, :] = ((16 * k + 31) <= (16 * o + q)).astype(np.float32)
    c["cmpmask"] = cm
    inv = (np.float32(500000.0) ** (-np.arange(0, 16, 2, dtype=np.float32) / np.float32(16))).astype(np.float32)
    c["invfreq"] = np.broadcast_to(inv[None, :], (128, 8)).copy()
    return c


CONST_SHAPES = {
    "ident": [128, 128], "triL": [128, 128], "triU": [128, 128], "triS": [128, 128],
    "triA": [128, 256], "triB": [128, 256], "negA": [128, 256], "negB": [128, 256],
    "ones": [128, 128], "iota_row": [128, 128], "iota_col": [128, 1], "qhalf": [128, 1],
    "ebig": [128, 8192], "cover": [128, 4, 128], "cmpmask": [128, 4, 128], "invfreq": [128, 8],
}


def prepare_inputs(inp, SEQ, B):
    NCH = SEQ // 256
    NOWN = NCH // 4
    NPOS = SEQ + 768
    NT = NPOS // 128
    consts = make_consts()
    x = np.asarray(inp["x"], np.float32)
    positions = np.asarray(inp["positions"], np.int32)
    g = lambda n: np.asarray(inp[n])[0]
    rep = lambda v: np.broadcast_to(np.asarray(v, np.float32).reshape(1, -1), (128, np.asarray(v).size)).copy()
    shared = {}
    shared["w_in"] = np.ascontiguousarray(g("w_in"), np.float32)
    shared["cmp_k_w1"] = np.ascontiguousarray(g("cmp_k_w1"))
    shared["cmp_k_w2"] = np.ascontiguousarray(g("cmp_k_w2"))
    shared["cmp_k_peT"] = np.ascontiguousarray(g("cmp_k_pe").T)
    shared["cmp_k_b1"] = np.ascontiguousarray(g("cmp_k_b1").reshape(2, 128).T)
    shared["cmp_v_w1"] = np.ascontiguousarray(g("cmp_v_w1"))
    shared["cmp_v_w2"] = np.ascontiguousarray(g("cmp_v_w2"))
    shared["cmp_v_peT"] = np.ascontiguousarray(g("cmp_v_pe").T)
    shared["cmp_v_b1"] = np.ascontiguousarray(g("cmp_v_b1").reshape(2, 128).T)
    cw = g("conv_w").reshape(4, 2048)
    shared["conv_w"] = np.ascontiguousarray(cw.T.reshape(16, 128, 4).transpose(1, 0, 2))
    shared["conv_b"] = np.ascontiguousarray(g("conv_b").reshape(16, 128).T)
    shared["dt_bias"] = rep(g("dt_bias"))
    shared["a_log"] = rep(g("a_log"))
    shared["d_skip"] = rep(g("d_skip"))
    shared["ssm_norm_w"] = rep(g("ssm_norm_w"))
    shared["w_out"] = np.ascontiguousarray(g("w_out"))
    shared["ln1_g"] = rep(g("ln1_g"))
    shared["ln1_b"] = rep(g("ln1_b"))
    shared["ln2_g"] = rep(g("ln2_g"))
    shared["ln2_b"] = rep(g("ln2_b"))
    shared["w_router"] = np.ascontiguousarray(np.concatenate([g("w_router_group"), g("w_router_expert")], axis=1))
    shared["b_router"] = rep(np.concatenate([g("b_router_group"), g("b_router_expert")]))
    shared["w_gate"] = np.ascontiguousarray(g("w_gate"))
    shared["w_up"] = np.ascontiguousarray(g("w_up"))
    shared["w_down"] = np.ascontiguousarray(g("w_down"))
    for k_, v_ in consts.items():
        shared["c_" + k_] = v_
    maps = []
    for b in range(B):
        xTb = np.ascontiguousarray(x[b].T)
        for j in range(4):
            m = dict(shared)
            off = 768 - 256 * j
            xT = np.zeros((D, NPOS), np.float32)
            xT[:, off:off + SEQ] = xTb
            m["xT"] = xT
            own = np.concatenate([x[b, 256 * (4 * i + j):256 * (4 * i + j + 1)] for i in range(NOWN)], axis=0)
            m["x_own"] = np.ascontiguousarray(own)
            posb = np.zeros((NPOS,), np.int32)
            posb[off:off + SEQ] = positions[b]
            m["pos_tm"] = np.ascontiguousarray(posb.reshape(NT, 128).T)
            NCT = NOWN // 2
            cidx = 16 * np.arange(128 * NCT) + 31
            m["cpos"] = np.ascontiguousarray(posb[cidx].reshape(NCT, 128).T)
            kval = np.zeros((NPOS,), np.float32)
            kval[off:off + SEQ] = 1.0
            m["kvalid"] = np.ascontiguousarray(kval.reshape(NT, 128).T)
            cst = 16 * np.arange(128 * NCT)
            cval = ((cst >= off) & (cst + 32 <= off + SEQ)).astype(np.float32)
            m["cvalid"] = np.ascontiguousarray(cval.reshape(NCT, 128).T)
            f0 = np.zeros((128, 128), np.float32)
            f0[:, 12 - 4 * j] = 1.0
            m["forced0"] = f0
            maps.append(m)
    return maps


def build(SEQ, stop="all", E_N=32, debug=False):
    NCH = SEQ // 256
    NOWN = NCH // 4
    NBC = NCH + 3
    NPOS = NBC * 256
    NT = NPOS // 128
    NOT = NOWN * 2
    NTOK = NOWN * 256
    nc = bass.Bass("TRN2", target_bir_lowering=False)

    def din(name, shape, dt=F32):
        return nc.dram_tensor(name, list(shape), dt, kind="ExternalInput").ap()

    def dscr(name, shape, dt):
        return nc.dram_tensor(name, list(shape), dt, kind="Internal").ap()

    xT = din("xT", [D, NPOS])
    x_own = din("x_own", [NTOK, D])
    pos_tm = din("pos_tm", [128, NT], I32)
    NCT = NOWN // 2
    cpos = din("cpos", [128, NCT], I32)
    kvalid_d = din("kvalid", [128, NT])
    cvalid_d = din("cvalid", [128, NCT])
    forced0_d = din("forced0", [128, 128])
    w_in = din("w_in", [D, 4928])
    cmpw = {}
    for kv in ("k", "v"):
        cmpw[kv] = dict(w1=din("cmp_%s_w1" % kv, [2048, 256]), w2=din("cmp_%s_w2" % kv, [256, 64]),
                        peT=din("cmp_%s_peT" % kv, [64, 32]), b1=din("cmp_%s_b1" % kv, [128, 2]))
    conv_w_d = din("conv_w", [128, 16, 4])
    conv_b_d = din("conv_b", [128, 16])
    dt_bias_d = din("dt_bias", [128, 16])
    a_log_d = din("a_log", [128, 16])
    d_skip_d = din("d_skip", [128, 16])
    normw_d = din("ssm_norm_w", [128, 1024])
    w_out_d = din("w_out", [2048, 2048])
    ln1g_d, ln1b_d = din("ln1_g", [128, 2048]), din("ln1_b", [128, 2048])
    ln2g_d, ln2b_d = din("ln2_g", [128, 2048]), din("ln2_b", [128, 2048])
    w_router_d = din("w_router", [2048, 36])
    b_router_d = din("b_router", [128, 36])
    w_gate_d = din("w_gate", [E_N, 2048, 512])
    w_up_d = din("w_up", [E_N, 2048, 512])
    w_down_d = din("w_down", [E_N, 512, 2048])
    cd = {k_: din("c_" + k_, shp) for k_, shp in CONST_SHAPES.items()}

    out_d = nc.dram_tensor("out", [NTOK, D], F32, kind="ExternalOutput").ap()
    mix_d = dscr("mix_scr", [NTOK, 2048], BF16)
    st_xs = dscr("st_xs", [NOT, 128, 1024], BF16)
    st_H = dscr("st_H", [NOWN, 128, 1024], BF16)
    st_BT = dscr("st_BT", [NOWN, 128, 4 * 256], BF16)
    st_sm = dscr("st_sm", [NOT, 128, 48], F32)
    h32_d = dscr("h32_scr", [NTOK, 2048], F32)
    hbf_d = dscr("hbf_scr", [NTOK, 2048], BF16)
    dbg = {}
    if debug:
        dbg["mix"] = nc.dram_tensor("dbg_mix", [NTOK, 2048], F32, kind="ExternalOutput").ap()
        dbg["h"] = nc.dram_tensor("dbg_h", [NTOK, 2048], F32, kind="ExternalOutput").ap()

    with ExitStack() as top:
        P = Prog(nc, top)
        kb = KB(nc, P)

        uid = [0]

        def sb(st, name, shape, dt=F32):
            uid[0] += 1
            return st.enter_context(nc.sbuf_tensor("%s_%d" % (name, uid[0]), list(shape), dt))

        def ps(st, name, shape, dt=F32):
            uid[0] += 1
            return st.enter_context(nc.psum_tensor("%s_%d" % (name, uid[0]), list(shape), dt))

        stage_ctr = [0]

        def load_cast(st_tiles, dst, src, nelem_shape, res_dst, eng_cast="auto", pr=(0, 128)):
            i = stage_ctr[0] % len(st_tiles)
            stage_ctr[0] += 1
            stg = st_tiles[i]
            n = 1
            for s_ in nelem_shape[1:]:
                n *= s_
            view = stg[pr[0]:pr[1], 0:n]
            if len(nelem_shape) == 3:
                view = view.rearrange("p (a b) -> p a b", a=nelem_shape[1])
            kb.dma(view, src, r=(), w=[("stage", i)])
            if eng_cast == "auto":
                eng_cast = ("scalar", "gpsimd")[stage_ctr[0] % 2]
            if eng_cast == "scalar":
                kb.act(dst, view, AF.Copy, r=[("stage", i)], w=res_dst)
            else:
                kb.e(eng_cast, "tensor_copy", out=dst, in_=view, r=[("stage", i)], w=res_dst)

        def load_consts(st, names, bf=()):
            t = {}
            for n_ in names:
                shp = CONST_SHAPES[n_]
                t[n_] = sb(st, "c_" + n_, shp)
                kb.dma(t[n_][:], cd[n_], w=["c_" + n_])
            for n_ in bf:
                shp = CONST_SHAPES[n_]
                t[n_ + "_bf"] = sb(st, "cb_" + n_, shp, BF16)
                kb.e("vector", "tensor_copy", out=t[n_ + "_bf"][:], in_=t[n_][:], r=["c_" + n_], w=["cb_" + n_])
            return t

        def phase_S1():
            with ExitStack() as st:
                C = load_consts(st, ["ident", "triL", "ones"], bf=["ident"])
                wS = sb(st, "wS", [128, 16, 1552], BF16)
                stg = [sb(st, "stgA", [128, 4096]), sb(st, "stgB", [128, 4096])]
                xTc = [sb(st, "xTc0", [128, 16, 256], BF16), sb(st, "xTc1", [128, 16, 256], BF16)]
                raw = [sb(st, "raw0", [128, 12, 259]), sb(st, "raw1", [128, 12, 259])]
                cacc_l = [sb(st, "cacc0", [128, 12, 256]), sb(st, "cacc1", [128, 12, 256])]
                xbT_l = [sb(st, "xbT0", [128, 12, 256], BF16), sb(st, "xbT1", [128, 12, 256], BF16)]
                cw = sb(st, "cw", [128, 16, 4])
                cb_ = sb(st, "cb", [128, 16])
                dtb = sb(st, "dtb", [128, 16])
                aneg = sb(st, "aneg", [128, 16])
                kval = sb(st, "kval", [128, NT])
                H = sb(st, "H", [128, 1024])
                Hbf = sb(st, "Hbf", [128, 1024], BF16)
                sm_l = [sb(st, "sm0", [128, 2, 48]), sb(st, "sm1", [128, 2, 48])]
                tmp16_l = [sb(st, "tmp160", [128, 2, 16]), sb(st, "tmp161", [128, 2, 16])]
                wst_l = [sb(st, "wst0", [128, 2, 16]), sb(st, "wst1", [128, 2, 16])]
                dec_l = [sb(st, "dec0", [128, 16]), sb(st, "dec1", [128, 16])]
                xs_tok_l = [sb(st, "xs_tok0", [128, 2, 1024], BF16), sb(st, "xs_tok1", [128, 2, 1024], BF16)]
                xw_l = [sb(st, "xw0", [128, 2, 1024], BF16), sb(st, "xw1", [128, 2, 1024], BF16)]
                B_tok_l = [sb(st, "B_tok0", [128, 2, 512], BF16), sb(st, "B_tok1", [128, 2, 512], BF16)]
                p_fm = [ps(st, "p_fm0", [128, 512]), ps(st, "p_fm1", [128, 512])]
                p_dt2 = [ps(st, "p_dt0", [128, 512]), ps(st, "p_dt1", [128, 512])]
                p_trx = ps(st, "p_trx", [128, 1024], BF16)
                p_trb = ps(st, "p_trb", [128, 1024], BF16)
                p_st = ps(st, "p_st", [128, 1024])

                kb.dma(cw[:], conv_w_d, w=["cw"])
                kb.dma(cb_[:], conv_b_d, w=["cb"])
                kb.dma(dtb[:], dt_bias_d, w=["dtb"])
                kb.dma(aneg[:], a_log_d, w=["aneg"])
                kb.dma(kval[:], kvalid_d, w=["kval"])
                kb.act(aneg[:], aneg[:], AF.Exp, r=["aneg"], w=["aneg"])
                kb.e("vector", "tensor_scalar", out=aneg[:], in0=aneg[:], scalar1=-1.0, scalar2=None, op0=ALU.mult,
                     r=["aneg"], w=["aneg"])
                kb.e("vector", "memset", H[:], 0.0, w=["H"])
                kb.e("vector", "memset", raw[0][:, :, 0:3], 0.0, w=[("raw", 0)])
                for k2 in range(0, 16, 2):
                    src = w_in[k2 * 128:(k2 + 2) * 128, C_XS:C_XS + 1536].rearrange("(k p) n -> p k n", p=128)
                    load_cast(stg, wS[:, k2:k2 + 2, 0:1536], src, [128, 2, 1536], ["wS"])
                srcd = w_in[:, C_DT:C_DT + 16].rearrange("(k p) n -> p k n", p=128)
                load_cast(stg, wS[:, :, 1536:1552], srcd, [128, 16, 16], ["wS"])

                def stage_A(c):
                    xb = xTc[c % 2]
                    rw = raw[c % 2]
                    rwn = raw[(c + 1) % 2]
                    rxb = ("xTc", c % 2)
                    cacc, tmp16, wst, dec, sm = cacc_l[c % 2], tmp16_l[c % 2], wst_l[c % 2], dec_l[c % 2], sm_l[c % 2]
                    xbT, xs_tok, xw, B_tok = xbT_l[c % 2], xs_tok_l[c % 2], xw_l[c % 2], B_tok_l[c % 2]
                    Rn = lambda n_: (n_, c % 2)
                    p_dt = p_dt2[c % 2]
                    for k4 in range(0, 16, 4):
                        src = xT[k4 * 128:(k4 + 4) * 128, c * 256:(c + 1) * 256].rearrange("(k p) n -> p k n", p=128)
                        load_cast(stg, xb[:, k4:k4 + 4, :], src, [128, 4, 256], [rxb], eng_cast="scalar")
                    for m in range(12):
                        pf = p_fm[m % 2]
                        for k in range(16):
                            kb.mm(pf[:, 0:256], wS[:, k, m * 128:(m + 1) * 128], xb[:, k, :], k == 0, k == 15,
                                  r=["wS", rxb], w=[("p_fm", m % 2)])
                        kb.act(rw[:, m, 3:259], pf[:, 0:256], AF.Copy, r=[("p_fm", m % 2)], w=[("raw", c % 2)])
                    for t in range(2):
                        for k in range(16):
                            kb.mm(p_dt[:, t * 16:(t + 1) * 16], xb[:, k, t * 128:(t + 1) * 128], wS[:, k, 1536:1552],
                                  k == 0, k == 15, r=["wS", rxb], w=[("p_dt", c % 2)])
                def stage_B(c):
                    xb = xTc[c % 2]
                    rw = raw[c % 2]
                    rwn = raw[(c + 1) % 2]
                    rxb = ("xTc", c % 2)
                    cacc, tmp16, wst, dec, sm = cacc_l[c % 2], tmp16_l[c % 2], wst_l[c % 2], dec_l[c % 2], sm_l[c % 2]
                    xbT, xs_tok, xw, B_tok = xbT_l[c % 2], xs_tok_l[c % 2], xw_l[c % 2], B_tok_l[c % 2]
                    Rn = lambda n_: (n_, c % 2)
                    p_dt = p_dt2[c % 2]
                    kb.e("gpsimd", "tensor_copy", out=rwn[:, :, 0:3], in_=rw[:, :, 256:259],
                         r=[("raw", c % 2)], w=[("raw", (c + 1) % 2)])
                    for m in range(12):
                        kb.e("vector", "tensor_scalar", out=cacc[:, m, :], in0=rw[:, m, 0:256], scalar1=cw[:, m, 0:1],
                             scalar2=cb_[:, m:m + 1], op0=ALU.mult, op1=ALU.add, r=[("raw", c % 2), "cw", "cb"], w=[("cacc", c % 2, m)])
                    for tp in range(1, 4):
                        for m in range(12):
                            kb.e("vector", "scalar_tensor_tensor", out=cacc[:, m, :], in0=rw[:, m, tp:tp + 256],
                                 scalar=cw[:, m, tp:tp + 1], in1=cacc[:, m, :], op0=ALU.mult, op1=ALU.add,
                                 r=[("raw", c % 2), "cw", ("cacc", c % 2, m)], w=[("cacc", c % 2, m)])
                    kb.act(xbT[:], cacc[:], AF.Silu, r=[("cacc", c % 2, m) for m in range(12)], w=[Rn("xbT")])
                    pdt = p_dt[:, 0:32].rearrange("p (t h) -> p t h", t=2)
                    kb.e("vector", "tensor_tensor", out=tmp16[:], in0=pdt, in1=bc(dtb[:].unsqueeze(1), [128, 2, 16]),
                         op=ALU.add, r=[("p_dt", c % 2), "dtb"], w=[Rn("tmp16")])
                    kb.act(tmp16[:], tmp16[:], AF.Exp, r=[Rn("tmp16")], w=[Rn("tmp16")])
                    kb.act(tmp16[:], tmp16[:], AF.Ln, bias=1.0, r=[Rn("tmp16")], w=[Rn("tmp16")])
                    kb.e("vector", "tensor_tensor", out=sm[:, :, 0:16], in0=tmp16[:],
                         in1=bc(kval[:, 2 * c:2 * c + 2].unsqueeze(2), [128, 2, 16]), op=ALU.mult,
                         r=[Rn("tmp16"), "kval"], w=[Rn("sm")])
                    kb.e("vector", "tensor_tensor", out=sm[:, :, 32:48], in0=sm[:, :, 0:16],
                         in1=bc(aneg[:].unsqueeze(1), [128, 2, 16]), op=ALU.mult, r=[Rn("sm"), "aneg"], w=[Rn("sm")])
                    kb.mm(p_dt[:, 64:80], C["triL"][:], sm[:, 0, 32:48], True, True, r=[Rn("sm"), "c_triL"], w=[("p_dt", c % 2)])
                    kb.mm(p_dt[:, 80:96], C["ones"][:], sm[:, 0, 32:48], True, False, r=[Rn("sm"), "c_ones"], w=[("p_dt", c % 2)])
                    kb.mm(p_dt[:, 80:96], C["triL"][:], sm[:, 1, 32:48], False, True, r=[Rn("sm"), "c_triL"], w=[("p_dt", c % 2)])
                    kb.mm(p_dt[:, 128:144], C["ones"][:], sm[:, 0, 32:48], True, False, r=[Rn("sm"), "c_ones"], w=[("p_dt", c % 2)])
                    kb.mm(p_dt[:, 128:144], C["ones"][:], sm[:, 1, 32:48], False, True, r=[Rn("sm"), "c_ones"], w=[("p_dt", c % 2)])
                    pacs = p_dt[:, 64:96].rearrange("p (t h) -> p t h", t=2)
                    kb.act(sm[:, :, 16:32], pacs, AF.Copy, r=[("p_dt", c % 2)], w=[Rn("sm")])
                    kb.e("vector", "tensor_tensor", out=wst[:], in0=bc(p_dt[:, 128:144].unsqueeze(1), [128, 2, 16]),
                         in1=sm[:, :, 16:32], op=ALU.subtract, r=[("p_dt", c % 2), Rn("sm")], w=[Rn("wst")])
                    kb.act(wst[:], wst[:], AF.Exp, r=[Rn("wst")], w=[Rn("wst")])
                    kb.e("vector", "tensor_tensor", out=wst[:], in0=wst[:], in1=sm[:, :, 0:16], op=ALU.mult,
                         r=[Rn("wst"), Rn("sm")], w=[Rn("wst")])
                    kb.act(dec[:], p_dt[:, 128:144], AF.Exp, r=[("p_dt", c % 2)], w=[Rn("dec")])
                    for t in range(2):
                        for m in range(8):
                            kb.tr(p_trx[:, m * 128:(m + 1) * 128], xbT[:, m, t * 128:(t + 1) * 128], C["ident_bf"][:],
                                  r=[Rn("xbT"), "cb_ident"], w=["p_trx"])
                        kb.act(xs_tok[:, t, :], p_trx[:], AF.Copy, r=["p_trx"], w=[Rn("xs_tok")])
                        kb.e("vector", "tensor_tensor", out=xw[:, t, :].rearrange("p (h d) -> p h d", h=16),
                             in0=p_trx[:].rearrange("p (h d) -> p h d", h=16),
                             in1=bc(wst[:, t, :].unsqueeze(2), [128, 16, 64]), op=ALU.mult,
                             r=["p_trx", Rn("wst")], w=[Rn("xw")])
                        for g_ in range(4):
                            kb.tr(p_trb[:, g_ * 128:(g_ + 1) * 128], xbT[:, 8 + g_, t * 128:(t + 1) * 128], C["ident_bf"][:],
                                  r=[Rn("xbT"), "cb_ident"], w=["p_trb"])
                        kb.act(B_tok[:, t, :], p_trb[:, 0:512], AF.Copy, r=["p_trb"], w=[Rn("B_tok")])
                    for g_ in range(4):
                        for t in range(2):
                            kb.mm(p_st[:, g_ * 256:(g_ + 1) * 256], B_tok[:, t, g_ * 128:(g_ + 1) * 128],
                                  xw[:, t, g_ * 256:(g_ + 1) * 256], (t == 0) and (g_ % 2 == 0), t == 1,
                                  r=[Rn("B_tok"), Rn("xw")], w=["p_st"])
                    if c >= 3 and (c - 3) % 4 == 0:
                        i = (c - 3) // 4
                        kb.e("gpsimd", "tensor_copy", out=Hbf[:], in_=H[:], r=["H"], w=["Hbf"])
                        kb.dma(st_H[i], Hbf[:], r=["Hbf"], w=["st_H"])
                        kb.dma(st_BT[i], xbT[:, 8:12, :].rearrange("p a b -> p (a b)"), r=[Rn("xbT")], w=["st_BT"])
                        for t in range(2):
                            kb.dma(st_xs[2 * i + t], xs_tok[:, t, :], r=[Rn("xs_tok")], w=["st_xs"])
                            kb.dma(st_sm[2 * i + t], sm[:, t, :], r=[Rn("sm")], w=["st_sm"])
                    kb.e("vector", "tensor_tensor", out=H[:].rearrange("p (h d) -> p h d", h=16),
                         in0=H[:].rearrange("p (h d) -> p h d", h=16), in1=bc(dec[:].unsqueeze(2), [128, 16, 64]),
                         op=ALU.mult, r=["H", Rn("dec")], w=["H"])
                    kb.e("vector", "tensor_tensor", out=H[:], in0=H[:], in1=p_st[:], op=ALU.add, r=["H", "p_st"], w=["H"])
                stage_A(0)
                for c in range(NBC):
                    if c + 1 < NBC:
                        stage_A(c + 1)
                    stage_B(c)
                P.end_phase()

        def phase_S2():
            with ExitStack() as st:
                C = load_consts(st, ["ident", "triA", "triB", "negA", "negB", "ones"], bf=["ident"])
                wZC = sb(st, "wZC", [128, 16, 1536], BF16)
                stg = [sb(st, "stgA", [128, 4096]), sb(st, "stgB", [128, 4096])]
                xTo = sb(st, "xTo", [128, 16, 260], BF16)
                cw = sb(st, "cw", [128, 4, 4])
                cb_ = sb(st, "cb", [128, 4])
                dsk = sb(st, "dsk", [128, 16])
                normw = sb(st, "normw", [128, 1024])
                rawC = sb(st, "rawC", [128, 4, 259])
                caccC = sb(st, "caccC", [128, 4, 256])
                CT = sb(st, "CT", [128, 4, 256], BF16)
                BT = sb(st, "BT", [128, 4, 256], BF16)
                Hbf = sb(st, "Hbf", [128, 1024], BF16)
                xs_tok = sb(st, "xs_tok", [128, 2, 1024], BF16)
                sm = sb(st, "sm", [128, 2, 48])
                nacs = sb(st, "nacs", [128, 2, 16])
                ea = sb(st, "ea", [128, 2, 16])
                cbT = sb(st, "cbT", [128, 4, 2, 256])
                drep = [sb(st, "drep0", [128, 2, 128]), sb(st, "drep1", [128, 2, 128])]
                dtmp = [sb(st, "dtmp%d" % q_, [128, 256]) for q_ in range(4)]
                WT = [sb(st, "WT%d" % q_, [128, 256], BF16) for q_ in range(4)]
                sz = sb(st, "sz", [128, 1024])
                yv = sb(st, "yv", [128, 1024])
                y2 = sb(st, "y2", [128, 1024])
                ss = sb(st, "ss", [128, 4])
                ybf = sb(st, "ybf", [128, 1024], BF16)
                p_y = ps(st, "p_y", [128, 2048])
                p_w = ps(st, "p_w", [128, 1024])
                p_a = [ps(st, "p_a0", [128, 512]), ps(st, "p_a1", [128, 512])]

                kb.dma(cw[:], conv_w_d[:, 12:16, :], w=["cw"])
                kb.dma(cb_[:], conv_b_d[:, 12:16], w=["cb"])
                kb.dma(dsk[:], d_skip_d, w=["dsk"])
                kb.dma(normw[:], normw_d, w=["normw"])
                for k2 in range(0, 16, 2):
                    src = w_in[k2 * 128:(k2 + 2) * 128, C_Z:C_Z + 1024].rearrange("(k p) n -> p k n", p=128)
                    load_cast(stg, wZC[:, k2:k2 + 2, 0:1024], src, [128, 2, 1024], ["wZC"])
                for k4 in range(0, 16, 4):
                    src = w_in[k4 * 128:(k4 + 4) * 128, C_C:C_C + 512].rearrange("(k p) n -> p k n", p=128)
                    load_cast(stg, wZC[:, k4:k4 + 4, 1024:1536], src, [128, 4, 512], ["wZC"])

                for i in range(NOWN):
                    p0 = (3 + 4 * i) * 256
                    for k4 in range(0, 16, 4):
                        src = xT[k4 * 128:(k4 + 4) * 128, p0 - 4:p0 + 256].rearrange("(k p) n -> p k n", p=128)
                        load_cast(stg, xTo[:, k4:k4 + 4, :], src, [128, 4, 260], ["xTo"], eng_cast="scalar")
                    kb.dma(BT[:].rearrange("p a b -> p (a b)"), st_BT[i], w=["BT"])
                    kb.dma(Hbf[:], st_H[i], w=["Hbf"])
                    for t in range(2):
                        kb.dma(xs_tok[:, t, :], st_xs[2 * i + t], w=["xs_tok"])
                        kb.dma(sm[:, t, :], st_sm[2 * i + t], w=["sm"])
                    kb.e("vector", "tensor_scalar", out=nacs[:], in0=sm[:, :, 16:32], scalar1=-1.0, scalar2=None,
                         op0=ALU.mult, r=["sm"], w=["nacs"])
                    kb.act(ea[:], sm[:, :, 16:32], AF.Exp, r=["sm"], w=["ea"])
                    for g_ in range(4):
                        for k in range(16):
                            kb.mm(p_w[:, 0:259], wZC[:, k, 1024 + g_ * 128:1024 + (g_ + 1) * 128], xTo[:, k, 1:260],
                                  k == 0, k == 15, r=["wZC", "xTo"], w=["p_w"])
                        kb.act(rawC[:, g_, :], p_w[:, 0:259], AF.Copy, r=["p_w"], w=["rawC"])
                        kb.e("vector", "tensor_scalar", out=caccC[:, g_, :], in0=rawC[:, g_, 0:256], scalar1=cw[:, g_, 0:1],
                             scalar2=cb_[:, g_:g_ + 1], op0=ALU.mult, op1=ALU.add, r=["rawC", "cw", "cb"], w=["caccC"])
                        for tp in range(1, 4):
                            kb.e("vector", "scalar_tensor_tensor", out=caccC[:, g_, :], in0=rawC[:, g_, tp:tp + 256],
                                 scalar=cw[:, g_, tp:tp + 1], in1=caccC[:, g_, :], op0=ALU.mult, op1=ALU.add,
                                 r=["rawC", "cw", "caccC"], w=["caccC"])
                    kb.act(CT[:], caccC[:], AF.Silu, r=["caccC"], w=["CT"])
                    for g_ in range(4):
                        for s_ in range(2):
                            kb.mm(p_w[:, 512:768], BT[:, g_, s_ * 128:(s_ + 1) * 128], CT[:, g_, :], True, True,
                                  r=["BT", "CT"], w=["p_w"])
                            kb.act(cbT[:, g_, s_, :], p_w[:, 512:768], AF.Copy, r=["p_w"], w=["cbT"])
                    for h in range(16):
                        g_ = h // 4
                        dr = drep[h % 2]
                        pa = p_a[h % 2]
                        for s_ in range(2):
                            kb.act(dr[:, s_, :], C["ones"][:], AF.Identity, scale=sm[:, s_, 32 + h:33 + h],
                                   r=["sm", "c_ones"], w=[("drep", h % 2)])
                        kb.mm(pa[:, 0:256], dr[:, 0, :], C["triA"][:], True, False, r=[("drep", h % 2), "c_triA"], w=[("p_a", h % 2)])
                        kb.mm(pa[:, 0:256], dr[:, 1, :], C["triB"][:], False, True, r=[("drep", h % 2), "c_triB"], w=[("p_a", h % 2)])
                        for s_ in range(2):
                            dtm = dtmp[2 * (h % 2) + s_]
                            wt = WT[2 * (h % 2) + s_]
                            kb.e("vector", "tensor_tensor", out=dtm[:], in0=pa[:, 0:256], in1=C["negA" if s_ == 0 else "negB"][:],
                                 op=ALU.add, r=[("p_a", h % 2), "c_negA", "c_negB"], w=[("dtmp", 2 * (h % 2) + s_)])
                            kb.act(dtm[:], dtm[:], AF.Exp, bias=nacs[:, s_, h:h + 1], r=[("dtmp", 2 * (h % 2) + s_), "nacs"], w=[("dtmp", 2 * (h % 2) + s_)])
                            kb.e("vector", "scalar_tensor_tensor", out=wt[:], in0=dtm[:], scalar=sm[:, s_, h:h + 1],
                                 in1=cbT[:, g_, s_, :], op0=ALU.mult, op1=ALU.mult, r=[("dtmp", 2 * (h % 2) + s_), "sm", "cbT"], w=[("WT", 2 * (h % 2) + s_)])
                        first0 = (h % 8 == 0)
                        kb.mm(p_y[:, h * 64:(h + 1) * 64], WT[2 * (h % 2)][:, 0:128], xs_tok[:, 0, h * 64:(h + 1) * 64], first0, True,
                              r=[("WT", 2 * (h % 2)), "xs_tok"], w=["p_y"])
                        kb.mm(p_y[:, 1024 + h * 64:1024 + (h + 1) * 64], WT[2 * (h % 2)][:, 128:256], xs_tok[:, 0, h * 64:(h + 1) * 64],
                              first0, False, r=[("WT", 2 * (h % 2)), "xs_tok"], w=["p_y"])
                        kb.mm(p_y[:, 1024 + h * 64:1024 + (h + 1) * 64], WT[2 * (h % 2) + 1][:, 128:256], xs_tok[:, 1, h * 64:(h + 1) * 64],
                              False, True, r=[("WT", 2 * (h % 2) + 1), "xs_tok"], w=["p_y"])
                    for lt in range(2):
                        for cc in range(2):
                            for k in range(16):
                                kb.mm(p_w[:, cc * 512:(cc + 1) * 512], xTo[:, k, 4 + lt * 128:4 + (lt + 1) * 128],
                                      wZC[:, k, cc * 512:(cc + 1) * 512], k == 0, k == 15, r=["wZC", "xTo"], w=["p_w"])
                        kb.act(sz[:], p_w[:], AF.Silu, r=["p_w"], w=["sz"])
                        for g_ in range(4):
                            kb.mm(p_w[:, g_ * 256:(g_ + 1) * 256], CT[:, g_, lt * 128:(lt + 1) * 128], Hbf[:, g_ * 256:(g_ + 1) * 256],
                                  g_ % 2 == 0, True, r=["CT", "Hbf", "sz"], w=["p_w"])
                        v3 = lambda ap: ap.rearrange("p (h d) -> p h d", h=16)
                        kb.e("vector", "tensor_tensor", out=v3(yv[:]), in0=v3(p_w[:]), in1=bc(ea[:, lt, :].unsqueeze(2), [128, 16, 64]),
                             op=ALU.mult, r=["p_w", "ea"], w=["yv"])
                        kb.e("vector", "tensor_tensor", out=yv[:], in0=yv[:], in1=p_y[:, lt * 1024:(lt + 1) * 1024], op=ALU.add,
                             r=["yv", "p_y"], w=["yv"])
                        kb.e("vector", "tensor_tensor", out=v3(y2[:]), in0=v3(xs_tok[:, lt, :]), in1=bc(dsk[:].unsqueeze(2), [128, 16, 64]),
                             op=ALU.mult, r=["xs_tok", "dsk"], w=["y2"])
                        kb.e("vector", "tensor_tensor", out=yv[:], in0=yv[:], in1=y2[:], op=ALU.add, r=["yv", "y2"], w=["yv"])
                        kb.e("vector", "tensor_tensor", out=yv[:], in0=yv[:], in1=sz[:], op=ALU.mult, r=["yv", "sz"], w=["yv"])
                        kb.e("vector", "tensor_tensor", out=y2[:], in0=yv[:], in1=yv[:], op=ALU.mult, r=["yv"], w=["y2"])
                        kb.e("vector", "tensor_reduce", out=ss[:], in_=y2[:].rearrange("p (g d) -> p g d", g=4), axis=AX.X,
                             op=ALU.add, r=["y2"], w=["ss"])
                        kb.e("vector", "tensor_scalar", out=ss[:], in0=ss[:], scalar1=1.0 / 256.0, scalar2=EPS, op0=ALU.mult,
                             op1=ALU.add, r=["ss"], w=["ss"])
                        kb.act(ss[:], ss[:], AF.Sqrt, r=["ss"], w=["ss"])
                        kb.e("vector", "reciprocal", out=ss[:], in_=ss[:], r=["ss"], w=["ss"])
                        kb.e("vector", "tensor_tensor", out=yv[:].rearrange("p (g d) -> p g d", g=4),
                             in0=yv[:].rearrange("p (g d) -> p g d", g=4), in1=bc(ss[:].unsqueeze(2), [128, 4, 256]),
                             op=ALU.mult, r=["yv", "ss"], w=["yv"])
                        kb.e("vector", "tensor_tensor", out=ybf[:], in0=yv[:], in1=normw[:], op=ALU.mult, r=["yv", "normw"], w=["ybf"])
                        row0 = (2 * i + lt) * 128
                        kb.dma(mix_d[row0:row0 + 128, 1024:2048], ybf[:], r=["ybf"], w=["mix_d"])
                P.end_phase()


        TWO_PI = 6.283185307179586
        C1 = 6.28125
        C2 = TWO_PI - C1

        def rope_tables(st, pos_i, n, sin_t, cos_t, invf, tag):
            posf = sb(st, "posf" + tag, [128, n])
            ang = sb(st, "ang" + tag, [128, n, 8])
            kf = sb(st, "kf" + tag, [128, n, 8])
            ki = sb(st, "ki" + tag, [128, n, 8], I32)
            rr = sb(st, "rr" + tag, [128, n, 8])
            m1 = sb(st, "m1" + tag, [128, n, 8])
            R = ["rope" + tag]
            kb.e("vector", "tensor_copy", out=posf[:], in_=pos_i, r=R, w=R)
            kb.e("vector", "tensor_tensor", out=ang[:], in0=bc(posf[:].unsqueeze(2), [128, n, 8]),
                 in1=bc(invf.unsqueeze(1), [128, n, 8]), op=ALU.mult, r=R + ["c_invfreq"], w=R)
            kb.e("vector", "tensor_scalar", out=kf[:], in0=ang[:], scalar1=1.0 / TWO_PI, scalar2=None, op0=ALU.mult, r=R, w=R)
            kb.e("vector", "tensor_copy", out=ki[:], in_=kf[:], r=R, w=R)
            kb.e("vector", "tensor_copy", out=kf[:], in_=ki[:], r=R, w=R)
            kb.e("vector", "scalar_tensor_tensor", out=rr[:], in0=kf[:], scalar=-C1, in1=ang[:], op0=ALU.mult, op1=ALU.add, r=R, w=R)
            kb.e("vector", "scalar_tensor_tensor", out=rr[:], in0=kf[:], scalar=-C2, in1=rr[:], op0=ALU.mult, op1=ALU.add, r=R, w=R)
            kb.e("vector", "tensor_scalar", out=m1[:], in0=rr[:], scalar1=float(np.pi), scalar2=None, op0=ALU.is_gt, r=R, w=R)
            kb.e("vector", "scalar_tensor_tensor", out=rr[:], in0=m1[:], scalar=-TWO_PI, in1=rr[:], op0=ALU.mult, op1=ALU.add, r=R, w=R)
            kb.e("vector", "tensor_scalar", out=m1[:], in0=rr[:], scalar1=-float(np.pi), scalar2=None, op0=ALU.is_lt, r=R, w=R)
            kb.e("vector", "scalar_tensor_tensor", out=rr[:], in0=m1[:], scalar=TWO_PI, in1=rr[:], op0=ALU.mult, op1=ALU.add, r=R, w=R)
            kb.e("vector", "tensor_scalar", out=rr[:], in0=rr[:], scalar1=float(np.pi), scalar2=-float(np.pi), op0=ALU.min, op1=ALU.max, r=R, w=R)
            kb.act(sin_t, rr[:], AF.Sin, r=R, w=R + ["ropetab" + tag])
            kb.e("vector", "tensor_scalar", out=m1[:], in0=rr[:], scalar1=-1.0, scalar2=None, op0=ALU.mult, r=R, w=R)
            kb.e("vector", "tensor_tensor", out=m1[:], in0=m1[:], in1=rr[:], op=ALU.max, r=R, w=R)
            kb.e("vector", "tensor_scalar", out=m1[:], in0=m1[:], scalar1=-1.0, scalar2=float(np.pi / 2), op0=ALU.mult, op1=ALU.add, r=R, w=R)
            kb.act(cos_t, m1[:], AF.Sin, r=R, w=R + ["ropetab" + tag])

        def rope_apply(xin, out_view, cos_b, sin_b, tmps, shape, rin, rout, rtmp):
            x1 = xin[..., 0:8] if False else None

        def nsa_all():
            with ExitStack() as nsa:
                KselT = sb(nsa, "KselT", [128, NPOS], BF16)
                KwinT = sb(nsa, "KwinT", [128, NPOS], BF16)
                Vsel = sb(nsa, "Vsel", [128, NT, 2, 65], BF16)
                Vwin = sb(nsa, "Vwin", [128, NT, 2, 65], BF16)
                cosT = sb(nsa, "cosT", [128, NT, 8])
                sinT = sb(nsa, "sinT", [128, NT, 8])
                NCB = 128 * NCT
                kcT = sb(nsa, "kcT", [128, NCB], BF16)
                vc_ext = sb(nsa, "vc_ext", [128, NCT, 2, 193], BF16)
                with ExitStack() as cmpst:
                    KcT = sb(cmpst, "KcT", [128, NPOS], BF16)
                    VcT = sb(cmpst, "VcT", [128, NPOS], BF16)
                    with ExitStack() as st:
                        C = load_consts(st, ["ident", "invfreq"], bf=["ident"])
                        wK = sb(st, "wK", [128, 16, 768], BF16)
                        stg = [sb(st, "stgA", [128, 2048]), sb(st, "stgB", [128, 2048])]
                        xTc = [sb(st, "xTc0", [128, 16, 256], BF16), sb(st, "xTc1", [128, 16, 256], BF16)]
                        kval = sb(st, "kval", [128, NT])
                        posi = sb(st, "posi", [128, NT], I32)
                        ktok = sb(st, "ktok", [128, 2, 128], BF16)
                        tt = [sb(st, "ropet%d" % q_, [128, 2, 2, 8]) for q_ in range(4)]
                        p_fm = [ps(st, "p_fm0", [128, 512]), ps(st, "p_fm1", [128, 512])]
                        p_kv = [ps(st, "p_kv%d" % q_, [128, 512]) for q_ in range(4)]
                        p_tk = ps(st, "p_tk", [128, 1024], BF16)
                        kb.dma(kval[:], kvalid_d, w=["kval"])
                        kb.dma(posi[:], pos_tm, w=["ropeK"])
                        rope_tables(st, posi[:], NT, sinT[:], cosT[:], C["invfreq"][:], "K")
                        for k4 in range(0, 16, 2):
                            src = w_in[k4 * 128:(k4 + 2) * 128, C_KV:C_KV + 768].rearrange("(k p) n -> p k n", p=128)
                            load_cast(stg, wK[:, k4:k4 + 2, :], src, [128, 2, 768], ["wK"])
                        def stage_KA(c):
                            xb = xTc[c % 2]
                            rxb = ("xTc", c % 2)
                            for k4 in range(0, 16, 4):
                                src = xT[k4 * 128:(k4 + 4) * 128, c * 256:(c + 1) * 256].rearrange("(k p) n -> p k n", p=128)
                                load_cast(stg, xb[:, k4:k4 + 4, :], src, [128, 4, 256], [rxb], eng_cast="scalar")
                            for m in range(2):
                                pf = p_fm[m]
                                for k in range(16):
                                    kb.mm(pf[:, 0:256], wK[:, k, m * 128:(m + 1) * 128], xb[:, k, :], k == 0, k == 15,
                                          r=["wK", rxb], w=[("p_fm", m)])
                                dst = (KcT if m == 0 else VcT)
                                kb.act(dst[:, c * 256:(c + 1) * 256], pf[:, 0:256], AF.Copy, r=[("p_fm", m)], w=["KcT" if m == 0 else "VcT"])
                            for t in range(2):
                                T = 2 * c + t
                                pk = p_kv[2 * (c % 2) + t]
                                rpk = ("p_kv", 2 * (c % 2) + t)
                                for k in range(16):
                                    kb.mm(pk[:], xb[:, k, t * 128:(t + 1) * 128], wK[:, k, 256:768], k == 0, k == 15,
                                          r=["wK", rxb], w=[rpk])
                        def stage_KB(c):
                            for t in range(2):
                                T = 2 * c + t
                                pk = p_kv[2 * (c % 2) + t]
                                rpk = ("p_kv", 2 * (c % 2) + t)
                                pk4 = pk[:].rearrange("p (a g d) -> p a g d", a=4, g=2)
                                kt4 = ktok[:].rearrange("p a (g d) -> p a g d", g=2)
                                kb.act(kt4, pk4[:, 0:4:2, :, :], AF.Copy, r=[rpk], w=["ktok"])
                                x1 = pk4[:, 0:4:2, :, 0:8]
                                x2 = pk4[:, 0:4:2, :, 8:16]
                                cb4 = bc(cosT[:, T, :].unsqueeze(1).unsqueeze(1), [128, 2, 2, 8])
                                sb4 = bc(sinT[:, T, :].unsqueeze(1).unsqueeze(1), [128, 2, 2, 8])
                                RT = ["ropetabK"]
                                kb.e("vector", "tensor_tensor", out=tt[0][:], in0=x1, in1=cb4, op=ALU.mult, r=[rpk] + RT, w=["tt0"])
                                kb.e("vector", "tensor_tensor", out=tt[1][:], in0=x2, in1=sb4, op=ALU.mult, r=[rpk] + RT, w=["tt1"])
                                kb.e("vector", "tensor_tensor", out=tt[2][:], in0=x2, in1=cb4, op=ALU.mult, r=[rpk] + RT, w=["tt2"])
                                kb.e("vector", "tensor_tensor", out=tt[3][:], in0=x1, in1=sb4, op=ALU.mult, r=[rpk] + RT, w=["tt3"])
                                kb.e("vector", "tensor_tensor", out=kt4[:, :, :, 0:8], in0=tt[0][:], in1=tt[1][:], op=ALU.subtract,
                                     r=["tt0", "tt1"], w=["ktok"])
                                kb.e("vector", "tensor_tensor", out=kt4[:, :, :, 8:16], in0=tt[2][:], in1=tt[3][:], op=ALU.add,
                                     r=["tt2", "tt3"], w=["ktok"])
                                for s_ in range(2):
                                    kb.tr(p_tk[:, s_ * 128:(s_ + 1) * 128], ktok[:, s_, :], C["ident_bf"][:], r=["ktok", "cb_ident"], w=["p_tk"])
                                kb.act(KselT[:, T * 128:(T + 1) * 128], p_tk[:, 0:128], AF.Copy, r=["p_tk"], w=["KselT"])
                                kb.act(KwinT[:, T * 128:(T + 1) * 128], p_tk[:, 128:256], AF.Copy, r=["p_tk"], w=["KwinT"])
                                kb.e("vector", "tensor_copy", out=Vsel[:, T, :, 0:64], in_=pk4[:, 1, :, :], r=[rpk], w=["Vsel"])
                                kb.e("vector", "tensor_copy", out=Vwin[:, T, :, 0:64], in_=pk4[:, 3, :, :], r=[rpk], w=["Vwin"])
                                kvb = bc(kval[:, T:T + 1].unsqueeze(2), [128, 2, 1])
                                kb.e("vector", "tensor_copy", out=Vsel[:, T, :, 64:65], in_=kvb, r=["kval"], w=["Vsel"])
                                kb.e("vector", "tensor_copy", out=Vwin[:, T, :, 64:65], in_=kvb, r=["kval"], w=["Vwin"])
                        stage_KA(0)
                        for c in range(NBC):
                            if c + 1 < NBC:
                                stage_KA(c + 1)
                            stage_KB(c)
                        P.end_phase()
                    with ExitStack() as st:
                        C = load_consts(st, ["ident", "invfreq", "cover"], bf=["ident"])
                        stg = [sb(st, "stgA", [128, 4096]), sb(st, "stgB", [128, 4096])]
                        W1 = sb(st, "W1", [128, 32, 256], BF16)
                        peT = sb(st, "peT", [128, 32], BF16)
                        b1 = sb(st, "b1", [128, 2])
                        bias = sb(st, "bias", [128, 2])
                        w2 = sb(st, "w2", [128, 2, 64], BF16)
                        hidT = sb(st, "hidT", [128, 2, NCB], BF16)
                        gx = sb(st, "gx", [128, NCB])
                        gu = sb(st, "gu", [128, NCB])
                        cval = sb(st, "cval", [128, NCT])
                        cposi = sb(st, "cposi", [128, NCT], I32)
                        ccos = sb(st, "ccos", [128, NCT, 8])
                        csin = sb(st, "csin", [128, NCT, 8])
                        kctok = sb(st, "kctok", [128, NCT, 128], BF16)
                        tt = [sb(st, "ropec%d" % q_, [128, 8]) for q_ in range(4)]
                        p_h = [ps(st, "p_h0", [128, 512]), ps(st, "p_h1", [128, 512])]
                        p_b = ps(st, "p_b", [128, 512])
                        p_o = ps(st, "p_o", [128, 512])
                        p_t = ps(st, "p_t", [128, 1024], BF16)
                        kb.dma(cval[:], cvalid_d, w=["cval"])
                        kb.dma(cposi[:], cpos, w=["ropeC"])
                        rope_tables(st, cposi[:], NCT, csin[:], ccos[:], C["invfreq"][:], "C")
                        for kvn, Xc in (("k", KcT), ("v", VcT)):
                            cw_ = cmpw[kvn]
                            for half in range(2):
                                for jh in range(2):
                                    src = cw_["w1"].rearrange("(j d) h -> d j h", d=64)[:, jh * 16:(jh + 1) * 16, :]
                                    load_cast(stg, W1[half * 64:(half + 1) * 64, jh * 16:(jh + 1) * 16, :], src, [64, 16, 256], ["W1"],
                                              pr=(half * 64, (half + 1) * 64))
                                load_cast(stg, peT[half * 64:(half + 1) * 64, :], cw_["peT"], [64, 32], ["peT"], pr=(half * 64, (half + 1) * 64))
                            load_cast(stg, w2[:], cw_["w2"].rearrange("(c p) n -> p c n", p=128), [128, 2, 64], ["w2"])
                            kb.dma(b1[:], cw_["b1"], w=["b1"])
                            for hc in range(2):
                                for j in range(32):
                                    kb.mm(p_b[:, hc:hc + 1], W1[0:64, j, hc * 128:(hc + 1) * 128], peT[0:64, j:j + 1], j == 0, j == 31,
                                          r=["W1", "peT"], w=["p_b"])
                            kb.e("vector", "tensor_tensor", out=bias[:], in0=p_b[:, 0:2], in1=b1[:], op=ALU.add, r=["p_b", "b1"], w=["bias"])
                            for g_ in range(2):
                                for hc in range(2):
                                    for j in range(32):
                                        kb.mm(p_h[hc][:, 0:NCB], W1[g_ * 64:(g_ + 1) * 64, j, hc * 128:(hc + 1) * 128],
                                              Xc[g_ * 64:(g_ + 1) * 64, j:j + 16 * NCB:16], j == 0, j == 31,
                                              r=["W1", "KcT", "VcT"], w=[("p_h", hc)])
                                    kb.act(gx[:], p_h[hc][:, 0:NCB], AF.Identity, bias=bias[:, hc:hc + 1], r=[("p_h", hc), "bias"], w=["gx"])
                                    kb.e("vector", "tensor_tensor", out=gu[:], in0=gx[:], in1=gx[:], op=ALU.mult, r=["gx"], w=["gu"])
                                    kb.e("vector", "tensor_scalar", out=gu[:], in0=gu[:], scalar1=0.044715, scalar2=1.0, op0=ALU.mult, op1=ALU.add, r=["gu"], w=["gu"])
                                    kb.e("vector", "tensor_tensor", out=gu[:], in0=gu[:], in1=gx[:], op=ALU.mult, r=["gu", "gx"], w=["gu"])
                                    kb.act(gu[:], gu[:], AF.Tanh, scale=0.7978845608028654, r=["gu"], w=["gu"])
                                    kb.e("vector", "tensor_scalar", out=gu[:], in0=gu[:], scalar1=1.0, scalar2=0.5, op0=ALU.add, op1=ALU.mult, r=["gu"], w=["gu"])
                                    kb.e("vector", "tensor_tensor", out=hidT[:, hc, :], in0=gu[:], in1=gx[:], op=ALU.mult, r=["gu", "gx"], w=["hidT"])
                                for nt in range(NCT):
                                    for hc in range(2):
                                        kb.mm(p_o[:, 0:64], hidT[:, hc, nt * 128:(nt + 1) * 128], w2[:, hc, :], hc == 0, hc == 1,
                                              r=["hidT", "w2"], w=["p_o"])
                                    if kvn == "k":
                                        ko = kctok[:, nt, g_ * 64:(g_ + 1) * 64]
                                        kb.act(ko, p_o[:, 0:64], AF.Copy, r=["p_o"], w=["kctok"])
                                        x1 = p_o[:, 0:8]
                                        x2 = p_o[:, 8:16]
                                        cb_ = ccos[:, nt, :]
                                        sb_ = csin[:, nt, :]
                                        RT = ["ropetabC"]
                                        kb.e("vector", "tensor_tensor", out=tt[0][:], in0=x1, in1=cb_, op=ALU.mult, r=["p_o"] + RT, w=["tt0"])
                                        kb.e("vector", "tensor_tensor", out=tt[1][:], in0=x2, in1=sb_, op=ALU.mult, r=["p_o"] + RT, w=["tt1"])
                                        kb.e("vector", "tensor_tensor", out=tt[2][:], in0=x2, in1=cb_, op=ALU.mult, r=["p_o"] + RT, w=["tt2"])
                                        kb.e("vector", "tensor_tensor", out=tt[3][:], in0=x1, in1=sb_, op=ALU.mult, r=["p_o"] + RT, w=["tt3"])
                                        kb.e("vector", "tensor_tensor", out=ko[:, 0:8], in0=tt[0][:], in1=tt[1][:], op=ALU.subtract, r=["tt0", "tt1"], w=["kctok"])
                                        kb.e("vector", "tensor_tensor", out=ko[:, 8:16], in0=tt[2][:], in1=tt[3][:], op=ALU.add, r=["tt2", "tt3"], w=["kctok"])
                                    else:
                                        kb.e("vector", "tensor_scalar", out=vc_ext[:, nt, g_, 0:64], in0=p_o[:, 0:64], scalar1=cval[:, nt:nt + 1],
                                             scalar2=None, op0=ALU.mult, r=["p_o", "cval"], w=["vc_ext"])
                        for nt in range(NCT):
                            kb.tr(p_t[:, nt * 128:(nt + 1) * 128], kctok[:, nt, :], C["ident_bf"][:], r=["kctok", "cb_ident"], w=["p_t"])
                            kb.act(kcT[:, nt * 128:(nt + 1) * 128], p_t[:, nt * 128:(nt + 1) * 128], AF.Copy, r=["p_t"], w=["kcT"])
                            for g_ in range(2):
                                kb.e("vector", "tensor_copy", out=vc_ext[:, nt, g_, 64:65], in_=cval[:, nt:nt + 1], r=["cval"], w=["vc_ext"])
                                kb.e("vector", "tensor_scalar", out=vc_ext[:, nt, g_, 65:193], in0=C["cover"][:, nt, :], scalar1=cval[:, nt:nt + 1],
                                     scalar2=None, op0=ALU.mult, r=["c_cover", "cval"], w=["vc_ext"])
                        P.end_phase()
                QT = sb(nsa, "QT", [128, 8, NTOK], BF16)
                gates = sb(nsa, "gates", [128, NOT, 48])
                with ExitStack() as st:
                    C = load_consts(st, ["ident"], bf=["ident"])
                    stg = [sb(st, "stgA", [128, 2048]), sb(st, "stgB", [128, 2048])]
                    wQ = sb(st, "wQ", [128, 16, 1072], BF16)
                    xTo = [sb(st, "xTq0", [128, 16, 128], BF16), sb(st, "xTq1", [128, 16, 128], BF16)]
                    qtok = sb(st, "qtok", [128, 1024], BF16)
                    tt = [sb(st, "ropeq%d" % q_, [128, 16, 8]) for q_ in range(4)]
                    p_q = ps(st, "p_q", [128, 1024])
                    p_g = ps(st, "p_g", [128, 512])
                    p_t = ps(st, "p_t", [128, 1024], BF16)
                    for k2 in range(0, 16, 2):
                        src = w_in[k2 * 128:(k2 + 2) * 128, C_Q:C_Q + 1024].rearrange("(k p) n -> p k n", p=128)
                        load_cast(stg, wQ[:, k2:k2 + 2, 0:1024], src, [128, 2, 1024], ["wQ"])
                    srcg = w_in[:, C_GATE:C_GATE + 48].rearrange("(k p) n -> p k n", p=128)
                    load_cast(stg, wQ[:, :, 1024:1072], srcg, [128, 16, 48], ["wQ"])
                    for ot in range(NOT):
                        i, t = ot // 2, ot % 2
                        T = 6 + 8 * i + t
                        xb = xTo[ot % 2]
                        rxb = ("xTq", ot % 2)
                        for k8 in range(0, 16, 8):
                            src = xT[k8 * 128:(k8 + 8) * 128, T * 128:(T + 1) * 128].rearrange("(k p) n -> p k n", p=128)
                            load_cast(stg, xb[:, k8:k8 + 8, :], src, [128, 8, 128], [rxb], eng_cast="scalar")
                        for cc in range(2):
                            for k in range(16):
                                kb.mm(p_q[:, cc * 512:(cc + 1) * 512], xb[:, k, :], wQ[:, k, cc * 512:(cc + 1) * 512], k == 0, k == 15,
                                      r=["wQ", rxb], w=["p_q"])
                        for k in range(16):
                            kb.mm(p_g[:, 0:48], xb[:, k, :], wQ[:, k, 1024:1072], k == 0, k == 15, r=["wQ", rxb], w=["p_g"])
                        kb.act(gates[:, ot, :], p_g[:, 0:48], AF.Tanh, scale=0.5, r=["p_g"], w=["gates"])
                        kb.e("vector", "tensor_scalar", out=gates[:, ot, :], in0=gates[:, ot, :], scalar1=0.5, scalar2=0.5, op0=ALU.mult, op1=ALU.add,
                             r=["gates"], w=["gates"])
                        q4o = qtok[:].rearrange("p (r g d) -> p g r d", r=8, g=2)
                        pq4 = p_q[:].rearrange("p (g r d) -> p g r d", g=2, r=8)
                        kb.act(q4o, pq4, AF.Copy, r=["p_q"], w=["qtok"])
                        x1 = pq4[:, :, :, 0:8]
                        x2 = pq4[:, :, :, 8:16]
                        cb3 = bc(cosT[:, T, :].unsqueeze(1).unsqueeze(1), [128, 2, 8, 8])
                        sb3 = bc(sinT[:, T, :].unsqueeze(1).unsqueeze(1), [128, 2, 8, 8])
                        tv = [t_[:].rearrange("p (g r) e -> p g r e", g=2) for t_ in tt]
                        kb.e("vector", "tensor_tensor", out=tv[0], in0=x1, in1=cb3, op=ALU.mult, r=["p_q"], w=["tt0"])
                        kb.e("vector", "tensor_tensor", out=tv[1], in0=x2, in1=sb3, op=ALU.mult, r=["p_q"], w=["tt1"])
                        kb.e("vector", "tensor_tensor", out=tv[2], in0=x2, in1=cb3, op=ALU.mult, r=["p_q"], w=["tt2"])
                        kb.e("vector", "tensor_tensor", out=tv[3], in0=x1, in1=sb3, op=ALU.mult, r=["p_q"], w=["tt3"])
                        kb.e("vector", "tensor_tensor", out=q4o[:, :, :, 0:8], in0=tv[0], in1=tv[1], op=ALU.subtract, r=["tt0", "tt1"], w=["qtok"])
                        kb.e("vector", "tensor_tensor", out=q4o[:, :, :, 8:16], in0=tv[2], in1=tv[3], op=ALU.add, r=["tt2", "tt3"], w=["qtok"])
                        for r_ in range(8):
                            kb.tr(p_t[:, r_ * 128:(r_ + 1) * 128], qtok[:, r_ * 128:(r_ + 1) * 128], C["ident_bf"][:], r=["qtok", "cb_ident"], w=["p_t"])
                        kb.act(QT[:, :, ot * 128:(ot + 1) * 128], p_t[:].rearrange("p (r q) -> p r q", r=8), AF.Copy, r=["p_t"], w=["QT"])
                    P.end_phase()
                with ExitStack() as st:
                    C = load_consts(st, ["ident", "triL", "triU", "cmpmask", "iota_row", "qhalf"],
                                    bf=["ident", "triL", "triU", "cmpmask"])
                    stgN = [sb(st, "stgA", [128, 2048]), sb(st, "stgB", [128, 2048])]
                    C["ebig_bf"] = sb(st, "ebig_bf", [128, 8192], BF16)
                    for q_ in range(4):
                        load_cast(stgN, C["ebig_bf"][:, q_ * 2048:(q_ + 1) * 2048], cd["ebig"][:, q_ * 2048:(q_ + 1) * 2048], [128, 2048], ["cb_ebig"])
                    f0 = sb(st, "f0", [128, 128])
                    kb.dma(f0[:], forced0_d, w=["f0"])
                    pT = [sb(st, "pT%d" % q_, [128, 1024], BF16) for q_ in range(4)]
                    negc = sb(st, "negc", [128, 6, 4, 128], BF16)
                    negsel = sb(st, "negsel", [128, 4, 128], BF16)
                    selTb = sb(st, "selTb", [128, 128], BF16)
                    for mi, msrc in enumerate([C["triL"][:], C["triU"][:]] + [C["cmpmask"][:, v_, :] for v_ in range(4)]):
                        kb.e("vector", "tensor_scalar", out=negc[:, mi, :, :], in0=bc(msrc.unsqueeze(1), [128, 4, 128]), scalar1=-1.0, scalar2=-NEG,
                             op0=ALU.add, op1=ALU.mult, r=["c_triL", "c_triU", "c_cmpmask"], w=["negc"])
                    accC = sb(st, "accC", [128, 8, 193])
                    accS = sb(st, "accS", [128, 8, 65])
                    accW = sb(st, "accW", [128, 8, 65])
                    rz = sb(st, "rz", [128, 3, 8])
                    imp = sb(st, "imp", [128, 128])
                    imp2 = sb(st, "imp2", [128, 128])
                    vmask = sb(st, "vmask", [128, 128])
                    fmask = sb(st, "fmask", [128, 128])
                    f2 = sb(st, "f2", [128, 128])
                    cur = sb(st, "cur", [128, 2])
                    m8 = sb(st, "m8", [128, 16])
                    selb = sb(st, "selb", [128, 128], BF16)
                    selT = sb(st, "selT", [128, 128], BF16)
                    ynsa = sb(st, "ynsa", [128, 8, 64])
                    ytmp = sb(st, "ytmp", [128, 8, 64])
                    ybf = sb(st, "ybf", [128, 512], BF16)
                    p_s = [ps(st, "p_s0", [128, 1024]), ps(st, "p_s1", [128, 1024])]
                    p_acc = ps(st, "p_acc", [128, 2048])

                    DVE_MASK = lambda u: True

                    def attn_pass(qh, units, vcols, per_bank):
                        nu = len(units)

                        def emit_S(u):
                            kT, vext, mask, rk, rv = units[u]
                            sp = p_s[u % 2]
                            rsp = ("p_s", u % 2)
                            for hh in range(2):
                                dve_mask = (mask is not None) and mask[0] == "expand" and DVE_MASK(u)
                                kb.mm(sp[:, hh * 512:(hh + 1) * 512], kT, qh[hh], True, (mask is None) or dve_mask, r=[rk, "QT"], w=[rsp])
                                if mask is not None:
                                    kind, mval, rm = mask
                                    if kind == "expand" and DVE_MASK(u):
                                        pass
                                    elif kind == "expand":
                                        kb.mm(sp[:, hh * 512:(hh + 1) * 512], mval, negsel[:].rearrange("p r q -> p (r q)"), False, True,
                                              r=["cb_ebig", "negsel"], w=[rsp])
                                    else:
                                        kb.mm(sp[:, hh * 512:(hh + 1) * 512], C["ident_bf"][:], negc[:, mval, :, :].rearrange("p r q -> p (r q)"),
                                              False, True, r=["cb_ident", "negc"], w=[rsp])

                        def emit_PV(u):
                            kT, vext, mask, rk, rv = units[u]
                            pt = pT[u % 4]
                            rpt = ("pT", u % 4)
                            for r_ in range(8):
                                bank = r_ // per_bank
                                col = bank * 512 + (r_ % per_bank) * vcols
                                kb.mm(p_acc[:, col:col + vcols], pt[:, r_ * 128:(r_ + 1) * 128], vext,
                                      (u == 0) and (r_ % per_bank == 0), u == nu - 1, r=[rpt, rv], w=[("p_acc", bank)])

                        emit_S(0)
                        for u in range(nu):
                            kT, vext, mask, rk, rv = units[u]
                            if u + 1 < nu:
                                emit_S(u + 1)
                            pt = pT[u % 4]
                            rpt = ("pT", u % 4)
                            kb.act(pt[:], p_s[u % 2][:], AF.Exp, scale=0.125, r=[("p_s", u % 2)], w=[rpt])
                            if (mask is not None) and mask[0] == "expand" and DVE_MASK(u):
                                mps = p_acc[:, 1024 + (u % 2) * 128:1152 + (u % 2) * 128]
                                kb.mm(mps, mask[1], selTb[:], True, True, r=["cb_ebig", "selTb"], w=[("p_acc", 2)])
                                kb.e("vector", "tensor_tensor", out=pt[:].rearrange("p (r q) -> p r q", r=8),
                                     in0=pt[:].rearrange("p (r q) -> p r q", r=8), in1=bc(mps.unsqueeze(1), [128, 8, 128]),
                                     op=ALU.mult, r=[rpt, ("p_acc", 2)], w=[rpt])
                            if u >= 1:
                                emit_PV(u - 1)
                        emit_PV(nu - 1)

                    for ot in range(NOT):
                        i, t = ot // 2, ot % 2
                        T = 6 + 8 * i + t
                        gv = gates[:, ot, :].rearrange("p (h b) -> p h b", b=3)
                        for g_ in range(2):
                            gs = slice(g_ * 64, (g_ + 1) * 64)
                            qh = [QT[gs, 0:4, ot * 128:(ot + 1) * 128], QT[gs, 4:8, ot * 128:(ot + 1) * 128]]
                            ntl = (8 * T) // 128
                            units = []
                            for nt in range(ntl + 1):
                                mk = None
                                if nt == ntl:
                                    v = 2 * (i % 2) + t
                                    mk = ("const", 2 + v, None)
                                units.append((kcT[gs, nt * 128:(nt + 1) * 128], vc_ext[:, nt, g_, :], mk, "kcT", "vc_ext"))
                            attn_pass(qh, units, 193, 2)
                            for b_ in range(4):
                                kb.act(accC[:, 2 * b_:2 * b_ + 2, :], p_acc[:, b_ * 512:b_ * 512 + 386].rearrange("p (r c) -> p r c", r=2),
                                       AF.Copy, r=[("p_acc", b_)], w=["accC"])
                            kb.e("vector", "tensor_scalar", out=rz[:, 0, :], in0=accC[:, :, 64], scalar1=1e-30, scalar2=None, op0=ALU.max,
                                 r=["accC"], w=["rz"])
                            kb.e("vector", "reciprocal", out=rz[:, 0, :], in_=rz[:, 0, :], r=["rz"], w=["rz"])
                            kb.e("vector", "tensor_scalar", out=imp[:], in0=accC[:, 0, 65:193], scalar1=rz[:, 0, 0:1], scalar2=None, op0=ALU.mult,
                                 r=["accC", "rz"], w=["imp"])
                            for r_ in range(1, 8):
                                kb.e("vector", "scalar_tensor_tensor", out=imp[:], in0=accC[:, r_, 65:193], scalar=rz[:, 0, r_:r_ + 1], in1=imp[:],
                                     op0=ALU.mult, op1=ALU.add, r=["accC", "rz", "imp"], w=["imp"])
                            kb.e("vector", "tensor_scalar", out=cur[:, 0:1], in0=C["qhalf"][:], scalar1=float(2 * T), scalar2=None, op0=ALU.add,
                                 r=["c_qhalf"], w=["cur"])
                            kb.e("vector", "tensor_scalar", out=cur[:, 1:2], in0=C["qhalf"][:], scalar1=float(2 * T - 1), scalar2=None, op0=ALU.add,
                                 r=["c_qhalf"], w=["cur"])
                            kb.e("vector", "tensor_scalar", out=vmask[:], in0=C["iota_row"][:], scalar1=cur[:, 0:1], scalar2=None, op0=ALU.is_le,
                                 r=["c_iota_row", "cur"], w=["vmask"])
                            kb.e("vector", "tensor_scalar", out=fmask[:], in0=C["iota_row"][:], scalar1=cur[:, 0:1], scalar2=None, op0=ALU.is_equal,
                                 r=["c_iota_row", "cur"], w=["fmask"])
                            kb.e("vector", "tensor_scalar", out=f2[:], in0=C["iota_row"][:], scalar1=cur[:, 1:2], scalar2=None, op0=ALU.is_equal,
                                 r=["c_iota_row", "cur"], w=["f2"])
                            kb.e("vector", "tensor_tensor", out=fmask[:], in0=fmask[:], in1=f2[:], op=ALU.add, r=["fmask", "f2"], w=["fmask"])
                            kb.e("vector", "tensor_tensor", out=fmask[:], in0=fmask[:], in1=f0[:], op=ALU.add, r=["fmask", "f0"], w=["fmask"])
                            kb.e("vector", "tensor_tensor", out=fmask[:], in0=fmask[:], in1=vmask[:], op=ALU.add, r=["fmask", "vmask"], w=["fmask"])
                            kb.e("vector", "tensor_scalar", out=fmask[:], in0=fmask[:], scalar1=-1.0, scalar2=1.0e4, op0=ALU.add, op1=ALU.mult,
                                 r=["fmask"], w=["fmask"])
                            kb.e("vector", "tensor_tensor", out=imp[:], in0=imp[:], in1=vmask[:], op=ALU.mult, r=["imp", "vmask"], w=["imp"])
                            kb.e("vector", "tensor_tensor", out=imp[:], in0=imp[:], in1=fmask[:], op=ALU.add, r=["imp", "fmask"], w=["imp"])
                            kb.e("vector", "max", out=m8[:, 0:8], in_=imp[:], r=["imp"], w=["m8"])
                            kb.e("vector", "match_replace", out=imp2[:], in_to_replace=m8[:, 0:8], in_values=imp[:], imm_value=-1.0e30,
                                 r=["imp", "m8"], w=["imp2"])
                            kb.e("vector", "max", out=m8[:, 8:16], in_=imp2[:], r=["imp2"], w=["m8"])
                            kb.e("vector", "tensor_scalar", out=selb[:], in0=imp[:], scalar1=m8[:, 15:16], scalar2=None, op0=ALU.is_ge,
                                 r=["imp", "m8"], w=["selb"])
                            pst = p_acc[:, 1536:2048].bitcast(BF16)
                            kb.tr(pst[:, 0:128], selb[:], C["ident_bf"][:], r=["selb", "cb_ident"], w=[("p_acc", 3)])
                            kb.act(selTb[:], pst[:, 0:128], AF.Copy, r=[("p_acc", 3)], w=["selTb"])
                            kb.e("vector", "tensor_scalar", out=negsel[:], in0=bc(pst[:, 0:128].unsqueeze(1), [128, 4, 128]), scalar1=-1.0, scalar2=-NEG,
                                 op0=ALU.add, op1=ALU.mult, r=[("p_acc", 3)], w=["negsel"])
                            kb.e("vector", "tensor_tensor", out=rz[:, 0, :], in0=rz[:, 0, :], in1=gv[:, g_ * 8:(g_ + 1) * 8, 0], op=ALU.mult,
                                 r=["rz", "gates"], w=["rz"])
                            kb.e("vector", "tensor_tensor", out=ynsa[:], in0=accC[:, :, 0:64], in1=bc(rz[:, 0, :].unsqueeze(2), [128, 8, 64]),
                                 op=ALU.mult, r=["accC", "rz"], w=["ynsa"])
                            for br, (Kt, Vt, acc_sb, kts) in ((1, (KwinT, Vwin, accW, list(range(T - 4, T + 1)))),
                                                              (0, (KselT, Vsel, accS, list(range(0, T + 1))))):
                                units = []
                                for kt in kts:
                                    if br == 0:
                                        mk = ("expand", C["ebig_bf"][:, kt * 128:(kt + 1) * 128], None) if kt < T else ("const", 0, None)
                                    else:
                                        mk = ("const", 1, None) if kt == T - 4 else (("const", 0, None) if kt == T else None)
                                    units.append((Kt[gs, kt * 128:(kt + 1) * 128], Vt[:, kt, g_, :], mk, "KselT" if br == 0 else "KwinT",
                                                  "Vsel" if br == 0 else "Vwin"))
                                attn_pass(qh, units, 65, 4)
                                for b_ in range(2):
                                    kb.act(acc_sb[:, 4 * b_:4 * b_ + 4, :], p_acc[:, b_ * 512:b_ * 512 + 260].rearrange("p (r c) -> p r c", r=4),
                                           AF.Copy, r=[("p_acc", b_)], w=["acc%d" % br])
                                kb.e("vector", "tensor_scalar", out=rz[:, 1 + br, :], in0=acc_sb[:, :, 64], scalar1=1e-30, scalar2=None, op0=ALU.max,
                                     r=["acc%d" % br], w=["rz"])
                                kb.e("vector", "reciprocal", out=rz[:, 1 + br, :], in_=rz[:, 1 + br, :], r=["rz"], w=["rz"])
                                kb.e("vector", "tensor_tensor", out=rz[:, 1 + br, :], in0=rz[:, 1 + br, :], in1=gv[:, g_ * 8:(g_ + 1) * 8, 1 + br], op=ALU.mult,
                                     r=["rz", "gates"], w=["rz"])
                                kb.e("vector", "tensor_tensor", out=ytmp[:], in0=acc_sb[:, :, 0:64], in1=bc(rz[:, 1 + br, :].unsqueeze(2), [128, 8, 64]),
                                     op=ALU.mult, r=["acc%d" % br, "rz"], w=["ytmp"])
                                kb.e("vector", "tensor_tensor", out=ynsa[:], in0=ynsa[:], in1=ytmp[:], op=ALU.add, r=["ynsa", "ytmp"], w=["ynsa"])
                            kb.e("vector", "tensor_copy", out=ybf[:], in_=ynsa[:].rearrange("p r d -> p (r d)"), r=["ynsa"], w=["ybf"])
                            kb.dma(mix_d[ot * 128:(ot + 1) * 128, g_ * 512:(g_ + 1) * 512], ybf[:], r=["ybf"], w=["mix_d"])
                    P.end_phase()

        def dump_mix():
            with ExitStack() as st:
                t32 = sb(st, "t32", [128, 2048])
                tb = sb(st, "tb", [128, 2048], BF16)
                for tt_ in range(NOT):
                    kb.dma(tb[:], mix_d[tt_ * 128:(tt_ + 1) * 128, :], w=["tb"])
                    kb.e("vector", "tensor_copy", out=t32[:], in_=tb[:], r=["tb"], w=["t32"])
                    kb.dma(out_d[tt_ * 128:(tt_ + 1) * 128, :], t32[:], r=["t32"], w=["out"])
                P.end_phase()


        def layer_norm_tile(rin, outt, gt, bt, stats, mv, rres, wres):
            for c4 in range(4):
                kb.e("vector", "bn_stats", out=stats[:, c4, :], in_=rin[:, c4 * 512:(c4 + 1) * 512], r=[rres], w=["lnstats"])
            kb.e("vector", "bn_aggr", out=mv[:, 0:2], in_=stats[:].rearrange("p a b -> p (a b)"), r=["lnstats"], w=["lnmv"])
            kb.e("vector", "tensor_scalar", out=mv[:, 2:3], in0=mv[:, 1:2], scalar1=EPS, scalar2=None, op0=ALU.add, r=["lnmv"], w=["lnmv"])
            kb.act(mv[:, 2:3], mv[:, 2:3], AF.Sqrt, r=["lnmv"], w=["lnmv"])
            kb.e("vector", "reciprocal", out=mv[:, 2:3], in_=mv[:, 2:3], r=["lnmv"], w=["lnmv"])
            kb.e("vector", "tensor_scalar", out=rin, in0=rin, scalar1=mv[:, 0:1], scalar2=mv[:, 2:3], op0=ALU.subtract, op1=ALU.mult,
                 r=[rres, "lnmv"], w=[rres])
            kb.e("vector", "tensor_tensor", out=rin, in0=rin, in1=gt, op=ALU.mult, r=[rres, "lng"], w=[rres])
            kb.e("vector", "tensor_tensor", out=outt, in0=rin, in1=bt, op=ALU.add, r=[rres, "lnb"], w=[wres])

        def moe_all():
            HT = NOT // 2
            with ExitStack() as outer:
                slotidx = sb(outer, "slotidx", [128, NOT, 32])
                Wsp = sb(outer, "Wsp", [128, NOT, 32, 2], BF16)
                with ExitStack() as st:
                    C = load_consts(st, ["ident", "ones", "triS"], bf=["ident", "ones", "triS"])
                    stg = [sb(st, "stgA", [128, 4096]), sb(st, "stgB", [128, 4096])]
                    wo = sb(st, "wo", [128, 16, 2048], BF16)
                    wr = sb(st, "wr", [128, 16, 36])
                    brt = sb(st, "brt", [128, 36])
                    g1 = sb(st, "g1", [128, 2048])
                    b1_ = sb(st, "b1_", [128, 2048])
                    mixb2 = [sb(st, "mixb0", [128, 2048], BF16), sb(st, "mixb1", [128, 2048], BF16)]
                    mixT2 = [sb(st, "mixT0", [128, 16, 128], BF16), sb(st, "mixT1", [128, 16, 128], BF16)]
                    xt2 = [sb(st, "xt0", [128, 2048]), sb(st, "xt1", [128, 2048])]
                    rr2 = [sb(st, "rr0", [128, 2048]), sb(st, "rr1", [128, 2048])]
                    hh = sb(st, "hh", [128, 2048])
                    hb = sb(st, "hb", [128, 2048], BF16)
                    hT32 = sb(st, "hT32", [128, 16, 128])
                    stats = sb(st, "stats", [128, 4, 6])
                    mv = sb(st, "mv", [128, 4])
                    lg = sb(st, "lg", [128, 36])
                    sm_ = sb(st, "smr", [128, 16])
                    ohg = sb(st, "ohg", [128, 4])
                    eg = sb(st, "eg", [128, 4])
                    les = sb(st, "les", [128, 8])
                    m8 = sb(st, "m8", [128, 8])
                    oh1 = sb(st, "oh1", [128, 8])
                    oh2 = sb(st, "oh2", [128, 8])
                    A8 = sb(st, "A8", [128, 8])
                    W8 = sb(st, "W8", [128, 8])
                    Aall = sb(st, "Aall", [128, NOT, 32])
                    Abf = sb(st, "Abf", [128, NOT, 32], BF16)
                    Wall = sb(st, "Wall", [128, NOT, 32])
                    Whi = sb(st, "Whi", [128, NOT, 32], BF16)
                    Wtmp = sb(st, "Wtmp", [128, NOT, 32])
                    p_o = ps(st, "p_o", [128, 2048])
                    p_t = ps(st, "p_t", [128, 1024], BF16)
                    p_r = ps(st, "p_r", [128, 512])
                    p_l = ps(st, "p_l", [128, 512])
                    for k2 in range(0, 16, 2):
                        src = w_out_d[k2 * 128:(k2 + 2) * 128, :].rearrange("(k p) n -> p k n", p=128)
                        load_cast(stg, wo[:, k2:k2 + 2, :], src, [128, 2, 2048], ["wo"])
                    kb.dma(wr[:], w_router_d.rearrange("(k p) n -> p k n", p=128), w=["wr"])
                    kb.dma(brt[:], b_router_d, w=["brt"])
                    kb.dma(g1[:], ln1g_d, w=["lng"])
                    kb.dma(b1_[:], ln1b_d, w=["lnb"])
                    def stage_OA(ot):
                        rows = slice(ot * 128, (ot + 1) * 128)
                        mixb, mixT, xt, rr = mixb2[ot % 2], mixT2[ot % 2], xt2[ot % 2], rr2[ot % 2]
                        RO = lambda n_: (n_, ot % 2)
                        kb.dma(mixb[:], mix_d[rows, :], w=[RO("mixb")])
                        kb.dma(xt[:], x_own[rows, :], w=[RO("xt")])
                        for k8 in range(2):
                            for k in range(8):
                                kk = k8 * 8 + k
                                kb.tr(p_t[:, k * 128:(k + 1) * 128], mixb[:, kk * 128:(kk + 1) * 128], C["ident_bf"][:], r=[RO("mixb"), "cb_ident"], w=["p_t"])
                            kb.act(mixT[:, k8 * 8:(k8 + 1) * 8, :], p_t[:].rearrange("p (k q) -> p k q", k=8), AF.Copy, r=["p_t"], w=[RO("mixT")])
                        for dc in range(4):
                            for k in range(16):
                                kb.mm(p_o[:, dc * 512:(dc + 1) * 512], mixT[:, k, :], wo[:, k, dc * 512:(dc + 1) * 512], k == 0, k == 15,
                                      r=[RO("mixT"), "wo"], w=[("p_o", dc)])
                        kb.e("vector", "scalar_tensor_tensor", out=rr[:], in0=xt[:], scalar=ALPHA, in1=p_o[:], op0=ALU.mult, op1=ALU.add,
                             r=[RO("xt")] + [("p_o", dc) for dc in range(4)], w=[RO("rr")])
                    def stage_OB(ot):
                        rows = slice(ot * 128, (ot + 1) * 128)
                        mixb, mixT, xt, rr = mixb2[ot % 2], mixT2[ot % 2], xt2[ot % 2], rr2[ot % 2]
                        RO = lambda n_: (n_, ot % 2)
                        layer_norm_tile(rr[:], hh[:], g1[:], b1_[:], stats, mv, RO("rr"), "hh")
                        kb.dma(h32_d[rows, :], hh[:], r=["hh"], w=["h32_d"])
                        kb.act(hb[:], hh[:], AF.Copy, r=["hh"], w=["hb"])
                        kb.dma(hbf_d[rows, :], hb[:], r=["hb"], w=["hbf_d"])
                        for k4 in range(4):
                            for k in range(4):
                                kk = k4 * 4 + k
                                kb.tr(p_r[:, k * 128:(k + 1) * 128], hh[:, kk * 128:(kk + 1) * 128], C["ident"][:], r=["hh", "c_ident"], w=["p_r"])
                            kb.act(hT32[:, k4 * 4:(k4 + 1) * 4, :], p_r[:].rearrange("p (k q) -> p k q", k=4), AF.Copy, r=["p_r"], w=["hT32"])
                        for k in range(16):
                            kb.mm(p_l[:, 0:36], hT32[:, k, :], wr[:, k, :], k == 0, k == 15, r=["hT32", "wr"], w=["p_l"])
                        R = ["route"]
                        kb.e("vector", "tensor_tensor", out=lg[:], in0=p_l[:, 0:36], in1=brt[:], op=ALU.add, r=["p_l", "brt"], w=R)
                        kb.e("vector", "tensor_reduce", out=sm_[:, 0:1], in_=lg[:, 0:4], axis=AX.X, op=ALU.max, r=R, w=R)
                        kb.e("vector", "tensor_scalar", out=sm_[:, 1:2], in0=sm_[:, 0:1], scalar1=-1.0, scalar2=None, op0=ALU.mult, r=R, w=R)
                        kb.act(eg[:], lg[:, 0:4], AF.Exp, bias=sm_[:, 1:2], r=R, w=R)
                        kb.e("vector", "tensor_reduce", out=sm_[:, 2:3], in_=eg[:], axis=AX.X, op=ALU.add, r=R, w=R)
                        kb.e("vector", "reciprocal", out=sm_[:, 3:4], in_=sm_[:, 2:3], r=R, w=R)
                        kb.e("vector", "tensor_scalar", out=ohg[:], in0=lg[:, 0:4], scalar1=sm_[:, 0:1], scalar2=None, op0=ALU.is_equal, r=R, w=R)
                        kb.e("vector", "tensor_scalar", out=les[:], in0=lg[:, 4:12], scalar1=ohg[:, 0:1], scalar2=None, op0=ALU.mult, r=R, w=R)
                        for g_ in range(1, 4):
                            kb.e("vector", "scalar_tensor_tensor", out=les[:], in0=lg[:, 4 + 8 * g_:12 + 8 * g_], scalar=ohg[:, g_:g_ + 1], in1=les[:],
                                 op0=ALU.mult, op1=ALU.add, r=R, w=R)
                        kb.e("vector", "max", out=m8[:], in_=les[:], r=R, w=R)
                        kb.e("vector", "tensor_tensor", out=sm_[:, 4:5], in0=m8[:, 1:2], in1=m8[:, 0:1], op=ALU.subtract, r=R, w=R)
                        kb.act(sm_[:, 5:6], sm_[:, 4:5], AF.Exp, r=R, w=R)
                        kb.e("vector", "tensor_scalar", out=sm_[:, 6:7], in0=sm_[:, 5:6], scalar1=1.0, scalar2=None, op0=ALU.add, r=R, w=R)
                        kb.e("vector", "reciprocal", out=sm_[:, 6:7], in_=sm_[:, 6:7], r=R, w=R)
                        kb.e("vector", "tensor_tensor", out=sm_[:, 7:8], in0=sm_[:, 5:6], in1=sm_[:, 6:7], op=ALU.mult, r=R, w=R)
                        kb.e("vector", "tensor_tensor", out=sm_[:, 8:9], in0=sm_[:, 6:7], in1=sm_[:, 3:4], op=ALU.mult, r=R, w=R)
                        kb.e("vector", "tensor_tensor", out=sm_[:, 9:10], in0=sm_[:, 7:8], in1=sm_[:, 3:4], op=ALU.mult, r=R, w=R)
                        kb.e("vector", "tensor_scalar", out=oh1[:], in0=les[:], scalar1=m8[:, 0:1], scalar2=None, op0=ALU.is_equal, r=R, w=R)
                        kb.e("vector", "tensor_scalar", out=oh2[:], in0=les[:], scalar1=m8[:, 1:2], scalar2=None, op0=ALU.is_equal, r=R, w=R)
                        kb.e("vector", "tensor_tensor", out=A8[:], in0=oh1[:], in1=oh2[:], op=ALU.add, r=R, w=R)
                        kb.e("vector", "tensor_scalar", out=W8[:], in0=oh1[:], scalar1=sm_[:, 8:9], scalar2=None, op0=ALU.mult, r=R, w=R)
                        kb.e("vector", "scalar_tensor_tensor", out=W8[:], in0=oh2[:], scalar=sm_[:, 9:10], in1=W8[:], op0=ALU.mult, op1=ALU.add, r=R, w=R)
                        o3 = bc(ohg[:].unsqueeze(2), [128, 4, 8])
                        kb.e("vector", "tensor_tensor", out=Aall[:, ot, :].rearrange("p (g e) -> p g e", g=4), in0=o3,
                             in1=bc(A8[:].unsqueeze(1), [128, 4, 8]), op=ALU.mult, r=R, w=["Aall"])
                        kb.e("vector", "tensor_tensor", out=Wall[:, ot, :].rearrange("p (g e) -> p g e", g=4), in0=o3,
                             in1=bc(W8[:].unsqueeze(1), [128, 4, 8]), op=ALU.mult, r=R, w=["Wall"])
                    stage_OA(0)
                    for ot in range(NOT):
                        if ot + 1 < NOT:
                            stage_OA(ot + 1)
                        stage_OB(ot)
                    kb.e("vector", "tensor_copy", out=Abf[:], in_=Aall[:], r=["Aall"], w=["Abf"])
                    for tt in range(NOT):
                        hf = tt // HT
                        prev = list(range(hf * HT, tt))
                        for n_, tp in enumerate(prev):
                            kb.mm(p_l[:, 64:96], C["ones_bf"][:], Abf[:, tp, :], n_ == 0, False, r=["Abf", "cb_ones"], w=["p_l"])
                        kb.mm(p_l[:, 64:96], C["triS_bf"][:], Abf[:, tt, :], len(prev) == 0, True, r=["Abf", "cb_triS"], w=["p_l"])
                        kb.e("vector", "scalar_tensor_tensor", out=slotidx[:, tt, :], in0=p_l[:, 64:96], scalar=1.0, in1=Aall[:, tt, :],
                             op0=ALU.add, op1=ALU.mult, r=["p_l", "Aall"], w=["slotidx"])
                    kb.e("vector", "tensor_scalar", out=slotidx[:], in0=slotidx[:], scalar1=-1.0, scalar2=None, op0=ALU.add, r=["slotidx"], w=["slotidx"])
                    kb.e("vector", "tensor_copy", out=Whi[:], in_=Wall[:], r=["Wall"], w=["Whi"])
                    kb.e("vector", "tensor_copy", out=Wsp[:, :, :, 0], in_=Whi[:], r=["Whi"], w=["Wsp"])
                    kb.e("vector", "tensor_tensor", out=Wtmp[:], in0=Wall[:], in1=Whi[:], op=ALU.subtract, r=["Wall", "Whi"], w=["Wtmp"])
                    kb.e("vector", "tensor_copy", out=Wsp[:, :, :, 1], in_=Wtmp[:], r=["Wtmp"], w=["Wsp"])
                    P.end_phase()
                if stop == "h":
                    with ExitStack() as st:
                        t32 = sb(st, "t32", [128, 2048])
                        for tt_ in range(NOT):
                            kb.dma(t32[:], h32_d[tt_ * 128:(tt_ + 1) * 128, :], w=["t32"])
                            kb.dma(out_d[tt_ * 128:(tt_ + 1) * 128, :], t32[:], r=["t32"], w=["out"])
                        P.end_phase()
                    return
                for hf in range(2):
                    with ExitStack() as accst:
                        acc = sb(accst, "acc", [128, HT, 2048])
                        with ExitStack() as st:
                            C = load_consts(st, ["ident", "iota_row"], bf=["ident"])
                            stg = [sb(st, "stgA", [128, 2048]), sb(st, "stgB", [128, 2048]), sb(st, "stgC", [128, 2048]), sb(st, "stgD", [128, 2048])]
                            hbf = sb(st, "hbf", [128, HT, 2048], BF16)
                            wg = sb(st, "wg", [128, 16, 512], BF16)
                            wu = sb(st, "wu", [128, 16, 512], BF16)
                            wd = sb(st, "wd", [128, 4, 2048], BF16)
                            S = sb(st, "S", [128, HT, 128], BF16)
                            ST2 = [sb(st, "ST0", [128, HT * 128], BF16), sb(st, "ST1", [128, HT * 128], BF16)]
                            XeT = sb(st, "XeT", [128, 16, 128], BF16)
                            sgl = sb(st, "sgl", [128, 512])
                            aT2 = [sb(st, "aT0", [128, 512], BF16), sb(st, "aT1", [128, 512], BF16)]
                            wsl2 = [sb(st, "wsl0", [128, 4]), sb(st, "wsl1", [128, 4])]
                            Yw = sb(st, "Yw", [128, 2048], BF16)
                            pg = [ps(st, "p_g0", [128, 512]), ps(st, "p_g1", [128, 512])]
                            pG = ps(st, "p_G", [128, 512])
                            pU = ps(st, "p_U", [128, 512])
                            pY = ps(st, "p_Y", [128, 2048])
                            pUb = pU[:, :].bitcast(BF16)
                            kb.e("vector", "memset", acc[:].rearrange("p a b -> p (a b)"), 0.0, w=[("acc", 0), ("acc", 1)])
                            for tt in range(HT):
                                kb.dma(hbf[:, tt, :], hbf_d[(hf * HT + tt) * 128:(hf * HT + tt + 1) * 128, :], w=["hbf"])
                            def load_gu(e_):
                                for q in range(4):
                                    srcg = w_gate_d[e_, q * 512:(q + 1) * 512, :].rearrange("(p k) n -> p k n", k=4)
                                    load_cast(stg, wg[:, 4 * q:4 * q + 4, :], srcg, [128, 4, 512], ["wg"], eng_cast="scalar")
                                for q in range(4):
                                    srcu = w_up_d[e_, q * 512:(q + 1) * 512, :].rearrange("(p k) n -> p k n", k=4)
                                    load_cast(stg, wu[:, 4 * q:4 * q + 4, :], srcu, [128, 4, 512], ["wu"], eng_cast="scalar")

                            def load_d(e_):
                                for q in range(4):
                                    srcd = w_down_d[e_, q * 128:(q + 1) * 128, :]
                                    load_cast(stg, wd[:, q, :], srcd, [128, 2048], ["wd"], eng_cast="vector")

                            def stage_A(e_):
                                par = e_ % 2
                                STp, aTp, wslp = ST2[par], aT2[par], wsl2[par]
                                for tt in range(HT):
                                    kb.e("vector", "tensor_scalar", out=S[:, tt, :], in0=C["iota_row"][:], scalar1=slotidx[:, hf * HT + tt, e_:e_ + 1],
                                         scalar2=None, op0=ALU.is_equal, r=["c_iota_row", "slotidx"], w=["S"])
                                for tt in range(HT):
                                    kb.tr(pUb[:, tt * 128:(tt + 1) * 128], S[:, tt, :], C["ident_bf"][:], r=["S", "cb_ident"], w=["p_U"])
                                kb.act(STp[:], pUb[:, 0:HT * 128], AF.Copy, r=["p_U"], w=[("ST", par)])
                                for tt in range(HT):
                                    kb.mm(pG[:, 0:2], S[:, tt, :], Wsp[:, hf * HT + tt, e_, :], tt == 0, tt == HT - 1, r=["S", "Wsp"], w=["p_G"])
                                kb.e("vector", "tensor_reduce", out=wslp[:, 0:1], in_=pG[:, 0:2], axis=AX.X, op=ALU.add, r=["p_G"], w=[("wsl", par)])
                                for q in range(4):
                                    pgq = pg[q % 2]
                                    for d4 in range(4):
                                        dk = 4 * q + d4
                                        for tt in range(HT):
                                            kb.mm(pgq[:, d4 * 128:(d4 + 1) * 128], hbf[:, tt, q * 512 + d4:(q + 1) * 512:4], S[:, tt, :],
                                                  (tt == 0) and (d4 == 0), tt == HT - 1, r=["hbf", "S"], w=[("p_g", q % 2)])
                                    kb.act(XeT[:, 4 * q:4 * q + 4, :], pgq[:].rearrange("p (a b) -> p a b", a=4), AF.Copy, r=[("p_g", q % 2)], w=["XeT"])
                                for (pp, ww, rp, rw) in ((pG, wg, "p_G", "wg"), (pU, wu, "p_U", "wu")):
                                    for hc in range(4):
                                        for dk in range(16):
                                            kb.mm(pp[:, hc * 128:(hc + 1) * 128], ww[:, dk, hc * 128:(hc + 1) * 128], XeT[:, dk, :],
                                                  (dk == 0) and (hc == 0), dk == 15, r=[rw, "XeT"], w=[rp])
                                kb.act(sgl[:], pG[:], AF.Silu, r=["p_G"], w=["sgl"])
                                kb.e("vector", "tensor_tensor", out=aTp[:], in0=sgl[:], in1=pU[:], op=ALU.mult, r=["sgl", "p_U"], w=[("aT", par)])

                            def stage_B(e_):
                                par = e_ % 2
                                STp, aTp, wslp = ST2[par], aT2[par], wsl2[par]
                                for dc in range(4):
                                    for hc in range(4):
                                        kb.mm(pY[:, dc * 512:(dc + 1) * 512], aTp[:, hc * 128:(hc + 1) * 128], wd[:, hc, dc * 512:(dc + 1) * 512],
                                              hc == 0, hc == 3, r=[("aT", par), "wd"], w=[("p_Y", dc)])
                                if e_ + 1 < E_N:
                                    load_d(e_ + 1)
                                for h2 in range(2):
                                    kb.act(Yw[:, h2 * 1024:(h2 + 1) * 1024], pY[:, h2 * 1024:(h2 + 1) * 1024], AF.Identity, scale=wslp[:, 0:1],
                                           r=[("p_Y", 2 * h2), ("p_Y", 2 * h2 + 1), ("wsl", par)], w=[("Yw", h2)])
                                for tt in range(HT):
                                    for h2 in range(2):
                                        for dc in range(2 * h2, 2 * h2 + 2):
                                            kb.mm(pY[:, dc * 512:(dc + 1) * 512], STp[:, tt * 128:(tt + 1) * 128], Yw[:, dc * 512:(dc + 1) * 512],
                                                  True, True, r=[("ST", par), ("Yw", h2)], w=[("p_Y", dc)])
                                        kb.e("vector", "tensor_tensor", out=acc[:, tt, h2 * 1024:(h2 + 1) * 1024], in0=acc[:, tt, h2 * 1024:(h2 + 1) * 1024],
                                             in1=pY[:, h2 * 1024:(h2 + 1) * 1024], op=ALU.add,
                                             r=[("acc", h2), ("p_Y", 2 * h2), ("p_Y", 2 * h2 + 1)], w=[("acc", h2)])

                            load_gu(0)
                            load_d(0)
                            stage_A(0)
                            for e_ in range(E_N):
                                if e_ + 1 < E_N:
                                    load_gu(e_ + 1)
                                    stage_A(e_ + 1)
                                stage_B(e_)
                            P.end_phase()
                        with ExitStack() as st:
                            g2 = sb(st, "g2", [128, 2048])
                            b2_ = sb(st, "b2_", [128, 2048])
                            ht = [sb(st, "ht0", [128, 2048]), sb(st, "ht1", [128, 2048])]
                            oo = [sb(st, "oo0", [128, 2048]), sb(st, "oo1", [128, 2048])]
                            stats = sb(st, "stats", [128, 4, 6])
                            mv = sb(st, "mv", [128, 4])
                            kb.dma(g2[:], ln2g_d, w=["lng"])
                            kb.dma(b2_[:], ln2b_d, w=["lnb"])
                            for tt in range(HT):
                                rows = slice((hf * HT + tt) * 128, (hf * HT + tt + 1) * 128)
                                hx = ht[tt % 2]
                                ox = oo[tt % 2]
                                kb.dma(hx[:], h32_d[rows, :], w=[("ht", tt % 2)])
                                kb.e("vector", "scalar_tensor_tensor", out=hx[:], in0=hx[:], scalar=ALPHA, in1=acc[:, tt, :], op0=ALU.mult, op1=ALU.add,
                                     r=[("ht", tt % 2), "acc"], w=[("ht", tt % 2)])
                                layer_norm_tile(hx[:], ox[:], g2[:], b2_[:], stats, mv, ("ht", tt % 2), ("oo", tt % 2))
                                kb.dma(out_d[rows, :], ox[:], r=[("oo", tt % 2)], w=["out"])
                            P.end_phase()

        phase_S1()
        phase_S2()
        if stop == "mix":
            nsa_all()
            dump_mix()
            return nc
        if stop == "ssm":
            with ExitStack() as st:
                t32 = sb(st, "t32", [128, 2048])
                tb = sb(st, "tb", [128, 2048], BF16)
                for tt in range(NOT):
                    kb.dma(tb[:, 1024:2048], mix_d[tt * 128:(tt + 1) * 128, 1024:2048], w=["tb"])
                    kb.e("vector", "memset", t32[:, 0:1024], 0.0, w=["t32"])
                    kb.e("vector", "tensor_copy", out=t32[:, 1024:2048], in_=tb[:, 1024:2048], r=["tb"], w=["t32"])
                    kb.dma(out_d[tt * 128:(tt + 1) * 128, :], t32[:], r=["t32"], w=["out"])
                P.end_phase()
            return nc
        nsa_all()
        moe_all()
    return nc


def run(inputs, SEQ, B, stop="all", debug=False):
    maps = prepare_inputs(inputs, SEQ, B)
    nc = build(SEQ, stop=stop, debug=debug)
    if stop != "all":
        used = None
    res = run_bass_kernel_spmd(nc, maps, core_ids=list(range(4 * B)))
    NOWN = SEQ // 1024
    out = np.zeros((B, SEQ, D), np.float32)
    for b in range(B):
        for j in range(4):
            o = np.asarray(res.results[b * 4 + j]["out"])
            for i in range(NOWN):
                c = 4 * i + j
                out[b, c * 256:(c + 1) * 256] = o[i * 256:(i + 1) * 256]
    return out, res


def kernel(**inputs):
    out, _ = run(inputs, 8192, 2)
    return out
```
